# Optimizing a Trainium2 kernel written in Bass

```python
import jax, jax.numpy as jnp
from jax import lax
import numpy as np

D_MODEL = 1024
BATCH = 16
SEQ = 4096
DEPTH = 1

CONV_W = D_MODEL
CONV_K = 3
M_HEADS = 8
M_V = D_MODEL
M_DV = M_V // M_HEADS
M_DK = M_DV // 2
M_QK = M_HEADS * M_DK
QK_CONV_K = 4
CHUNK = 64
P_HEADS = 8
N_KEYS = 128
N_EXPERTS = N_KEYS * N_KEYS
P_TOPK = 16
P_DQ = 256
P_DKH = P_DQ // 2
P_BLOCK = 128
EPS = 1e-6

_IN_SIZES = (CONV_W, CONV_W, CONV_W, M_QK, M_QK, M_V, M_V, M_HEADS, M_HEADS, D_MODEL, D_MODEL)
IN_WIDTH = sum(_IN_SIZES)
SPLIT_IDX = tuple(int(s) for s in np.cumsum(_IN_SIZES)[:-1])

kernel_name = "hybrid_conv_mlstm_peer_block"


def rms_norm(x, w):
    xf = x.astype(jnp.float32)
    y = xf * lax.rsqrt(jnp.mean(xf * xf, axis=-1, keepdims=True) + EPS)
    return (y * w.astype(jnp.float32)).astype(x.dtype)


def modulate(h, shift, scale):
    return h * (1 + scale[:, None, :]) + shift[:, None, :]


def causal_dwconv(u, w):
    K = w.shape[0]
    S = u.shape[1]
    up = jnp.pad(u, ((0, 0), (K - 1, 0), (0, 0)))
    return sum(up[:, j:j + S] * w[j] for j in range(K))


def mlstm_chunkwise(q, k, v, i_pre, f_pre):
    B, H, S, DK = q.shape
    DV = v.shape[-1]
    NC = S // CHUNK
    q = q * (DK ** -0.5)
    logf = jax.nn.log_sigmoid(f_pre)

    def chunks(t):
        return jnp.moveaxis(t.reshape(B, H, NC, CHUNK, *t.shape[3:]), 2, 0)

    xs = (chunks(q), chunks(k), chunks(v), chunks(i_pre), chunks(logf))
    mask = jnp.tril(jnp.ones((CHUNK, CHUNK), dtype=bool))

    def step(carry, inp):
        C, n, m = carry
        qb, kb, vb, ib, lfb = inp
        b = jnp.cumsum(lfb, axis=-1)
        dlog = b[..., :, None] - b[..., None, :] + ib[..., None, :]
        dlog = jnp.where(mask, dlog, -jnp.inf)
        inter = b + m[..., None]
        m_t = jnp.maximum(inter, jnp.max(dlog, axis=-1))
        w_ts = jnp.exp(dlog - m_t[..., None])
        s_qk = jnp.einsum('bhtd,bhsd->bhts', qb, kb) * w_ts
        a_inter = jnp.exp(inter - m_t)
        num = jnp.einsum('bhts,bhsv->bhtv', s_qk, vb) + a_inter[..., None] * jnp.einsum('bhtd,bhdv->bhtv', qb, C)
        den = jnp.sum(s_qk, axis=-1) + a_inter * jnp.einsum('bhtd,bhd->bht', qb, n)
        h = num / jnp.maximum(jnp.abs(den), jnp.exp(-m_t))[..., None]
        b_last = b[..., -1]
        g = b_last[..., None] - b + ib
        m_new = jnp.maximum(b_last + m, jnp.max(g, axis=-1))
        decay = jnp.exp(b_last + m - m_new)
        ws = jnp.exp(g - m_new[..., None])
        C_new = decay[..., None, None] * C + jnp.einsum('bhs,bhsd,bhsv->bhdv', ws, kb, vb)
        n_new = decay[..., None] * n + jnp.einsum('bhs,bhsd->bhd', ws, kb)
        return (C_new, n_new, m_new), h

    init = (jnp.zeros((B, H, DK, DV), jnp.float32), jnp.zeros((B, H, DK), jnp.float32), jnp.zeros((B, H), jnp.float32))
    _, hs = lax.scan(step, init, xs)
    return jnp.moveaxis(hs, 0, 2).reshape(B, H, S, DV)


def token_mixer(h, w_in, conv_a_w, conv_qk_w, b_igate, b_fgate, mh_norm_w, w_branch_a, w_branch_m, w_out):
    B, S, _ = h.shape
    proj = h @ w_in
    c_gate, b_gate, x_in, q, k, v, o, i_pre, f_pre, ga, gm = jnp.split(proj, SPLIT_IDX, axis=-1)
    ya = b_gate * causal_dwconv(c_gate * x_in, conv_a_w)
    qk = jax.nn.silu(causal_dwconv(jnp.concatenate([q, k], axis=-1), conv_qk_w))
    q, k = jnp.split(qk, 2, axis=-1)

    def to_heads(t, d):
        return t.reshape(B, S, M_HEADS, d).transpose(0, 2, 1, 3).astype(jnp.float32)

    ig = (i_pre + b_igate).transpose(0, 2, 1).astype(jnp.float32)
    fg = (f_pre + b_fgate).transpose(0, 2, 1).astype(jnp.float32)
    hm = mlstm_chunkwise(to_heads(q, M_DK), to_heads(k, M_DK), to_heads(v, M_DV), ig, fg)
    hm = hm * lax.rsqrt(jnp.mean(hm * hm, axis=-1, keepdims=True) + EPS)
    hm = hm.transpose(0, 2, 1, 3).reshape(B, S, M_V) * mh_norm_w.astype(jnp.float32)
    ym = jax.nn.sigmoid(o) * hm.astype(h.dtype)
    mix = jax.nn.sigmoid(ga) * (ya @ w_branch_a) + jax.nn.sigmoid(gm) * (ym @ w_branch_m)
    return mix @ w_out


def peer(h, wq, subkeys, u, v):
    B, S, D = h.shape
    T = B * S
    ht = h.reshape(T, D)
    q = (ht @ wq).reshape(T, P_HEADS, 2, P_DKH)
    s = jnp.einsum('thcd,hcnd->thcn', q, subkeys).astype(jnp.float32)
    s1, i1 = lax.top_k(s[:, :, 0], P_TOPK)
    s2, i2 = lax.top_k(s[:, :, 1], P_TOPK)
    cand = (s1[..., :, None] + s2[..., None, :]).reshape(T, P_HEADS, P_TOPK * P_TOPK)
    sc, ci = lax.top_k(cand, P_TOPK)
    e = jnp.take_along_axis(i1, ci // P_TOPK, axis=-1) * N_KEYS + jnp.take_along_axis(i2, ci % P_TOPK, axis=-1)
    g = jax.nn.softmax(sc, axis=-1)
    nb = T // P_BLOCK

    def block(args):
        hb, eb, gb = args
        ub = jnp.take(u, eb, axis=0)
        z = jnp.einsum('thkd,td->thk', ub, hb)
        a = (jax.nn.gelu(z.astype(jnp.float32)) * gb).astype(hb.dtype)
        return jnp.einsum('thk,thkd->td', a, jnp.take(v, eb, axis=0))

    out = lax.map(block, (ht.reshape(nb, P_BLOCK, D), e.reshape(nb, P_BLOCK, P_HEADS, P_TOPK), g.reshape(nb, P_BLOCK, P_HEADS, P_TOPK)))
    return out.reshape(B, S, D).astype(h.dtype)


def setup_inputs(seed: int = 0) -> dict:
    key = jax.random.key(seed)
    ks = jax.random.split(key, 24)
    L, D = DEPTH, D_MODEL

    def nrm(k, shape, scale):
        return jax.random.normal(k, shape, jnp.float32) * scale

    return {
        "x": nrm(ks[0], (BATCH, SEQ, D), 1.0),
        "c": nrm(ks[1], (BATCH, D), 1.0),
        "w_ada": nrm(ks[2], (L, D, 6 * D), 0.2 * D ** -0.5),
        "b_ada": nrm(ks[3], (L, 6 * D), 0.02),
        "norm1_pre": 1.0 + nrm(ks[4], (L, D), 0.05),
        "norm1_post": 1.0 + nrm(ks[5], (L, D), 0.05),
        "w_in": nrm(ks[6], (L, D, IN_WIDTH), D ** -0.5),
        "conv_a_w": nrm(ks[7], (L, CONV_K, CONV_W), CONV_K ** -0.5),
        "conv_qk_w": nrm(ks[8], (L, QK_CONV_K, 2 * M_QK), QK_CONV_K ** -0.5),
        "b_igate": nrm(ks[9], (L, M_HEADS), 0.1),
        "b_fgate": jnp.linspace(3.0, 6.0, M_HEADS, dtype=jnp.float32)[None, :] + nrm(ks[10], (L, M_HEADS), 0.1),
        "mh_norm_w": 1.0 + nrm(ks[11], (L, M_V), 0.05),
        "w_branch_a": nrm(ks[12], (L, CONV_W, D), CONV_W ** -0.5),
        "w_branch_m": nrm(ks[13], (L, M_V, D), M_V ** -0.5),
        "w_out": nrm(ks[14], (L, D, D), D ** -0.5),
        "norm2_pre": 1.0 + nrm(ks[15], (L, D), 0.05),
        "norm2_post": 1.0 + nrm(ks[16], (L, D), 0.05),
        "peer_wq": nrm(ks[17], (L, D, P_HEADS * P_DQ), D ** -0.5),
        "peer_subkeys": nrm(ks[18], (L, P_HEADS, 2, N_KEYS, P_DKH), P_DKH ** -0.5),
        "peer_u": nrm(ks[19], (L, N_EXPERTS, D), D ** -0.5),
        "peer_v": nrm(ks[20], (L, N_EXPERTS, D), D ** -0.5),
    }


def reference(x, c, w_ada, b_ada, norm1_pre, norm1_post, w_in, conv_a_w, conv_qk_w, b_igate, b_fgate, mh_norm_w, w_branch_a, w_branch_m, w_out, norm2_pre, norm2_post, peer_wq, peer_subkeys, peer_u, peer_v):
    for l in range(DEPTH):
        ada = jax.nn.silu(c) @ w_ada[l] + b_ada[l]
        sh1, sc1, g1, sh2, sc2, g2 = jnp.split(ada, 6, axis=-1)
        h = modulate(rms_norm(x, norm1_pre[l]), sh1, sc1)
        y = token_mixer(h, w_in[l], conv_a_w[l], conv_qk_w[l], b_igate[l], b_fgate[l], mh_norm_w[l], w_branch_a[l], w_branch_m[l], w_out[l])
        x = x + g1[:, None, :] * rms_norm(y, norm1_post[l])
        h = modulate(rms_norm(x, norm2_pre[l]), sh2, sc2)
        y = peer(h, peer_wq[l], peer_subkeys[l], peer_u[l], peer_v[l])
        x = x + g2[:, None, :] * rms_norm(y, norm2_post[l])
    return x
```

```python
from contextlib import ExitStack
import numpy as np
import concourse.bass as bass
import concourse.mybir as mybir
from concourse.bass_utils import run_bass_kernel_spmd

F32 = mybir.dt.float32
BF16 = mybir.dt.bfloat16
U32 = mybir.dt.uint32
I32 = mybir.dt.int32
AF = mybir.ActivationFunctionType
ALU = mybir.AluOpType
AX = mybir.AxisListType

D = 1024
NCORES = 8
SEQ = 4096
NSEQ_CORE = 2
IN_W = 8208
EPS = 1e-6
NEG = -1.0e30

O_CG, O_BG, O_XIN, O_Q, O_K, O_V, O_O, O_I, O_F, O_GA, O_GM = 0, 1024, 2048, 3072, 3584, 4096, 5120, 6144, 6152, 6160, 7184

WBLOCKS = []
for c0 in range(0, 6144, 512):
    WBLOCKS.append(("w_in", c0, 512))
WBLOCKS.append(("w_in", 6144, 16))
for c0 in range(6160, 8208, 512):
    WBLOCKS.append(("w_in", c0, 512))
for nm in ("w_branch_a", "w_branch_m", "w_out"):
    for c0 in (0, 512):
        WBLOCKS.append((nm, c0, 512))
for c0 in range(0, 2048, 512):
    WBLOCKS.append(("peer_wq", c0, 512))
NBLK = len(WBLOCKS)
BLK = {(nm, c0): i for i, (nm, c0, n) in enumerate(WBLOCKS)}


class K:
    def __init__(self, nc, es):
        self.nc = nc
        self.es = es
        self.eng = {"pe": nc.tensor, "act": nc.scalar, "dve": nc.vector, "pool": nc.gpsimd, "sp": nc.sync}
        self.esem = {e: es.enter_context(nc.semaphore("sem_" + e)) for e in self.eng}
        self.ecnt = {e: 0 for e in self.eng}
        self.semobj = {("e", e): self.esem[e] for e in self.eng}
        self.waited = {e: {} for e in self.eng}
        self.tr = {}
        self.dsem = {}
        self.dcnt = {}
        self.same_engine_sync = {"pe": False, "act": True, "dve": True, "pool": True, "sp": True}
        self.rec = None

    def _reg(self, h, name):
        self.tr[name] = {"w": {}, "r": {}}
        return h

    def sb(self, name, shape, dt, es=None):
        return self._reg((es or self.es).enter_context(self.nc.sbuf_tensor(name, shape, dt)), name)

    def ps(self, name, shape, dt):
        return self._reg(self.es.enter_context(self.nc.psum_tensor(name, shape, dt)), name)

    def dram(self, name, shape, dt, kind):
        t = self.nc.dram_tensor(name, shape, dt, kind=kind)
        self.tr[name] = {"w": {}, "r": {}}
        return t.ap()

    def _rec(self, ap):
        return self.tr[ap.tensor.name if hasattr(ap, "tensor") else ap.name]

    def _name(self, ap):
        try:
            return ap.tensor.name
        except Exception:
            return ap.name

    def _deps(self, reads, writes):
        ev = {}
        for ap in reads:
            rec = self.tr[self._name(ap)]
            for k, v in rec["w"].items():
                ev[k] = max(ev.get(k, 0), v)
        for ap in writes:
            rec = self.tr[self._name(ap)]
            for k, v in rec["w"].items():
                ev[k] = max(ev.get(k, 0), v)
            for k, v in rec["r"].items():
                ev[k] = max(ev.get(k, 0), v)
        return ev

    def _wait(self, e, ev):
        for k, v in ev.items():
            if k == ("e", e) and not self.same_engine_sync[e]:
                continue
            if self.waited[e].get(k, 0) < v:
                self.eng[e].wait_ge(self.semobj[k], v)
                self.waited[e][k] = v

    def _mark(self, key, val, reads, writes):
        for ap in reads:
            rec = self.tr[self._name(ap)]
            rec["r"][key] = max(rec["r"].get(key, 0), val)
        for ap in writes:
            rec = self.tr[self._name(ap)]
            rec["w"][key] = max(rec["w"].get(key, 0), val)

    def op(self, e, fn, reads, writes):
        if self.rec is not None:
            self.rec.append(("op", e, fn, list(reads), list(writes)))
            return None
        self._wait(e, self._deps(reads, writes))
        ins = fn(self.eng[e])
        self.ecnt[e] += 1
        ins.then_inc(self.esem[e], 1)
        self._mark(("e", e), self.ecnt[e], reads, writes)
        return ins

    def dma(self, e, out, in_, fn=None, extra_reads=(), **kw):
        if self.rec is not None:
            self.rec.append(("dma", e, out, in_, fn, list(extra_reads), kw))
            return None
        rds = [in_] + list(extra_reads)
        self._wait(e, self._deps(rds, [out]))
        dn = self._name(out)
        if dn not in self.dsem:
            self.dsem[dn] = self.es.enter_context(self.nc.semaphore("dsem_" + dn))
            self.dcnt[dn] = 0
            self.semobj[("d", dn)] = self.dsem[dn]
        if fn is None:
            ins = self.eng[e].dma_start(out=out, in_=in_, **kw)
        else:
            ins = fn(self.eng[e])
        self.dcnt[dn] += 16
        ins.then_inc(self.dsem[dn], 16)
        self._mark(("d", dn), self.dcnt[dn], rds, [out])
        return ins

    def record(self, gen):
        self.rec = []
        for _ in gen:
            pass
        r, self.rec = self.rec, None
        return r

    def emit_merged(self, a, b):
        items = [((i + 0.5) / len(a), 0, i, x) for i, x in enumerate(a)] + [((j + 0.5) / len(b), 1, j, x) for j, x in enumerate(b)]
        items.sort(key=lambda t: (t[0], t[1], t[2]))
        for _, _, _, x in items:
            if x[0] == "op":
                self.op(x[1], x[2], x[3], x[4])
            else:
                self.dma(x[1], x[2], x[3], fn=x[4], extra_reads=x[5], **x[6])

    def barrier(self):
        ev = {}
        for e in self.eng:
            if self.ecnt[e]:
                ev[("e", e)] = self.ecnt[e]
        for dn, c in self.dcnt.items():
            ev[("d", dn)] = c
        for e in self.eng:
            for k, v in ev.items():
                if k == ("e", e):
                    continue
                if self.waited[e].get(k, 0) < v:
                    self.eng[e].wait_ge(self.semobj[k], v)
                    self.waited[e][k] = v

    def mm(self, out, lhsT, rhs, start=True, stop=True):
        return self.op("pe", lambda g: g.matmul(out, lhsT, rhs, start=start, stop=stop), [lhsT, rhs], [out])

    def tp(self, out, in_, ident):
        return self.op("pe", lambda g: g.transpose(out, in_, ident), [in_, ident], [out])

    def act(self, out, in_, func, bias=None, scale=None, accum_out=None, extra_reads=()):
        kw = {}
        rd = [in_] + list(extra_reads)
        if bias is not None:
            kw["bias"] = bias
            if not isinstance(bias, (int, float)):
                rd.append(bias)
        if scale is not None:
            kw["scale"] = scale
            if not isinstance(scale, (int, float)):
                rd.append(scale)
        wr = [out]
        if accum_out is not None:
            kw["accum_out"] = accum_out
            wr.append(accum_out)
        return self.op("act", lambda g: g.activation(out, in_, func, **kw), rd, wr)

    def tt(self, out, in0, in1, op, e="dve"):
        return self.op(e, lambda g: g.tensor_tensor(out, in0, in1, op), [in0, in1], [out])

    def ts(self, out, in0, s1, s2, op0, op1=None, e="dve", accum_out=None):
        rd = [in0] + [s for s in (s1, s2) if s is not None and not isinstance(s, (int, float))]
        wr = [out] + ([accum_out] if accum_out is not None else [])
        kw = {}
        if op1 is not None:
            kw["op1"] = op1
        if accum_out is not None:
            kw["accum_out"] = accum_out
        return self.op(e, lambda g: g.tensor_scalar(out, in0, s1, s2, op0, **kw), rd, wr)

    def stt(self, out, in0, scalar, in1, op0, op1, accum_out=None):
        rd = [in0, in1] + ([scalar] if not isinstance(scalar, (int, float)) else [])
        wr = [out] + ([accum_out] if accum_out is not None else [])
        kw = {"accum_out": accum_out} if accum_out is not None else {}
        return self.op("dve", lambda g: g.scalar_tensor_tensor(out, in0, scalar, in1, op0, op1, **kw), rd, wr)

    def cp(self, out, in_, e="dve"):
        if e == "act":
            return self.op("act", lambda g: g.copy(out, in_), [in_], [out])
        return self.op(e, lambda g: g.tensor_copy(out, in_), [in_], [out])

    def memset(self, ap, val, e="dve"):
        return self.op(e, lambda g: g.memset(ap, val), [], [ap])

    def red(self, out, in_, op, axis=AX.X):
        return self.op("dve", lambda g: g.tensor_reduce(out, in_, axis, op), [in_], [out])

    def recip(self, out, in_):
        return self.op("dve", lambda g: g.reciprocal(out, in_), [in_], [out])


def build_program(nseq=NSEQ_CORE, ntile=SEQ // 128, dbg=None, stop_after=None):
    nc = bass.Bass("TRN2", target_bir_lowering=False)
    es = ExitStack()
    ntok = nseq * ntile * 128
    with es:
        k = K(nc, es)
        def din(name, shape, dt=F32):
            return nc.dram_tensor(name, shape, dt, kind="ExternalInput").ap()

        x_d = din("x", [nseq * SEQ, D])
        c_d = din("c", [nseq, D])
        w_ada_d = din("w_ada", [D, 6 * D])
        b_ada_d = din("b_ada", [1, 6 * D])
        n1pre_d = din("norm1_pre", [1, D])
        n1post_d = din("norm1_post", [1, D])
        w_in_d = din("w_in", [D, IN_W])
        conv_a_d = din("conv_a_w", [3, D])
        conv_qk_d = din("conv_qk_w", [4, D])
        big_d = din("b_igate", [1, 8])
        bfg_d = din("b_fgate", [1, 8])
        mhw_d = din("mh_norm_w", [1, D])
        wsrc = {"w_in": w_in_d,
                "w_branch_a": din("w_branch_a", [D, D]),
                "w_branch_m": din("w_branch_m", [D, D]),
                "w_out": din("w_out", [D, D]),
                "peer_wq": din("peer_wq", [D, 2048])}
        n2pre_d = din("norm2_pre", [1, D])
        n2post_d = din("norm2_post", [1, D])
        sk_d = din("peer_subkeys", [16, 128, 128])
        pu_d = din("peer_u", [16384, D])
        pv_d = din("peer_v", [16384, D])
        cst_d = din("cst", [128, 1024])
        for nm in ("x", "c", "w_ada", "b_ada", "norm1_pre", "norm1_post", "w_in", "conv_a_w", "conv_qk_w", "b_igate",
                   "b_fgate", "mh_norm_w", "w_branch_a", "w_branch_m", "w_out", "peer_wq", "norm2_pre", "norm2_post",
                   "peer_subkeys", "peer_u", "peer_v", "cst"):
            k.tr[nm] = {"w": {}, "r": {}}
        out_d = k.dram("out", [nseq * SEQ, D], F32, "ExternalOutput")
        wsc_d = k.dram("wsc", [NBLK, 128, 8 * 512], BF16, "Internal")
        ada_d = k.dram("ada_sc", [nseq, 6 * D], F32, "Internal")
        ub_d = k.dram("ub_sc", [16384, D], BF16, "Internal")
        vb_d = k.dram("vb_sc", [16384, D], BF16, "Internal")
        dbg_d = None
        if dbg is not None:
            dbg_d = k.dram("dbg", [128, dbg], F32, "ExternalOutput")

        cst = k.sb("cst_sb", [128, 1024], F32)
        identf = cst[:, 0:128]
        tri2 = cst[:, 128:256]
        ones128 = cst[:, 256:384]
        rowmask = cst[:, 384:386]
        iota16 = cst[:, 576:592]
        onescol = cst[:, 592:593]
        identb_t = k.sb("identb", [128, 128], BF16)
        onesb_t = k.sb("onesb", [128, 1], BF16)
        wbuf = [k.sb("wbuf%d" % i, [128, 8, 512], BF16) for i in range(2)]
        rep = k.sb("rep", [128, 6, 1024], F32)
        xt = [k.sb("xt%d" % i, [128, D], F32) for i in range(2)]
        Fs = [k.sb("F%d" % i, [128, D], F32) for i in range(6)]
        Hs = [k.sb("H%d" % i, [128, D], BF16) for i in range(8)]
        UA = k.sb("UA", [128, 8, 130], F32)
        UQ = k.sb("UQ", [128, 8, 131], F32)
        cwa = k.sb("cwa", [128, 3, 8], F32)
        cwq = k.sb("cwq", [128, 4, 8], F32)
        skT = k.sb("skT", [128, 16, 128], BF16)
        Cf = k.sb("Cf", [128, 4, 128], F32)
        Cb = k.sb("Cb", [128, 4, 128], BF16)
        nf = k.sb("nf", [128, 4], F32)
        nb = k.sb("nb", [128, 4], BF16)
        sm = k.sb("sm", [128, 256], F32)
        sm2 = k.sb("sm2", [128, 256], F32)
        bif = k.sb("bif", [128, 16], F32)
        qT = k.sb("qT", [128, 16, 128], BF16)
        sc = k.sb("sc", [128, 16, 128], F32)
        scb = k.sb("scb", [128, 128], F32)
        cand = k.sb("cand", [128, 256], F32)
        candb = k.sb("candb", [128, 256], F32)
        v1 = k.sb("v1", [128, 8, 16], F32)
        v2 = k.sb("v2", [128, 8, 16], F32)
        i1 = k.sb("i1", [128, 8, 16], U32)
        i2 = k.sb("i2", [128, 8, 16], U32)
        cv = k.sb("cv", [128, 8, 16], F32)
        ci = k.sb("ci", [128, 8, 16], U32)
        tk0 = k.sb("tk0", [128, 8, 16, 16], F32)
        tkf = [k.sb("tkf%d" % i, [128, 8, 16], F32) for i in range(6)]
        tku = [k.sb("tku%d" % i, [128, 8, 16], U32) for i in range(2)]
        zz = k.sb("zz", [128, 128], F32)
        aa = k.sb("aa", [128, 128], F32)

        PB = [k.ps("PB%d" % i, [128, 512], F32) for i in range(7)]
        PT = k.ps("PT", [128, 1024], BF16)
        es2 = ExitStack()
        stage = [k.sb("stage%d" % i, [128, 8, 512], F32, es=es2) for i in range(2)]

        k.dma("sp", cst[:], cst_d[:, :])
        k.cp(identb_t[:], identf)
        k.cp(onesb_t[:], onescol)
        identb = identb_t[:]

        for j in range(3):
            k.dma("sp", cwa[:, j, :], conv_a_d[j:j + 1, :].rearrange("o (ch p) -> p (o ch)", p=128), allow_slow_non_contiguous=True)
        for j in range(4):
            k.dma("sp", cwq[:, j, :], conv_qk_d[j:j + 1, :].rearrange("o (ch p) -> p (o ch)", p=128), allow_slow_non_contiguous=True)
        k.dma("sp", bif[:, 0:8], big_d.partition_broadcast(128))
        k.dma("sp", bif[:, 8:16], bfg_d.partition_broadcast(128))

        cT = sm[:, 0:8 * nseq].rearrange("p (b kc) -> p b kc", b=nseq)
        for b_ in range(nseq):
            k.dma("sp", cT[:, b_, :], c_d[b_:b_ + 1, :].rearrange("o (kc p) -> p (o kc)", p=128), allow_slow_non_contiguous=True)
        scT = sm2[:, 0:8 * nseq].rearrange("p (b kc) -> p b kc", b=nseq)
        k.act(scT, cT, AF.Silu)
        adat = Fs[0]
        ada_sb = rep
        ada_flat = ada_sb[0:nseq].rearrange("p a d -> p (a d)")
        nvec = Fs[1]
        w_ada_v = w_ada_d.rearrange("(kc p) n -> p kc n", p=128)
        for g in range(12):
            st = stage[g % 2]
            k.dma("sp", st[:], w_ada_v[:, :, g * 512:(g + 1) * 512])
            for kc in range(8):
                k.mm(PB[g % 2][0:nseq, :], scT[:, :, kc], st[:, kc, :], start=(kc == 0), stop=(kc == 7))
            k.cp(ada_flat[:, g * 512:(g + 1) * 512], PB[g % 2][0:nseq, :])
        bad = Fs[2]
        for g in range(6):
            k.dma("sp", Fs[2 + (g % 2)][0:nseq, :], b_ada_d[:, g * D:(g + 1) * D].partition_broadcast(nseq))
            k.tt(ada_sb[0:nseq, g, :], ada_sb[0:nseq, g, :], Fs[2 + (g % 2)][0:nseq, :], ALU.add)
        comb = Fs[4]
        res6 = [Fs[4], Fs[5], Hs[0], Hs[1]]
        nv = Fs[1]
        k.dma("sp", nv[0:nseq, :], n1pre_d.partition_broadcast(nseq))
        k.stt(Fs[4][0:nseq, :], ada_sb[0:nseq, 1, :], 1.0, nv[0:nseq, :], ALU.add, ALU.mult)
        k.dma("sp", ada_d[:, 0 * D:1 * D], Fs[4][0:nseq, :])
        k.dma("sp", ada_d[:, 1 * D:2 * D], ada_sb[0:nseq, 0, :])
        k.dma("sp", Fs[2][0:nseq, :], n1post_d.partition_broadcast(nseq))
        k.tt(Fs[5][0:nseq, :], ada_sb[0:nseq, 2, :], Fs[2][0:nseq, :], ALU.mult)
        k.dma("sp", ada_d[:, 2 * D:3 * D], Fs[5][0:nseq, :])
        k.dma("sp", Fs[3][0:nseq, :], n2pre_d.partition_broadcast(nseq))
        k.stt(Fs[0][0:nseq, :], ada_sb[0:nseq, 4, :], 1.0, Fs[3][0:nseq, :], ALU.add, ALU.mult)
        k.dma("sp", ada_d[:, 3 * D:4 * D], Fs[0][0:nseq, :])
        k.dma("sp", ada_d[:, 4 * D:5 * D], ada_sb[0:nseq, 3, :])
        k.dma("sp", nv[0:nseq, :], n2post_d.partition_broadcast(nseq))
        k.tt(Fs[4][0:nseq, :], ada_sb[0:nseq, 5, :], nv[0:nseq, :], ALU.mult)
        k.dma("sp", ada_d[:, 5 * D:6 * D], Fs[4][0:nseq, :])

        mhw = sm[:, 16:24]
        k.dma("sp", mhw, mhw_d.rearrange("o (kc p) -> p (o kc)", p=128), allow_slow_non_contiguous=True)
        for bi, (nm, c0, ncol) in enumerate(WBLOCKS):
            st = stage[bi % 2]
            wb = wbuf[bi % 2]
            src = wsrc[nm].rearrange("(kc p) n -> p kc n", p=128)
            k.dma("sp", st[:, :, 0:ncol], src[:, :, c0:c0 + ncol])
            if nm == "w_branch_m":
                for kc in range(8):
                    k.ts(wb[:, kc, 0:ncol], st[:, kc, 0:ncol], mhw[:, kc:kc + 1], None, ALU.mult)
            else:
                k.cp(wb[:, 0:4, 0:ncol], st[:, 0:4, 0:ncol], e="dve")
                k.cp(wb[:, 4:8, 0:ncol], st[:, 4:8, 0:ncol], e="act")
            k.dma("pool", wsc_d[bi].rearrange("p (kc n) -> p kc n", n=512)[:, :, 0:ncol], wb[:, :, 0:ncol])
        for (src_d, dst_d) in ((pu_d, ub_d), (pv_d, vb_d)):
            sv_ = src_d.rearrange("(c p r) d -> c p (r d)", p=128, r=4)
            dv_ = dst_d.rearrange("(c p r) d -> c p (r d)", p=128, r=4)
            for c_ in range(32):
                stf = stage[c_ % 2][:].rearrange("p a n -> p (a n)")
                wbf = wbuf[c_ % 2][:].rearrange("p a n -> p (a n)")
                k.dma("sp", stf, sv_[c_])
                k.cp(wbf[:, 0:2048], stf[:, 0:2048], e="dve")
                k.cp(wbf[:, 2048:4096], stf[:, 2048:4096], e="act")
                k.dma("pool", dv_[c_], wbf)
        for hc in range(16):
            st = stage[hc % 2]
            k.dma("sp", st[:, 0, 0:128], sk_d[hc])
            k.tp(PB[hc % 2][:, 0:128], st[:, 0, 0:128], identf)
            k.cp(skT[:, hc, :], PB[hc % 2][:, 0:128])
        k.barrier()
        es2.close()
        gbufs = [k.sb("gbuf%d" % i, [128, D], BF16) for i in range(16)]
        X1p = [k.sb("X1p%d" % i, [128, D], F32) for i in range(2)]
        H2p = [k.sb("H2p%d" % i, [128, D], F32) for i in range(2)]
        eidxp = [k.sb("eidxp%d" % i, [128, 128], U32) for i in range(2)]
        gatep = [k.sb("gatep%d" % i, [128, 128], F32) for i in range(2)]
        JKb = k.sb("JKb", [128, D], BF16)
        TMPt = k.sb("TMPt", [128, D], F32)
        OUTt = k.sb("OUTt", [128, D], F32)
        Dg = [k.sb("Dg%d" % i, [128, 128], BF16) for i in range(4)]
        smb = k.sb("smb", [128, 8], F32)

        wstate = {"n": 0}

        def load_block(bi):
            wb = wbuf[wstate["n"] % 2]
            wstate["n"] += 1
            ncol = WBLOCKS[bi][2]
            k.dma("sp", wb[:, :, 0:ncol], wsc_d[bi].rearrange("p (kc n) -> p kc n", n=512)[:, :, 0:ncol])
            return wb

        def dump(ap, col0, ncols, parts=128):
            if dbg_d is not None:
                k.dma("sp", dbg_d[0:parts, col0:col0 + ncols], ap)

        for s in range(nseq):
            k.dma("sp", rep[:].rearrange("p a d -> p (a d)"), ada_d[s:s + 1, :].partition_broadcast(128))
            W1, SH1, G1N, W2, SH2, G2N = [rep[:, i, :] for i in range(6)]
            k.memset(UA[:, :, 0:2], 0.0)
            k.memset(UQ[:, :, 0:3], 0.0)
            k.memset(Cf[:], 0.0)
            k.memset(Cb[:], 0.0)
            k.memset(nf[:], 0.0)
            k.memset(nb[:], 0.0)
            def front(s, ti, par):
                r0 = s * SEQ + ti * 128
                eidx, gate = eidxp[par], gatep[par]
                X = xt[ti % 2]
                k.dma("sp", X[:], x_d[r0:r0 + 128, :])
                ssq = sm[:, 32:33]
                k.act(Fs[0][:], X[:], AF.Square, accum_out=ssq)
                rstd = sm[:, 33:34]
                k.ts(sm[:, 34:35], ssq, 1.0 / D, EPS, ALU.mult, ALU.add)
                k.act(sm[:, 35:36], sm[:, 34:35], AF.Sqrt)
                k.recip(rstd, sm[:, 35:36])
                k.stt(Fs[0][:], X[:], rstd, W1, ALU.mult, ALU.mult)
                hb = Hs[0]
                k.tt(hb[:], Fs[0][:], SH1, ALU.add)
                for kc in range(8):
                    k.tp(PT[:, kc * 128:(kc + 1) * 128], hb[:, kc * 128:(kc + 1) * 128], identb)
                hT = Hs[1]
                k.cp(hT[:], PT[:])
                hTv = hT[:].rearrange("p (kc t) -> p kc t", t=128)
                if stop_after == "hT":
                    k.cp(Fs[1][:], hT[:])
                    dump(Fs[1][:], 0, 1024)
                    return
                yield
                CG, BG, XIN = Fs[1], Fs[2], Fs[3]
                SGA, SGM = Hs[2], Hs[3]
                Vb = Hs[4]
                SO = Fs[4]
                IFt = sm[:, 40:56]

                def fm_block(bi, dst_fn, pbi):
                    wb = load_block(bi)
                    pb = PB[pbi]
                    for j in range(4):
                        for kc in range(8):
                            k.mm(pb[:, j * 128:(j + 1) * 128], wb[:, kc, j * 128:(j + 1) * 128], hTv[:, kc, :],
                                 start=(kc == 0), stop=(kc == 7))
                    dst_fn(pb)

                def evac_plain(dst3, e):
                    def f(pb):
                        k.cp(dst3, pb[:].rearrange("p (j t) -> p j t", t=128), e=e)
                    return f

                def evac_sig(dst3):
                    def f(pb):
                        k.act(dst3, pb[:].rearrange("p (j t) -> p j t", t=128), AF.Sigmoid)
                    return f

                def v3(t, lo):
                    return t[:].rearrange("p (j t) -> p j t", t=128)[:, lo:lo + 4, :]

                fm_block(0, evac_plain(v3(CG, 0), "act"), 0)
                fm_block(1, evac_plain(v3(CG, 4), "dve"), 1)
                yield
                fm_block(2, evac_plain(v3(BG, 0), "act"), 0)
                fm_block(3, evac_plain(v3(BG, 4), "dve"), 1)
                yield
                fm_block(4, evac_plain(v3(XIN, 0), "act"), 0)
                fm_block(5, evac_plain(v3(XIN, 4), "dve"), 1)
                yield
                fm_block(6, evac_plain(UQ[:, 0:4, 3:131], "act"), 0)
                fm_block(7, evac_plain(UQ[:, 4:8, 3:131], "dve"), 1)
                yield
                for gi, bi in enumerate((8, 9, 10, 11)):
                    wb = load_block(bi)
                    pb = PB[2 + gi % 2]
                    for kc in range(8):
                        k.mm(pb[:], hTv[:, kc, :], wb[:, kc, :], start=(kc == 0), stop=(kc == 7))
                    if gi < 2:
                        k.cp(Vb[:, gi * 512:(gi + 1) * 512], pb[:], e="dve")
                    else:
                        k.act(SO[:, (gi - 2) * 512:(gi - 1) * 512], pb[:], AF.Sigmoid)
                wb = load_block(12)
                for kc in range(8):
                    k.mm(PB[4][:, 0:16], hTv[:, kc, :], wb[:, kc, 0:16], start=(kc == 0), stop=(kc == 7))
                k.tt(IFt, PB[4][:, 0:16], bif[:], ALU.add)
                yield
                fm_block(13, evac_sig(v3(SGA, 0)), 0)
                fm_block(14, evac_sig(v3(SGA, 4)), 1)
                yield
                fm_block(15, evac_sig(v3(SGM, 0)), 0)
                fm_block(16, evac_sig(v3(SGM, 4)), 1)
                if stop_after == "proj":
                    dump(CG[:], 0, 1024)
                    dump(UQ[:, :, 3:131], 1024, 1024)
                    k.cp(Fs[0][:], Vb[:])
                    dump(Fs[0][:], 2048, 1024)
                    dump(SO[:], 3072, 1024)
                    dump(IFt, 4096, 16)
                    k.cp(Fs[5][:], SGM[:])
                    dump(Fs[5][:], 4112, 1024)
                    return
                yield
                CG3 = CG[:].rearrange("p (j t) -> p j t", t=128)
                BG3 = BG[:].rearrange("p (j t) -> p j t", t=128)
                XIN3 = XIN[:].rearrange("p (j t) -> p j t", t=128)
                k.tt(UA[:, :, 2:130], CG3, XIN3, ALU.mult)
                T0 = Fs[0][:].rearrange("p (j t) -> p j t", t=128)
                T1 = CG3
                k.tt(T0, UA[:, :, 0:128], cwa[:, 0, :].unsqueeze(2).to_broadcast([128, 8, 128]), ALU.mult)
                k.tt(T1, UA[:, :, 1:129], cwa[:, 1, :].unsqueeze(2).to_broadcast([128, 8, 128]), ALU.mult)
                k.tt(T0, T0, T1, ALU.add)
                k.tt(T1, UA[:, :, 2:130], cwa[:, 2, :].unsqueeze(2).to_broadcast([128, 8, 128]), ALU.mult)
                k.tt(T0, T0, T1, ALU.add)
                yaT = Hs[5]
                k.tt(yaT[:].rearrange("p (j t) -> p j t", t=128), T0, BG3, ALU.mult)
                k.cp(UA[:, :, 0:2], UA[:, :, 128:130])
                yield
                T1 = XIN3
                k.tt(T0, UQ[:, :, 0:128], cwq[:, 0, :].unsqueeze(2).to_broadcast([128, 8, 128]), ALU.mult)
                for j in range(1, 4):
                    k.tt(T1, UQ[:, :, j:j + 128], cwq[:, j, :].unsqueeze(2).to_broadcast([128, 8, 128]), ALU.mult)
                    k.tt(T0, T0, T1, ALU.add)
                qkT = Hs[6]
                qk3 = qkT[:].rearrange("p (j t) -> p j t", t=128)
                k.act(qk3, T0, AF.Silu)
                k.cp(UQ[:, :, 0:3], UQ[:, :, 128:131])
                yield
                IG = IFt[:, 0:8]
                FG = IFt[:, 8:16]
                lf = sm[:, 56:64]
                k.act(lf, FG, AF.Exp, scale=-1.0)
                k.act(lf, lf, AF.Ln, bias=1.0)
                k.ts(lf, lf, -1.0, None, ALU.mult)
                k.mm(PB[4][:, 16:24], tri2, lf)
                k.mm(PB[4][:, 24:32], ones128, lf)
                Bt = sm[:, 64:72]
                BL = sm[:, 72:80]
                k.cp(Bt, PB[4][:, 16:24])
                k.cp(BL, PB[4][:, 24:32])
                u = sm[:, 88:96]
                eB = sm[:, 96:104]
                wk = sm[:, 104:112]
                eBL = sm[:, 112:116]
                k.tt(u, IG, Bt, ALU.subtract)
                k.tt(wk, u, BL, ALU.add)
                k.act(u, u, AF.Exp)
                k.act(wk, wk, AF.Exp)
                k.act(eB, Bt, AF.Exp)
                BLv = BL.rearrange("p (j two) -> p j two", two=2)
                k.act(eBL[0:64, :], BLv[0:64, :, 0], AF.Exp)
                k.act(eBL[64:128, :], BLv[64:128, :, 1], AF.Exp)
                for j in range(4):
                    k.tp(PT[:, j * 128:(j + 1) * 128], qk3[:, 4 + j, :], identb)
                kw = Hs[7]
                kw3 = kw[:, 0:512].rearrange("p (h d) -> p h d", d=64)
                k.tt(kw3, PT[:, 0:512].rearrange("p (h d) -> p h d", d=64), wk.unsqueeze(2).to_broadcast([128, 8, 64]), ALU.mult)
                yield
                Qm = [Fs[0][:, 0:256].bitcast(BF16).rearrange("p (j t) -> p j t", t=128),
                      Fs[0][:, 256:512].bitcast(BF16).rearrange("p (j t) -> p j t", t=128)]
                for e_ in range(2):
                    k.ts(Qm[e_], qk3[:, 0:4, :], rowmask[:, e_:e_ + 1], None, ALU.mult)
                SB = [PB[2], PB[3]]
                for h in range(8):
                    k.mm(SB[h // 4][:, (h % 4) * 128:(h % 4 + 1) * 128], qk3[:, 4 + h // 2, :], Qm[h % 2][:, h // 2, :])
                SwT = Hs[1]
                Sw3 = SwT[:].rearrange("p (h t) -> p h t", t=128)
                S3 = Fs[5][:].rearrange("p (h t) -> p h t", t=128)
                for g2 in range(2):
                    k.tt(S3[:, g2 * 4:(g2 + 1) * 4, :], SB[g2][:].rearrange("p (h t) -> p h t", t=128),
                         u[:, g2 * 4:(g2 + 1) * 4].unsqueeze(2).to_broadcast([128, 4, 128]), ALU.mult)
                k.tt(Sw3, S3, tri2.unsqueeze(1).to_broadcast([128, 8, 128]), ALU.mult)
                yield
                Vb3 = Vb[:].rearrange("p (h d) -> p h d", d=128)
                NUM = [PB[0], PB[1]]
                DEN = PB[4][:, 40:48]
                for h in range(8):
                    q_h = Qm[h % 2][:, h // 2, :]
                    numo = NUM[h // 4][:, (h % 4) * 128:(h % 4 + 1) * 128]
                    k.mm(numo, Sw3[:, h, :], Vb3[:, h, :], start=True, stop=False)
                    k.mm(numo, q_h, Cb[:, h // 2, :], start=False, stop=True)
                    k.mm(DEN[:, h:h + 1], Sw3[:, h, :], onesb_t[:], start=True, stop=False)
                    k.mm(DEN[:, h:h + 1], q_h, nb[:, h // 2:h // 2 + 1], start=False, stop=True)
                yield
                kwp = kw[:, 0:512].rearrange("p (j d) -> p j d", d=128)
                for h in range(8):
                    k.mm(PB[2 + h % 2][:, (h // 2) * 128:(h // 2 + 1) * 128], kwp[:, h // 2, :], Vb3[:, h, :])
                for j in range(4):
                    k.mm(PB[4][:, 48 + j:49 + j], kwp[:, j, :], onesb_t[:])
                k.tt(Cf[:], Cf[:], eBL.unsqueeze(2).to_broadcast([128, 4, 128]), ALU.mult)
                k.tt(Cf[0:64], Cf[0:64], PB[2][0:64, :].rearrange("p (j d) -> p j d", d=128), ALU.add)
                k.tt(Cf[64:128], Cf[64:128], PB[3][64:128, :].rearrange("p (j d) -> p j d", d=128), ALU.add)
                k.cp(Cb[:], Cf[:], e="act")
                k.tt(nf[:], nf[:], eBL, ALU.mult)
                k.tt(nf[:], nf[:], PB[4][:, 48:52], ALU.add)
                k.cp(nb[:], nf[:], e="act")
                yield
                dn = sm[:, 120:128]
                k.stt(dn, DEN, 0.125, eB, ALU.mult, ALU.mult)
                k.act(dn, dn, AF.Abs)
                k.ts(dn, dn, 1.0, None, ALU.max)
                k.recip(dn, dn)
                k.stt(dn, eB, 0.125, dn, ALU.mult, ALU.mult)
                HN = Fs[5]
                HN3 = HN[:].rearrange("p (h d) -> p h d", d=128)
                for g2 in range(2):
                    k.tt(HN3[:, g2 * 4:(g2 + 1) * 4, :], NUM[g2][:].rearrange("p (h d) -> p h d", d=128),
                         dn[:, g2 * 4:(g2 + 1) * 4].unsqueeze(2).to_broadcast([128, 4, 128]), ALU.mult)
                if stop_after == "mlstm":
                    dump(HN[:], 0, 1024)
                    k.cp(Fs[0][:], yaT[:])
                    dump(Fs[0][:], 1024, 1024)
                    return
                yield
                k.tt(Fs[0][:], HN[:], HN[:], ALU.mult)
                hss = sm[:, 128:136]
                k.red(hss, Fs[0][:].rearrange("p (h d) -> p h d", d=128), ALU.add)
                k.ts(hss, hss, 1.0 / 128, EPS, ALU.mult, ALU.add)
                k.act(hss, hss, AF.Sqrt)
                k.recip(hss, hss)
                k.tt(HN3, HN3, hss.unsqueeze(2).to_broadcast([128, 8, 128]), ALU.mult)
                ymb = Hs[0]
                k.tt(ymb[:], HN[:], SO[:], ALU.mult)
                for kc in range(8):
                    k.tp(PT[:, kc * 128:(kc + 1) * 128], ymb[:, kc * 128:(kc + 1) * 128], identb)
                ymT = Hs[4]
                k.cp(ymT[:], PT[:])
                ymT3 = ymT[:].rearrange("p (kc t) -> p kc t", t=128)
                yaT3 = yaT[:].rearrange("p (kc t) -> p kc t", t=128)
                yield
                MIX = Fs[0]
                MIX3 = MIX[:].rearrange("p (j t) -> p j t", t=128)

                def branch(bname, src3, sg, first):
                    for half in range(2):
                        wb = load_block(BLK[(bname, half * 512)])
                        pb = PB[2 + half]
                        for j in range(4):
                            for kc in range(8):
                                k.mm(pb[:, j * 128:(j + 1) * 128], wb[:, kc, j * 128:(j + 1) * 128], src3[:, kc, :],
                                     start=(kc == 0), stop=(kc == 7))
                        sg3 = sg[:].rearrange("p (j t) -> p j t", t=128)[:, half * 4:(half + 1) * 4, :]
                        dst = MIX3[:, half * 4:(half + 1) * 4, :]
                        p3 = pb[:].rearrange("p (j t) -> p j t", t=128)
                        if first:
                            k.tt(dst, p3, sg3, ALU.mult)
                        else:
                            t3 = Fs[1][:].rearrange("p (j t) -> p j t", t=128)[:, half * 4:(half + 1) * 4, :]
                            k.tt(t3, p3, sg3, ALU.mult)
                            k.tt(dst, dst, t3, ALU.add)

                branch("w_branch_a", yaT3, SGA, True)
                yield
                branch("w_branch_m", ymT3, SGM, False)
                yield
                mixT = Hs[2]
                k.cp(mixT[:], MIX[:], e="act")
                mixT3 = mixT[:].rearrange("p (kc t) -> p kc t", t=128)
                for half in range(2):
                    wb = load_block(BLK[("w_out", half * 512)])
                    for kc in range(8):
                        k.mm(PB[half][:], mixT3[:, kc, :], wb[:, kc, :], start=(kc == 0), stop=(kc == 7))
                yield
                Y = Fs[1]
                k.cp(Y[:, 0:512], PB[0][:], e="act")
                k.cp(Y[:, 512:1024], PB[1][:], e="dve")
                ssq2 = sm[:, 136:137]
                k.act(Fs[0][:], Y[:], AF.Square, accum_out=ssq2)
                k.ts(ssq2, ssq2, 1.0 / D, EPS, ALU.mult, ALU.add)
                k.act(ssq2, ssq2, AF.Sqrt)
                k.recip(ssq2, ssq2)
                k.stt(Fs[0][:], Y[:], ssq2, G1N, ALU.mult, ALU.mult)
                X1 = X1p[par]
                k.tt(X1[:], Fs[0][:], X[:], ALU.add)
                if stop_after == "sub1":
                    k.dma("sp", out_d[r0:r0 + 128, :], X1[:])
                    return
                yield
                ssq3 = sm[:, 137:138]
                k.act(Fs[0][:], X1[:], AF.Square, accum_out=ssq3)
                k.ts(ssq3, ssq3, 1.0 / D, EPS, ALU.mult, ALU.add)
                k.act(ssq3, ssq3, AF.Sqrt)
                k.recip(ssq3, ssq3)
                k.stt(Fs[0][:], X1[:], ssq3, W2, ALU.mult, ALU.mult)
                H2 = H2p[par]
                k.tt(H2[:], Fs[0][:], SH2, ALU.add)
                h2b = Hs[0]
                k.cp(h2b[:], H2[:], e="act")
                for kc in range(8):
                    k.tp(PT[:, kc * 128:(kc + 1) * 128], h2b[:, kc * 128:(kc + 1) * 128], identb)
                h2T = Hs[1]
                k.cp(h2T[:], PT[:])
                h2T3 = h2T[:].rearrange("p (kc t) -> p kc t", t=128)
                yield
                for g4 in range(4):
                    wb = load_block(BLK[("peer_wq", g4 * 512)])
                    pb = PB[2 + g4 % 2]
                    for j in range(4):
                        for kc in range(8):
                            k.mm(pb[:, j * 128:(j + 1) * 128], wb[:, kc, j * 128:(j + 1) * 128], h2T3[:, kc, :],
                                 start=(kc == 0), stop=(kc == 7))
                    k.cp(qT[:, g4 * 4:(g4 + 1) * 4, :], pb[:].rearrange("p (j t) -> p j t", t=128), e=("act" if g4 % 2 else "dve"))
                yield
                for g4 in range(4):
                    pb = PB[g4 % 2]
                    for j in range(4):
                        hc = g4 * 4 + j
                        k.mm(pb[:, j * 128:(j + 1) * 128], qT[:, hc, :], skT[:, hc, :])
                    k.cp(sc[:, g4 * 4:(g4 + 1) * 4, :], pb[:].rearrange("p (j n) -> p j n", n=128), e=("act" if g4 % 2 else "dve"))
                yield
                dv = k.eng["dve"]
                for h in range(8):
                    for half, (vv, ii) in enumerate(((v1, i1), (v2, i2))):
                        s_ = sc[:, 2 * h + half, :]
                        k.op("dve", lambda g, vv=vv, h=h, s_=s_: g.max(vv[:, h, 0:8], s_), [s_], [vv[:]])
                        k.op("dve", lambda g, vv=vv, ii=ii, h=h, s_=s_: g.max_index(ii[:, h, 0:8], vv[:, h, 0:8], s_), [s_, vv[:]], [ii[:]])
                        k.op("dve", lambda g, vv=vv, h=h, s_=s_: g.match_replace(scb[:], vv[:, h, 0:8], s_, NEG), [s_, vv[:]], [scb[:]])
                        k.op("dve", lambda g, vv=vv, h=h: g.max(vv[:, h, 8:16], scb[:]), [scb[:]], [vv[:]])
                        k.op("dve", lambda g, vv=vv, ii=ii, h=h: g.max_index(ii[:, h, 8:16], vv[:, h, 8:16], scb[:]), [scb[:], vv[:]], [ii[:]])
                    yield
                    c3 = cand[:].rearrange("p (a b) -> p a b", b=16)
                    k.tt(c3, v1[:, h, :].unsqueeze(2).to_broadcast([128, 16, 16]),
                         v2[:, h, :].unsqueeze(1).to_broadcast([128, 16, 16]), ALU.add)
                    k.op("dve", lambda g, h=h: g.max(cv[:, h, 0:8], cand[:]), [cand[:]], [cv[:]])
                    k.op("dve", lambda g, h=h: g.max_index(ci[:, h, 0:8], cv[:, h, 0:8], cand[:]), [cand[:], cv[:]], [ci[:]])
                    k.op("dve", lambda g, h=h: g.match_replace(candb[:], cv[:, h, 0:8], cand[:], NEG), [cand[:], cv[:]], [candb[:]])
                    k.op("dve", lambda g, h=h: g.max(cv[:, h, 8:16], candb[:]), [candb[:]], [cv[:]])
                    k.op("dve", lambda g, h=h: g.max_index(ci[:, h, 8:16], cv[:, h, 8:16], candb[:]), [candb[:], cv[:]], [ci[:]])
                yield
                ua, ub_ = tku[0], tku[1]
                k.ts(ua[:], ci[:], 4, None, ALU.logical_shift_right)
                k.ts(ub_[:], ci[:], 15, None, ALU.bitwise_and)
                af, bf_, i1f, i2f, isel, jsel = tkf
                k.cp(af[:], ua[:])
                k.cp(bf_[:], ub_[:])
                k.cp(i1f[:], i1[:])
                k.cp(i2f[:], i2[:])
                io4 = iota16.unsqueeze(1).unsqueeze(1).to_broadcast([128, 8, 16, 16])
                for (xf, tab, dst) in ((af, i1f, isel), (bf_, i2f, jsel)):
                    k.tt(tk0[:], xf[:].unsqueeze(3).to_broadcast([128, 8, 16, 16]), io4, ALU.is_equal)
                    k.tt(tk0[:], tk0[:], tab[:].unsqueeze(2).to_broadcast([128, 8, 16, 16]), ALU.mult)
                    k.red(dst[:], tk0[:], ALU.add)
                ef = af
                k.stt(ef[:], isel[:], 128.0, jsel[:], ALU.mult, ALU.add)
                k.cp(eidx[:].rearrange("p (h k) -> p h k", k=16), ef[:])
                yield
                gx = bf_
                k.tt(gx[:], cv[:], cv[:, :, 0:1].to_broadcast([128, 8, 16]), ALU.subtract)
                k.act(gx[:], gx[:], AF.Exp)
                gs = sm[:, 144:152]
                k.red(gs, gx[:], ALU.add)
                k.recip(gs, gs)
                k.tt(gate[:].rearrange("p (h k) -> p h k", k=16), gx[:], gs.unsqueeze(2).to_broadcast([128, 8, 16]), ALU.mult)
                if stop_after == "topk":
                    k.cp(Fs[0][:, 0:128], eidx[:])
                    dump(Fs[0][:, 0:128], 0, 128)
                    dump(gate[:], 128, 128)
                    dump(H2[:], 256, 1024)
                    return
                yield
            def back(s, ti, par):
                r0 = s * SEQ + ti * 128
                X1, H2, eidx, gate = X1p[par], H2p[par], eidxp[par], gatep[par]
                NG = len(gbufs)
                for sl in range(128):
                    gb = gbufs[sl % NG][:]
                    k.dma("pool", gb, ub_d, fn=lambda g, gb=gb, sl=sl, eidx=eidx: g.indirect_dma_start(
                        out=gb, out_offset=None, in_=ub_d[:, :],
                        in_offset=bass.IndirectOffsetOnAxis(ap=eidx[:, sl:sl + 1], axis=0)), extra_reads=[eidx[:]])
                    k.stt(JKb[:], gb, 1.0, H2[:], ALU.mult, ALU.mult, accum_out=zz[:, sl:sl + 1])
                    if sl % 4 == 3:
                        yield
                k.tt(aa[:], zz[:], zz[:], ALU.mult)
                k.ts(aa[:], aa[:], 0.044715, 1.0, ALU.mult, ALU.add)
                k.tt(aa[:], aa[:], zz[:], ALU.mult)
                k.act(aa[:], aa[:], AF.Sigmoid, scale=1.5957691216057308)
                k.tt(aa[:], aa[:], zz[:], ALU.mult)
                k.tt(aa[:], aa[:], gate[:], ALU.mult)
                yield
                for sl in range(128):
                    gb = gbufs[sl % NG][:]
                    k.dma("pool", gb, vb_d, fn=lambda g, gb=gb, sl=sl, eidx=eidx: g.indirect_dma_start(
                        out=gb, out_offset=None, in_=vb_d[:, :],
                        in_offset=bass.IndirectOffsetOnAxis(ap=eidx[:, sl:sl + 1], axis=0)), extra_reads=[eidx[:]])
                    dg = Dg[sl % 4]
                    k.act(dg[:], identb, AF.Copy, scale=aa[:, sl:sl + 1])
                    k.mm(PB[5][:], dg[:], gb[:, 0:512], start=(sl == 0), stop=(sl == 127))
                    k.mm(PB[6][:], dg[:], gb[:, 512:1024], start=(sl == 0), stop=(sl == 127))
                    if sl % 4 == 3:
                        yield
                ssq4 = smb[:, 0:1]
                k.act(TMPt[:, 0:512], PB[5][:], AF.Square, accum_out=ssq4)
                k.act(TMPt[:, 512:1024], PB[6][:], AF.Square, accum_out=smb[:, 1:2])
                k.tt(ssq4, ssq4, smb[:, 1:2], ALU.add)
                k.ts(ssq4, ssq4, 1.0 / D, EPS, ALU.mult, ALU.add)
                k.act(ssq4, ssq4, AF.Sqrt)
                k.recip(ssq4, ssq4)
                k.stt(TMPt[:, 0:512], PB[5][:], ssq4, rep[:, 5, 0:512], ALU.mult, ALU.mult)
                k.stt(TMPt[:, 512:1024], PB[6][:], ssq4, rep[:, 5, 512:1024], ALU.mult, ALU.mult)
                k.tt(OUTt[:], TMPt[:], X1[:], ALU.add)
                k.dma("sp", out_d[r0:r0 + 128, :], OUTt[:])
                yield

            prev = None
            for ti in range(ntile):
                fa = k.record(front(s, ti, ti % 2))
                fb = k.record(back(*prev)) if (prev is not None and stop_after is None) else []
                if fb:
                    k.emit_merged(fa, fb)
                else:
                    k.emit_merged(fa, [])
                prev = (s, ti, ti % 2)
            if stop_after is None:
                k.emit_merged(k.record(back(*prev)), [])
        k.barrier()
    return nc


def make_consts():
    c = np.zeros((128, 1024), np.float32)
    c[:, 0:128] = np.eye(128, dtype=np.float32)
    s = np.arange(128)[:, None]
    t = np.arange(128)[None, :]
    c[:, 128:256] = (s <= t).astype(np.float32)
    c[:, 256:384] = 1.0
    c[:, 384] = (np.arange(128) < 64).astype(np.float32)
    c[:, 385] = (np.arange(128) >= 64).astype(np.float32)
    c[:, 576:592] = np.arange(16, dtype=np.float32)[None, :]
    c[:, 592] = 1.0
    return c


_PROG = {}


def kernel(x, c, w_ada, b_ada, norm1_pre, norm1_post, w_in, conv_a_w, conv_qk_w, b_igate, b_fgate, mh_norm_w,
           w_branch_a, w_branch_m, w_out, norm2_pre, norm2_post, peer_wq, peer_subkeys, peer_u, peer_v):
    f = lambda a: np.ascontiguousarray(np.asarray(a, dtype=np.float32))
    if "nc" not in _PROG:
        _PROG["nc"] = build_program()
    nc = _PROG["nc"]
    shared = {
        "w_ada": f(w_ada[0]), "b_ada": f(b_ada), "norm1_pre": f(norm1_pre), "norm1_post": f(norm1_post),
        "w_in": f(w_in[0]), "conv_a_w": f(conv_a_w[0]), "conv_qk_w": f(conv_qk_w[0]), "b_igate": f(b_igate),
        "b_fgate": f(b_fgate), "mh_norm_w": f(mh_norm_w), "w_branch_a": f(w_branch_a[0]),
        "w_branch_m": f(w_branch_m[0]), "w_out": f(w_out[0]), "peer_wq": f(peer_wq[0]),
        "norm2_pre": f(norm2_pre), "norm2_post": f(norm2_post),
        "peer_subkeys": f(peer_subkeys[0]).reshape(16, 128, 128), "peer_u": f(peer_u[0]), "peer_v": f(peer_v[0]),
        "cst": make_consts(),
    }
    xs = f(x)
    cs = f(c)
    in_maps = []
    for i in range(NCORES):
        m = dict(shared)
        m["x"] = xs[2 * i:2 * i + 2].reshape(2 * SEQ, D)
        m["c"] = cs[2 * i:2 * i + 2]
        in_maps.append(m)
    res = run_bass_kernel_spmd(nc, in_maps, core_ids=list(range(NCORES)))
    out = np.concatenate([r["out"].reshape(2, SEQ, D) for r in res.results], axis=0)
    return out.astype(np.float32)
```

```python
from contextlib import ExitStack
import numpy as np
import concourse.bass as bass
import concourse.mybir as mybir
from concourse.bass_utils import run_bass_kernel_spmd

F32 = mybir.dt.float32
BF16 = mybir.dt.bfloat16
U32 = mybir.dt.uint32
I32 = mybir.dt.int32
AF = mybir.ActivationFunctionType
ALU = mybir.AluOpType
AX = mybir.AxisListType

D = 1024
NCORES = 8
SEQ = 4096
NSEQ_CORE = 2
IN_W = 8208
EPS = 1e-6
NEG = -1.0e30

O_CG, O_BG, O_XIN, O_Q, O_K, O_V, O_O, O_I, O_F, O_GA, O_GM = 0, 1024, 2048, 3072, 3584, 4096, 5120, 6144, 6152, 6160, 7184

WBLOCKS = []
for c0 in range(0, 6144, 512):
    WBLOCKS.append(("w_in", c0, 512))
WBLOCKS.append(("w_in", 6144, 16))
for c0 in range(6160, 8208, 512):
    WBLOCKS.append(("w_in", c0, 512))
for nm in ("w_branch_a", "w_branch_m", "w_out"):
    for c0 in (0, 512):
        WBLOCKS.append((nm, c0, 512))
for c0 in range(0, 2048, 512):
    WBLOCKS.append(("peer_wq", c0, 512))
NBLK = len(WBLOCKS)
NGBUF = 16
RESIDENT = tuple(range(0, 4))
BLK = {(nm, c0): i for i, (nm, c0, n) in enumerate(WBLOCKS)}


class K:
    def __init__(self, nc, es):
        self.nc = nc
        self.es = es
        self.eng = {"pe": nc.tensor, "act": nc.scalar, "dve": nc.vector, "pool": nc.gpsimd, "sp": nc.sync}
        self.esem = {e: es.enter_context(nc.semaphore("sem_" + e)) for e in self.eng}
        self.ecnt = {e: 0 for e in self.eng}
        self.semobj = {("e", e): self.esem[e] for e in self.eng}
        self.waited = {e: {} for e in self.eng}
        self.tr = {}
        self.dsem = {}
        self.dcnt = {}
        self.same_engine_sync = {"pe": False, "act": True, "dve": True, "pool": True, "sp": True}
        self.rec = None

    def _reg(self, h, name):
        self.tr[name] = {"w": {}, "r": {}}
        return h

    def sb(self, name, shape, dt, es=None):
        return self._reg((es or self.es).enter_context(self.nc.sbuf_tensor(name, shape, dt)), name)

    def ps(self, name, shape, dt):
        return self._reg(self.es.enter_context(self.nc.psum_tensor(name, shape, dt)), name)

    def dram(self, name, shape, dt, kind):
        t = self.nc.dram_tensor(name, shape, dt, kind=kind)
        self.tr[name] = {"w": {}, "r": {}}
        return t.ap()

    def _rec(self, ap):
        return self.tr[ap.tensor.name if hasattr(ap, "tensor") else ap.name]

    def _name(self, ap):
        try:
            return ap.tensor.name
        except Exception:
            return ap.name

    def _deps(self, reads, writes):
        ev = {}
        for ap in reads:
            rec = self.tr[self._name(ap)]
            for k, v in rec["w"].items():
                ev[k] = max(ev.get(k, 0), v)
        for ap in writes:
            rec = self.tr[self._name(ap)]
            for k, v in rec["w"].items():
                ev[k] = max(ev.get(k, 0), v)
            for k, v in rec["r"].items():
                ev[k] = max(ev.get(k, 0), v)
        return ev

    def _wait(self, e, ev):
        for k, v in ev.items():
            if k == ("e", e) and not self.same_engine_sync[e]:
                continue
            if self.waited[e].get(k, 0) < v:
                self.eng[e].wait_ge(self.semobj[k], v)
                self.waited[e][k] = v

    def _mark(self, key, val, reads, writes):
        for ap in reads:
            rec = self.tr[self._name(ap)]
            rec["r"][key] = max(rec["r"].get(key, 0), val)
        for ap in writes:
            rec = self.tr[self._name(ap)]
            rec["w"][key] = max(rec["w"].get(key, 0), val)

    def op(self, e, fn, reads, writes):
        if self.rec is not None:
            self.rec.append(("op", e, fn, list(reads), list(writes)))
            return None
        self._wait(e, self._deps(reads, writes))
        ins = fn(self.eng[e])
        self.ecnt[e] += 1
        ins.then_inc(self.esem[e], 1)
        self._mark(("e", e), self.ecnt[e], reads, writes)
        return ins

    def dma(self, e, out, in_, fn=None, extra_reads=(), **kw):
        if self.rec is not None:
            self.rec.append(("dma", e, out, in_, fn, list(extra_reads), kw))
            return None
        rds = [in_] + list(extra_reads)
        self._wait(e, self._deps(rds, [out]))
        dn = self._name(out)
        if dn not in self.dsem:
            self.dsem[dn] = self.es.enter_context(self.nc.semaphore("dsem_" + dn))
            self.dcnt[dn] = 0
            self.semobj[("d", dn)] = self.dsem[dn]
        if fn is None:
            ins = self.eng[e].dma_start(out=out, in_=in_, **kw)
        else:
            ins = fn(self.eng[e])
        self.dcnt[dn] += 16
        ins.then_inc(self.dsem[dn], 16)
        self._mark(("d", dn), self.dcnt[dn], rds, [out])
        return ins

    def record(self, gen):
        self.rec = []
        for _ in gen:
            pass
        r, self.rec = self.rec, None
        return r

    def emit_merged(self, a, b):
        items = [((i + 0.5) / len(a), 0, i, x) for i, x in enumerate(a)] + [((j + 0.5) / len(b), 1, j, x) for j, x in enumerate(b)]
        items.sort(key=lambda t: (t[0], t[1], t[2]))
        for _, _, _, x in items:
            if x[0] == "op":
                self.op(x[1], x[2], x[3], x[4])
            else:
                self.dma(x[1], x[2], x[3], fn=x[4], extra_reads=x[5], **x[6])

    def barrier(self):
        ev = {}
        for e in self.eng:
            if self.ecnt[e]:
                ev[("e", e)] = self.ecnt[e]
        for dn, c in self.dcnt.items():
            ev[("d", dn)] = c
        for e in self.eng:
            for k, v in ev.items():
                if k == ("e", e):
                    continue
                if self.waited[e].get(k, 0) < v:
                    self.eng[e].wait_ge(self.semobj[k], v)
                    self.waited[e][k] = v

    def mm(self, out, lhsT, rhs, start=True, stop=True):
        return self.op("pe", lambda g: g.matmul(out, lhsT, rhs, start=start, stop=stop), [lhsT, rhs], [out])

    def tp(self, out, in_, ident):
        return self.op("pe", lambda g: g.transpose(out, in_, ident), [in_, ident], [out])

    def act(self, out, in_, func, bias=None, scale=None, accum_out=None, extra_reads=()):
        kw = {}
        rd = [in_] + list(extra_reads)
        if bias is not None:
            kw["bias"] = bias
            if not isinstance(bias, (int, float)):
                rd.append(bias)
        if scale is not None:
            kw["scale"] = scale
            if not isinstance(scale, (int, float)):
                rd.append(scale)
        wr = [out]
        if accum_out is not None:
            kw["accum_out"] = accum_out
            wr.append(accum_out)
        return self.op("act", lambda g: g.activation(out, in_, func, **kw), rd, wr)

    def tt(self, out, in0, in1, op, e="dve"):
        return self.op(e, lambda g: g.tensor_tensor(out, in0, in1, op), [in0, in1], [out])

    def ts(self, out, in0, s1, s2, op0, op1=None, e="dve", accum_out=None):
        rd = [in0] + [s for s in (s1, s2) if s is not None and not isinstance(s, (int, float))]
        wr = [out] + ([accum_out] if accum_out is not None else [])
        kw = {}
        if op1 is not None:
            kw["op1"] = op1
        if accum_out is not None:
            kw["accum_out"] = accum_out
        return self.op(e, lambda g: g.tensor_scalar(out, in0, s1, s2, op0, **kw), rd, wr)

    def stt(self, out, in0, scalar, in1, op0, op1, accum_out=None):
        rd = [in0, in1] + ([scalar] if not isinstance(scalar, (int, float)) else [])
        wr = [out] + ([accum_out] if accum_out is not None else [])
        kw = {"accum_out": accum_out} if accum_out is not None else {}
        return self.op("dve", lambda g: g.scalar_tensor_tensor(out, in0, scalar, in1, op0, op1, **kw), rd, wr)

    def cp(self, out, in_, e="dve"):
        if e == "act":
            return self.op("act", lambda g: g.copy(out, in_), [in_], [out])
        return self.op(e, lambda g: g.tensor_copy(out, in_), [in_], [out])

    def memset(self, ap, val, e="dve"):
        return self.op(e, lambda g: g.memset(ap, val), [], [ap])

    def red(self, out, in_, op, axis=AX.X):
        return self.op("dve", lambda g: g.tensor_reduce(out, in_, axis, op), [in_], [out])

    def recip(self, out, in_):
        return self.op("dve", lambda g: g.reciprocal(out, in_), [in_], [out])


def build_program(nseq=NSEQ_CORE, ntile=SEQ // 128, dbg=None, stop_after=None):
    nc = bass.Bass("TRN2", target_bir_lowering=False)
    es = ExitStack()
    ntok = nseq * ntile * 128
    with es:
        k = K(nc, es)
        def din(name, shape, dt=F32):
            return nc.dram_tensor(name, shape, dt, kind="ExternalInput").ap()

        x_d = din("x", [nseq * SEQ, D])
        c_d = din("c", [nseq, D])
        w_ada_d = din("w_ada", [D, 6 * D])
        b_ada_d = din("b_ada", [1, 6 * D])
        n1pre_d = din("norm1_pre", [1, D])
        n1post_d = din("norm1_post", [1, D])
        w_in_d = din("w_in", [D, IN_W])
        conv_a_d = din("conv_a_w", [3, D])
        conv_qk_d = din("conv_qk_w", [4, D])
        big_d = din("b_igate", [1, 8])
        bfg_d = din("b_fgate", [1, 8])
        mhw_d = din("mh_norm_w", [1, D])
        wsrc = {"w_in": w_in_d,
                "w_branch_a": din("w_branch_a", [D, D]),
                "w_branch_m": din("w_branch_m", [D, D]),
                "w_out": din("w_out", [D, D]),
                "peer_wq": din("peer_wq", [D, 2048])}
        n2pre_d = din("norm2_pre", [1, D])
        n2post_d = din("norm2_post", [1, D])
        sk_d = din("peer_subkeys", [16, 128, 128])
        pu_d = din("peer_u", [16384, D])
        pv_d = din("peer_v", [16384, D])
        cst_d = din("cst", [128, 1024])
        for nm in ("x", "c", "w_ada", "b_ada", "norm1_pre", "norm1_post", "w_in", "conv_a_w", "conv_qk_w", "b_igate",
                   "b_fgate", "mh_norm_w", "w_branch_a", "w_branch_m", "w_out", "peer_wq", "norm2_pre", "norm2_post",
                   "peer_subkeys", "peer_u", "peer_v", "cst"):
            k.tr[nm] = {"w": {}, "r": {}}
        out_d = k.dram("out", [nseq * SEQ, D], F32, "ExternalOutput")
        wsc_d = k.dram("wsc", [NBLK, 128, 8 * 512], BF16, "Internal")
        ada_d = k.dram("ada_sc", [nseq, 6 * D], F32, "Internal")
        ub_d = k.dram("ub_sc", [16384, D], BF16, "Internal")
        vb_d = k.dram("vb_sc", [16384, D], BF16, "Internal")
        dbg_d = None
        if dbg is not None:
            dbg_d = k.dram("dbg", [128, dbg], F32, "ExternalOutput")

        cst = k.sb("cst_sb", [128, 1024], F32)
        identf = cst[:, 0:128]
        tri2 = cst[:, 128:256]
        ones128 = cst[:, 256:384]
        rowmask = cst[:, 384:386]
        iota16 = cst[:, 576:592]
        onescol = cst[:, 592:593]
        identb_t = k.sb("identb", [128, 128], BF16)
        onesb_t = k.sb("onesb", [128, 1], BF16)
        wbuf = [k.sb("wbuf%d" % i, [128, 8, 512], BF16) for i in range(2)]
        rep = k.sb("rep", [128, 6, 1024], F32)
        Fs = [k.sb("F%d" % i, [128, D], F32) for i in range(6)]
        Hs = [k.sb("H%d" % i, [128, D], BF16) for i in range(8)]
        UA = k.sb("UA", [128, 8, 130], F32)
        UQ = k.sb("UQ", [128, 8, 131], F32)
        cwa = k.sb("cwa", [128, 3, 8], F32)
        cwq = k.sb("cwq", [128, 4, 8], F32)
        skT = k.sb("skT", [128, 16, 128], BF16)
        Cf = k.sb("Cf", [128, 4, 128], F32)
        Cb = k.sb("Cb", [128, 4, 128], BF16)
        nf = k.sb("nf", [128, 4], F32)
        nb = k.sb("nb", [128, 4], BF16)
        sm = k.sb("sm", [128, 256], F32)
        sm2 = k.sb("sm2", [128, 256], F32)
        bif = k.sb("bif", [128, 16], F32)
        qT = k.sb("qT", [128, 16, 128], BF16)
        scb = k.sb("scb", [128, 128], F32)
        cand = k.sb("cand", [128, 256], F32)
        candb = k.sb("candb", [128, 256], F32)
        v1 = k.sb("v1", [128, 8, 16], F32)
        v2 = k.sb("v2", [128, 8, 16], F32)
        i1 = k.sb("i1", [128, 8, 16], U32)
        i2 = k.sb("i2", [128, 8, 16], U32)
        cv = k.sb("cv", [128, 8, 16], F32)
        ci = k.sb("ci", [128, 8, 16], U32)
        tkf = [k.sb("tkf%d" % i, [128, 8, 16], F32) for i in range(6)]
        tku = [k.sb("tku%d" % i, [128, 8, 16], U32) for i in range(2)]
        zz = k.sb("zz", [128, 128], F32)
        aa = k.sb("aa", [128, 128], F32)

        PB = [k.ps("PB%d" % i, [128, 512], F32) for i in range(7)]
        PT = k.ps("PT", [128, 1024], BF16)
        es2 = ExitStack()
        stage = [k.sb("stage%d" % i, [128, 8, 512], F32, es=es2) for i in range(2)]

        k.dma("sp", cst[:], cst_d[:, :])
        k.cp(identb_t[:], identf)
        k.cp(onesb_t[:], onescol)
        identb = identb_t[:]

        for j in range(3):
            k.dma("sp", cwa[:, j, :], conv_a_d[j:j + 1, :].rearrange("o (ch p) -> p (o ch)", p=128), allow_slow_non_contiguous=True)
        for j in range(4):
            k.dma("sp", cwq[:, j, :], conv_qk_d[j:j + 1, :].rearrange("o (ch p) -> p (o ch)", p=128), allow_slow_non_contiguous=True)
        k.dma("sp", bif[:, 0:8], big_d.partition_broadcast(128))
        k.dma("sp", bif[:, 8:16], bfg_d.partition_broadcast(128))

        cT = sm[:, 0:8 * nseq].rearrange("p (b kc) -> p b kc", b=nseq)
        for b_ in range(nseq):
            k.dma("sp", cT[:, b_, :], c_d[b_:b_ + 1, :].rearrange("o (kc p) -> p (o kc)", p=128), allow_slow_non_contiguous=True)
        scT = sm2[:, 0:8 * nseq].rearrange("p (b kc) -> p b kc", b=nseq)
        k.act(scT, cT, AF.Silu)
        adat = Fs[0]
        ada_sb = rep
        ada_flat = ada_sb[0:nseq].rearrange("p a d -> p (a d)")
        nvec = Fs[1]
        w_ada_v = w_ada_d.rearrange("(kc p) n -> p kc n", p=128)
        for g in range(12):
            st = stage[g % 2]
            k.dma("sp", st[:], w_ada_v[:, :, g * 512:(g + 1) * 512])
            for kc in range(8):
                k.mm(PB[g % 2][0:nseq, :], scT[:, :, kc], st[:, kc, :], start=(kc == 0), stop=(kc == 7))
            k.cp(ada_flat[:, g * 512:(g + 1) * 512], PB[g % 2][0:nseq, :])
        bad = Fs[2]
        for g in range(6):
            k.dma("sp", Fs[2 + (g % 2)][0:nseq, :], b_ada_d[:, g * D:(g + 1) * D].partition_broadcast(nseq))
            k.tt(ada_sb[0:nseq, g, :], ada_sb[0:nseq, g, :], Fs[2 + (g % 2)][0:nseq, :], ALU.add)
        comb = Fs[4]
        res6 = [Fs[4], Fs[5], Hs[0], Hs[1]]
        nv = Fs[1]
        k.dma("sp", nv[0:nseq, :], n1pre_d.partition_broadcast(nseq))
        k.stt(Fs[4][0:nseq, :], ada_sb[0:nseq, 1, :], 1.0, nv[0:nseq, :], ALU.add, ALU.mult)
        k.dma("sp", ada_d[:, 0 * D:1 * D], Fs[4][0:nseq, :])
        k.dma("sp", ada_d[:, 1 * D:2 * D], ada_sb[0:nseq, 0, :])
        k.dma("sp", Fs[2][0:nseq, :], n1post_d.partition_broadcast(nseq))
        k.tt(Fs[5][0:nseq, :], ada_sb[0:nseq, 2, :], Fs[2][0:nseq, :], ALU.mult)
        k.dma("sp", ada_d[:, 2 * D:3 * D], Fs[5][0:nseq, :])
        k.dma("sp", Fs[3][0:nseq, :], n2pre_d.partition_broadcast(nseq))
        k.stt(Fs[0][0:nseq, :], ada_sb[0:nseq, 4, :], 1.0, Fs[3][0:nseq, :], ALU.add, ALU.mult)
        k.dma("sp", ada_d[:, 3 * D:4 * D], Fs[0][0:nseq, :])
        k.dma("sp", ada_d[:, 4 * D:5 * D], ada_sb[0:nseq, 3, :])
        k.dma("sp", nv[0:nseq, :], n2post_d.partition_broadcast(nseq))
        k.tt(Fs[4][0:nseq, :], ada_sb[0:nseq, 5, :], nv[0:nseq, :], ALU.mult)
        k.dma("sp", ada_d[:, 5 * D:6 * D], Fs[4][0:nseq, :])

        mhw = sm[:, 16:24]
        k.dma("sp", mhw, mhw_d.rearrange("o (kc p) -> p (o kc)", p=128), allow_slow_non_contiguous=True)
        for bi, (nm, c0, ncol) in enumerate(WBLOCKS):
            st = stage[bi % 2]
            wb = wbuf[bi % 2]
            src = wsrc[nm].rearrange("(kc p) n -> p kc n", p=128)
            k.dma("sp", st[:, :, 0:ncol], src[:, :, c0:c0 + ncol])
            if nm == "w_branch_m":
                for kc in range(8):
                    k.ts(wb[:, kc, 0:ncol], st[:, kc, 0:ncol], mhw[:, kc:kc + 1], None, ALU.mult)
            else:
                k.cp(wb[:, 0:4, 0:ncol], st[:, 0:4, 0:ncol], e="dve")
                k.cp(wb[:, 4:8, 0:ncol], st[:, 4:8, 0:ncol], e="act")
            k.dma("pool", wsc_d[bi].rearrange("p (kc n) -> p kc n", n=512)[:, :, 0:ncol], wb[:, :, 0:ncol])
        for (src_d, dst_d) in ((pu_d, ub_d), (pv_d, vb_d)):
            sv_ = src_d.rearrange("(c p r) d -> c p (r d)", p=128, r=4)
            dv_ = dst_d.rearrange("(c p r) d -> c p (r d)", p=128, r=4)
            for c_ in range(32):
                stf = stage[c_ % 2][:].rearrange("p a n -> p (a n)")
                wbf = wbuf[c_ % 2][:].rearrange("p a n -> p (a n)")
                k.dma("sp", stf, sv_[c_])
                k.cp(wbf[:, 0:2048], stf[:, 0:2048], e="dve")
                k.cp(wbf[:, 2048:4096], stf[:, 2048:4096], e="act")
                k.dma("pool", dv_[c_], wbf)
        for hc in range(16):
            st = stage[hc % 2]
            k.dma("sp", st[:, 0, 0:128], sk_d[hc])
            k.tp(PB[hc % 2][:, 0:128], st[:, 0, 0:128], identf)
            k.cp(skT[:, hc, :], PB[hc % 2][:, 0:128])
        k.barrier()
        es2.close()
        gbufs = [k.sb("gbuf%d" % i, [128, D], BF16) for i in range(NGBUF)]
        resident = {}
        for bi in RESIDENT:
            rt = k.sb("wres%d" % bi, [128, 8, 512], BF16)
            k.dma("sp", rt[:], wsc_d[bi].rearrange("p (kc n) -> p kc n", n=512))
            resident[bi] = rt
        X1p = [k.sb("X1p%d" % i, [128, D], F32) for i in range(2)]
        H2p = [k.sb("H2p%d" % i, [128, D], F32) for i in range(2)]
        eidxp = [k.sb("eidxp%d" % i, [128, 128], U32) for i in range(2)]
        gatep = [k.sb("gatep%d" % i, [128, 128], F32) for i in range(2)]
        JKb = k.sb("JKb", [128, D], BF16)
        TMPt = k.sb("TMPt", [128, D], F32)
        Dg = [k.sb("Dg%d" % i, [128, 128], BF16) for i in range(4)]
        smb = k.sb("smb", [128, 8], F32)

        wstate = {"n": 0}

        def load_block(bi):
            if bi in resident:
                return resident[bi]
            wb = wbuf[wstate["n"] % 2]
            wstate["n"] += 1
            ncol = WBLOCKS[bi][2]
            k.dma("sp", wb[:, :, 0:ncol], wsc_d[bi].rearrange("p (kc n) -> p kc n", n=512)[:, :, 0:ncol])
            return wb

        def dump(ap, col0, ncols, parts=128):
            if dbg_d is not None:
                k.dma("sp", dbg_d[0:parts, col0:col0 + ncols], ap)

        for s in range(nseq):
            k.dma("sp", rep[:].rearrange("p a d -> p (a d)"), ada_d[s:s + 1, :].partition_broadcast(128))
            W1, SH1, G1N, W2, SH2, G2N = [rep[:, i, :] for i in range(6)]
            k.memset(UA[:, :, 0:2], 0.0)
            k.memset(UQ[:, :, 0:3], 0.0)
            k.memset(Cf[:], 0.0)
            k.memset(Cb[:], 0.0)
            k.memset(nf[:], 0.0)
            k.memset(nb[:], 0.0)
            def front(s, ti, par):
                r0 = s * SEQ + ti * 128
                eidx, gate = eidxp[par], gatep[par]
                X = X1p[par]
                k.dma("sp", X[:], x_d[r0:r0 + 128, :])
                ssq = sm[:, 32:33]
                k.act(Fs[0][:], X[:], AF.Square, accum_out=ssq)
                rstd = sm[:, 33:34]
                k.ts(sm[:, 34:35], ssq, 1.0 / D, EPS, ALU.mult, ALU.add)
                k.act(sm[:, 35:36], sm[:, 34:35], AF.Sqrt)
                k.recip(rstd, sm[:, 35:36])
                k.stt(Fs[0][:], X[:], rstd, W1, ALU.mult, ALU.mult)
                hb = Hs[0]
                k.tt(hb[:], Fs[0][:], SH1, ALU.add)
                for kc in range(8):
                    k.tp(PT[:, kc * 128:(kc + 1) * 128], hb[:, kc * 128:(kc + 1) * 128], identb)
                hT = Hs[1]
                k.cp(hT[:], PT[:])
                hTv = hT[:].rearrange("p (kc t) -> p kc t", t=128)
                if stop_after == "hT":
                    k.cp(Fs[1][:], hT[:])
                    dump(Fs[1][:], 0, 1024)
                    return
                yield
                CG, BG, XIN = Fs[1], Fs[2], Fs[3]
                SGA, SGM = Hs[2], Hs[3]
                Vb = Hs[4]
                SO = Fs[4]
                IFt = sm[:, 40:56]

                def fm_block(bi, dst_fn, pbi):
                    wb = load_block(bi)
                    pb = PB[pbi]
                    for j in range(4):
                        for kc in range(8):
                            k.mm(pb[:, j * 128:(j + 1) * 128], wb[:, kc, j * 128:(j + 1) * 128], hTv[:, kc, :],
                                 start=(kc == 0), stop=(kc == 7))
                    dst_fn(pb)

                def evac_plain(dst3, e):
                    def f(pb):
                        k.cp(dst3, pb[:].rearrange("p (j t) -> p j t", t=128), e=e)
                    return f

                def evac_sig(dst3):
                    def f(pb):
                        k.act(dst3, pb[:].rearrange("p (j t) -> p j t", t=128), AF.Sigmoid)
                    return f

                def v3(t, lo):
                    return t[:].rearrange("p (j t) -> p j t", t=128)[:, lo:lo + 4, :]

                fm_block(0, evac_plain(v3(CG, 0), "act"), 0)
                fm_block(1, evac_plain(v3(CG, 4), "dve"), 1)
                yield
                fm_block(2, evac_plain(v3(BG, 0), "act"), 0)
                fm_block(3, evac_plain(v3(BG, 4), "dve"), 1)
                yield
                fm_block(4, evac_plain(v3(XIN, 0), "act"), 0)
                fm_block(5, evac_plain(v3(XIN, 4), "dve"), 1)
                yield
                fm_block(6, evac_plain(UQ[:, 0:4, 3:131], "act"), 0)
                fm_block(7, evac_plain(UQ[:, 4:8, 3:131], "dve"), 1)
                yield
                for gi, bi in enumerate((8, 9, 10, 11)):
                    wb = load_block(bi)
                    pb = PB[2 + gi % 2]
                    for kc in range(8):
                        k.mm(pb[:], hTv[:, kc, :], wb[:, kc, :], start=(kc == 0), stop=(kc == 7))
                    if gi < 2:
                        k.cp(Vb[:, gi * 512:(gi + 1) * 512], pb[:], e="dve")
                    else:
                        k.act(SO[:, (gi - 2) * 512:(gi - 1) * 512], pb[:], AF.Sigmoid)
                wb = load_block(12)
                for kc in range(8):
                    k.mm(PB[4][:, 0:16], hTv[:, kc, :], wb[:, kc, 0:16], start=(kc == 0), stop=(kc == 7))
                k.tt(IFt, PB[4][:, 0:16], bif[:], ALU.add)
                yield
                fm_block(13, evac_sig(v3(SGA, 0)), 0)
                fm_block(14, evac_sig(v3(SGA, 4)), 1)
                yield
                fm_block(15, evac_sig(v3(SGM, 0)), 0)
                fm_block(16, evac_sig(v3(SGM, 4)), 1)
                if stop_after == "proj":
                    dump(CG[:], 0, 1024)
                    dump(UQ[:, :, 3:131], 1024, 1024)
                    k.cp(Fs[0][:], Vb[:])
                    dump(Fs[0][:], 2048, 1024)
                    dump(SO[:], 3072, 1024)
                    dump(IFt, 4096, 16)
                    k.cp(Fs[5][:], SGM[:])
                    dump(Fs[5][:], 4112, 1024)
                    return
                yield
                CG3 = CG[:].rearrange("p (j t) -> p j t", t=128)
                BG3 = BG[:].rearrange("p (j t) -> p j t", t=128)
                XIN3 = XIN[:].rearrange("p (j t) -> p j t", t=128)
                k.tt(UA[:, :, 2:130], CG3, XIN3, ALU.mult)
                T0 = Fs[0][:].rearrange("p (j t) -> p j t", t=128)
                T1 = CG3
                k.tt(T0, UA[:, :, 0:128], cwa[:, 0, :].unsqueeze(2).to_broadcast([128, 8, 128]), ALU.mult)
                k.tt(T1, UA[:, :, 1:129], cwa[:, 1, :].unsqueeze(2).to_broadcast([128, 8, 128]), ALU.mult)
                k.tt(T0, T0, T1, ALU.add)
                k.tt(T1, UA[:, :, 2:130], cwa[:, 2, :].unsqueeze(2).to_broadcast([128, 8, 128]), ALU.mult)
                k.tt(T0, T0, T1, ALU.add)
                yaT = Hs[5]
                k.tt(yaT[:].rearrange("p (j t) -> p j t", t=128), T0, BG3, ALU.mult)
                k.cp(UA[:, :, 0:2], UA[:, :, 128:130])
                yield
                T1 = XIN3
                k.tt(T0, UQ[:, :, 0:128], cwq[:, 0, :].unsqueeze(2).to_broadcast([128, 8, 128]), ALU.mult)
                for j in range(1, 4):
                    k.tt(T1, UQ[:, :, j:j + 128], cwq[:, j, :].unsqueeze(2).to_broadcast([128, 8, 128]), ALU.mult)
                    k.tt(T0, T0, T1, ALU.add)
                qkT = Hs[6]
                qk3 = qkT[:].rearrange("p (j t) -> p j t", t=128)
                k.act(qk3, T0, AF.Silu)
                k.cp(UQ[:, :, 0:3], UQ[:, :, 128:131])
                yield
                IG = IFt[:, 0:8]
                FG = IFt[:, 8:16]
                lf = sm[:, 56:64]
                k.act(lf, FG, AF.Exp, scale=-1.0)
                k.act(lf, lf, AF.Ln, bias=1.0)
                k.ts(lf, lf, -1.0, None, ALU.mult)
                k.mm(PB[4][:, 16:24], tri2, lf)
                k.mm(PB[4][:, 24:32], ones128, lf)
                Bt = sm[:, 64:72]
                BL = sm[:, 72:80]
                k.cp(Bt, PB[4][:, 16:24])
                k.cp(BL, PB[4][:, 24:32])
                u = sm[:, 88:96]
                eB = sm[:, 96:104]
                wk = sm[:, 104:112]
                eBL = sm[:, 112:116]
                k.tt(u, IG, Bt, ALU.subtract)
                k.tt(wk, u, BL, ALU.add)
                k.act(u, u, AF.Exp)
                k.act(wk, wk, AF.Exp)
                k.act(eB, Bt, AF.Exp)
                BLv = BL.rearrange("p (j two) -> p j two", two=2)
                k.act(eBL[0:64, :], BLv[0:64, :, 0], AF.Exp)
                k.act(eBL[64:128, :], BLv[64:128, :, 1], AF.Exp)
                for j in range(4):
                    k.tp(PT[:, j * 128:(j + 1) * 128], qk3[:, 4 + j, :], identb)
                kw = Hs[7]
                kw3 = kw[:, 0:512].rearrange("p (h d) -> p h d", d=64)
                k.tt(kw3, PT[:, 0:512].rearrange("p (h d) -> p h d", d=64), wk.unsqueeze(2).to_broadcast([128, 8, 64]), ALU.mult)
                yield
                Qm = [Fs[0][:, 0:256].bitcast(BF16).rearrange("p (j t) -> p j t", t=128),
                      Fs[0][:, 256:512].bitcast(BF16).rearrange("p (j t) -> p j t", t=128)]
                for e_ in range(2):
                    k.ts(Qm[e_], qk3[:, 0:4, :], rowmask[:, e_:e_ + 1], None, ALU.mult)
                SB = [PB[2], PB[3]]
                for h in range(8):
                    k.mm(SB[h // 4][:, (h % 4) * 128:(h % 4 + 1) * 128], qk3[:, 4 + h // 2, :], Qm[h % 2][:, h // 2, :])
                SwT = Hs[1]
                Sw3 = SwT[:].rearrange("p (h t) -> p h t", t=128)
                S3 = Fs[5][:].rearrange("p (h t) -> p h t", t=128)
                for g2 in range(2):
                    k.tt(S3[:, g2 * 4:(g2 + 1) * 4, :], SB[g2][:].rearrange("p (h t) -> p h t", t=128),
                         u[:, g2 * 4:(g2 + 1) * 4].unsqueeze(2).to_broadcast([128, 4, 128]), ALU.mult)
                k.tt(Sw3, S3, tri2.unsqueeze(1).to_broadcast([128, 8, 128]), ALU.mult)
                yield
                Vb3 = Vb[:].rearrange("p (h d) -> p h d", d=128)
                NUM = [PB[0], PB[1]]
                DEN = PB[4][:, 40:48]
                for h in range(8):
                    q_h = Qm[h % 2][:, h // 2, :]
                    numo = NUM[h // 4][:, (h % 4) * 128:(h % 4 + 1) * 128]
                    k.mm(numo, Sw3[:, h, :], Vb3[:, h, :], start=True, stop=False)
                    k.mm(numo, q_h, Cb[:, h // 2, :], start=False, stop=True)
                    k.mm(DEN[:, h:h + 1], Sw3[:, h, :], onesb_t[:], start=True, stop=False)
                    k.mm(DEN[:, h:h + 1], q_h, nb[:, h // 2:h // 2 + 1], start=False, stop=True)
                yield
                kwp = kw[:, 0:512].rearrange("p (j d) -> p j d", d=128)
                for h in range(8):
                    k.mm(PB[2 + h % 2][:, (h // 2) * 128:(h // 2 + 1) * 128], kwp[:, h // 2, :], Vb3[:, h, :])
                for j in range(4):
                    k.mm(PB[4][:, 48 + j:49 + j], kwp[:, j, :], onesb_t[:])
                k.tt(Cf[:], Cf[:], eBL.unsqueeze(2).to_broadcast([128, 4, 128]), ALU.mult)
                k.tt(Cf[0:64], Cf[0:64], PB[2][0:64, :].rearrange("p (j d) -> p j d", d=128), ALU.add)
                k.tt(Cf[64:128], Cf[64:128], PB[3][64:128, :].rearrange("p (j d) -> p j d", d=128), ALU.add)
                k.cp(Cb[:], Cf[:], e="act")
                k.tt(nf[:], nf[:], eBL, ALU.mult)
                k.tt(nf[:], nf[:], PB[4][:, 48:52], ALU.add)
                k.cp(nb[:], nf[:], e="act")
                yield
                dn = sm[:, 120:128]
                k.stt(dn, DEN, 0.125, eB, ALU.mult, ALU.mult)
                k.act(dn, dn, AF.Abs)
                k.ts(dn, dn, 1.0, None, ALU.max)
                k.recip(dn, dn)
                k.stt(dn, eB, 0.125, dn, ALU.mult, ALU.mult)
                HN = Fs[5]
                HN3 = HN[:].rearrange("p (h d) -> p h d", d=128)
                for g2 in range(2):
                    k.tt(HN3[:, g2 * 4:(g2 + 1) * 4, :], NUM[g2][:].rearrange("p (h d) -> p h d", d=128),
                         dn[:, g2 * 4:(g2 + 1) * 4].unsqueeze(2).to_broadcast([128, 4, 128]), ALU.mult)
                if stop_after == "mlstm":
                    dump(HN[:], 0, 1024)
                    k.cp(Fs[0][:], yaT[:])
                    dump(Fs[0][:], 1024, 1024)
                    return
                yield
                k.tt(Fs[0][:], HN[:], HN[:], ALU.mult)
                hss = sm[:, 128:136]
                k.red(hss, Fs[0][:].rearrange("p (h d) -> p h d", d=128), ALU.add)
                k.ts(hss, hss, 1.0 / 128, EPS, ALU.mult, ALU.add)
                k.act(hss, hss, AF.Sqrt)
                k.recip(hss, hss)
                k.tt(HN3, HN3, hss.unsqueeze(2).to_broadcast([128, 8, 128]), ALU.mult)
                ymb = Hs[0]
                k.tt(ymb[:], HN[:], SO[:], ALU.mult)
                for kc in range(8):
                    k.tp(PT[:, kc * 128:(kc + 1) * 128], ymb[:, kc * 128:(kc + 1) * 128], identb)
                ymT = Hs[4]
                k.cp(ymT[:], PT[:])
                ymT3 = ymT[:].rearrange("p (kc t) -> p kc t", t=128)
                yaT3 = yaT[:].rearrange("p (kc t) -> p kc t", t=128)
                yield
                MIX = Fs[0]
                MIX3 = MIX[:].rearrange("p (j t) -> p j t", t=128)

                def branch(bname, src3, sg, first):
                    for half in range(2):
                        wb = load_block(BLK[(bname, half * 512)])
                        pb = PB[2 + half]
                        for j in range(4):
                            for kc in range(8):
                                k.mm(pb[:, j * 128:(j + 1) * 128], wb[:, kc, j * 128:(j + 1) * 128], src3[:, kc, :],
                                     start=(kc == 0), stop=(kc == 7))
                        sg3 = sg[:].rearrange("p (j t) -> p j t", t=128)[:, half * 4:(half + 1) * 4, :]
                        dst = MIX3[:, half * 4:(half + 1) * 4, :]
                        p3 = pb[:].rearrange("p (j t) -> p j t", t=128)
                        if first:
                            k.tt(dst, p3, sg3, ALU.mult)
                        else:
                            t3 = Fs[1][:].rearrange("p (j t) -> p j t", t=128)[:, half * 4:(half + 1) * 4, :]
                            k.tt(t3, p3, sg3, ALU.mult)
                            k.tt(dst, dst, t3, ALU.add)

                branch("w_branch_a", yaT3, SGA, True)
                yield
                branch("w_branch_m", ymT3, SGM, False)
                yield
                mixT = Hs[2]
                k.cp(mixT[:], MIX[:], e="act")
                mixT3 = mixT[:].rearrange("p (kc t) -> p kc t", t=128)
                for half in range(2):
                    wb = load_block(BLK[("w_out", half * 512)])
                    for kc in range(8):
                        k.mm(PB[half][:], mixT3[:, kc, :], wb[:, kc, :], start=(kc == 0), stop=(kc == 7))
                yield
                Y = Fs[1]
                k.cp(Y[:, 0:512], PB[0][:], e="act")
                k.cp(Y[:, 512:1024], PB[1][:], e="dve")
                ssq2 = sm[:, 136:137]
                k.act(Fs[0][:], Y[:], AF.Square, accum_out=ssq2)
                k.ts(ssq2, ssq2, 1.0 / D, EPS, ALU.mult, ALU.add)
                k.act(ssq2, ssq2, AF.Sqrt)
                k.recip(ssq2, ssq2)
                k.stt(Fs[0][:], Y[:], ssq2, G1N, ALU.mult, ALU.mult)
                X1 = X1p[par]
                k.tt(X1[:], Fs[0][:], X[:], ALU.add)
                if stop_after == "sub1":
                    k.dma("sp", out_d[r0:r0 + 128, :], X1[:])
                    return
                yield
                ssq3 = sm[:, 137:138]
                k.act(Fs[0][:], X1[:], AF.Square, accum_out=ssq3)
                k.ts(ssq3, ssq3, 1.0 / D, EPS, ALU.mult, ALU.add)
                k.act(ssq3, ssq3, AF.Sqrt)
                k.recip(ssq3, ssq3)
                k.stt(Fs[0][:], X1[:], ssq3, W2, ALU.mult, ALU.mult)
                H2 = H2p[par]
                k.tt(H2[:], Fs[0][:], SH2, ALU.add)
                h2b = Hs[0]
                k.cp(h2b[:], H2[:], e="act")
                for kc in range(8):
                    k.tp(PT[:, kc * 128:(kc + 1) * 128], h2b[:, kc * 128:(kc + 1) * 128], identb)
                h2T = Hs[1]
                k.cp(h2T[:], PT[:])
                h2T3 = h2T[:].rearrange("p (kc t) -> p kc t", t=128)
                yield
                for g4 in range(4):
                    wb = load_block(BLK[("peer_wq", g4 * 512)])
                    pb = PB[2 + g4 % 2]
                    for j in range(4):
                        for kc in range(8):
                            k.mm(pb[:, j * 128:(j + 1) * 128], wb[:, kc, j * 128:(j + 1) * 128], h2T3[:, kc, :],
                                 start=(kc == 0), stop=(kc == 7))
                    k.cp(qT[:, g4 * 4:(g4 + 1) * 4, :], pb[:].rearrange("p (j t) -> p j t", t=128), e=("act" if g4 % 2 else "dve"))
                yield
                scv = [Fs[1][:].rearrange("p (j n) -> p j n", n=128), Fs[2][:].rearrange("p (j n) -> p j n", n=128)]
                tk0v = Fs[3][:].rearrange("p (h a b) -> p h a b", a=16, b=16)
                for g4 in range(4):
                    pb = PB[g4 % 2]
                    for j in range(4):
                        hc = g4 * 4 + j
                        k.mm(pb[:, j * 128:(j + 1) * 128], qT[:, hc, :], skT[:, hc, :])
                    k.cp(scv[g4 // 2][:, (g4 % 2) * 4:(g4 % 2) * 4 + 4, :], pb[:].rearrange("p (j n) -> p j n", n=128), e=("act" if g4 % 2 else "dve"))
                yield
                dv = k.eng["dve"]
                for h in range(8):
                    for half, (vv, ii) in enumerate(((v1, i1), (v2, i2))):
                        s_ = scv[(2 * h + half) // 8][:, (2 * h + half) % 8, :]
                        k.op("dve", lambda g, vv=vv, h=h, s_=s_: g.max(vv[:, h, 0:8], s_), [s_], [vv[:]])
                        k.op("dve", lambda g, vv=vv, ii=ii, h=h, s_=s_: g.max_index(ii[:, h, 0:8], vv[:, h, 0:8], s_), [s_, vv[:]], [ii[:]])
                        k.op("dve", lambda g, vv=vv, h=h, s_=s_: g.match_replace(scb[:], vv[:, h, 0:8], s_, NEG), [s_, vv[:]], [scb[:]])
                        k.op("dve", lambda g, vv=vv, h=h: g.max(vv[:, h, 8:16], scb[:]), [scb[:]], [vv[:]])
                        k.op("dve", lambda g, vv=vv, ii=ii, h=h: g.max_index(ii[:, h, 8:16], vv[:, h, 8:16], scb[:]), [scb[:], vv[:]], [ii[:]])
                    yield
                    c3 = cand[:].rearrange("p (a b) -> p a b", b=16)
                    k.tt(c3, v1[:, h, :].unsqueeze(2).to_broadcast([128, 16, 16]),
                         v2[:, h, :].unsqueeze(1).to_broadcast([128, 16, 16]), ALU.add)
                    k.op("dve", lambda g, h=h: g.max(cv[:, h, 0:8], cand[:]), [cand[:]], [cv[:]])
                    k.op("dve", lambda g, h=h: g.max_index(ci[:, h, 0:8], cv[:, h, 0:8], cand[:]), [cand[:], cv[:]], [ci[:]])
                    k.op("dve", lambda g, h=h: g.match_replace(candb[:], cv[:, h, 0:8], cand[:], NEG), [cand[:], cv[:]], [candb[:]])
                    k.op("dve", lambda g, h=h: g.max(cv[:, h, 8:16], candb[:]), [candb[:]], [cv[:]])
                    k.op("dve", lambda g, h=h: g.max_index(ci[:, h, 8:16], cv[:, h, 8:16], candb[:]), [candb[:], cv[:]], [ci[:]])
                yield
                ua, ub_ = tku[0], tku[1]
                k.ts(ua[:], ci[:], 4, None, ALU.logical_shift_right)
                k.ts(ub_[:], ci[:], 15, None, ALU.bitwise_and)
                af, bf_, i1f, i2f, isel, jsel = tkf
                k.cp(af[:], ua[:])
                k.cp(bf_[:], ub_[:])
                k.cp(i1f[:], i1[:])
                k.cp(i2f[:], i2[:])
                io4 = iota16.unsqueeze(1).unsqueeze(1).to_broadcast([128, 4, 16, 16])
                for (xf, tab, dst) in ((af, i1f, isel), (bf_, i2f, jsel)):
                    for hh in range(2):
                        hs4 = slice(hh * 4, hh * 4 + 4)
                        k.tt(tk0v, xf[:, hs4, :].unsqueeze(3).to_broadcast([128, 4, 16, 16]), io4, ALU.is_equal)
                        k.tt(tk0v, tk0v, tab[:, hs4, :].unsqueeze(2).to_broadcast([128, 4, 16, 16]), ALU.mult)
                        k.red(dst[:, hs4, :], tk0v, ALU.add)
                ef = af
                k.stt(ef[:], isel[:], 128.0, jsel[:], ALU.mult, ALU.add)
                k.cp(eidx[:].rearrange("p (h k) -> p h k", k=16), ef[:])
                yield
                gx = bf_
                k.tt(gx[:], cv[:], cv[:, :, 0:1].to_broadcast([128, 8, 16]), ALU.subtract)
                k.act(gx[:], gx[:], AF.Exp)
                gs = sm[:, 144:152]
                k.red(gs, gx[:], ALU.add)
                k.recip(gs, gs)
                k.tt(gate[:].rearrange("p (h k) -> p h k", k=16), gx[:], gs.unsqueeze(2).to_broadcast([128, 8, 16]), ALU.mult)
                if stop_after == "topk":
                    k.cp(Fs[0][:, 0:128], eidx[:])
                    dump(Fs[0][:, 0:128], 0, 128)
                    dump(gate[:], 128, 128)
                    dump(H2[:], 256, 1024)
                    return
                yield
            def back(s, ti, par):
                r0 = s * SEQ + ti * 128
                X1, H2, eidx, gate = X1p[par], H2p[par], eidxp[par], gatep[par]
                NG = len(gbufs)
                for sl in range(128):
                    gb = gbufs[sl % NG][:]
                    k.dma("pool", gb, ub_d, fn=lambda g, gb=gb, sl=sl, eidx=eidx: g.indirect_dma_start(
                        out=gb, out_offset=None, in_=ub_d[:, :],
                        in_offset=bass.IndirectOffsetOnAxis(ap=eidx[:, sl:sl + 1], axis=0)), extra_reads=[eidx[:]])
                    k.stt(JKb[:], gb, 1.0, H2[:], ALU.mult, ALU.mult, accum_out=zz[:, sl:sl + 1])
                    if sl % 4 == 3:
                        yield
                k.tt(aa[:], zz[:], zz[:], ALU.mult)
                k.ts(aa[:], aa[:], 0.044715, 1.0, ALU.mult, ALU.add)
                k.tt(aa[:], aa[:], zz[:], ALU.mult)
                k.act(aa[:], aa[:], AF.Sigmoid, scale=1.5957691216057308)
                k.tt(aa[:], aa[:], zz[:], ALU.mult)
                k.tt(aa[:], aa[:], gate[:], ALU.mult)
                yield
                for sl in range(128):
                    gb = gbufs[sl % NG][:]
                    k.dma("pool", gb, vb_d, fn=lambda g, gb=gb, sl=sl, eidx=eidx: g.indirect_dma_start(
                        out=gb, out_offset=None, in_=vb_d[:, :],
                        in_offset=bass.IndirectOffsetOnAxis(ap=eidx[:, sl:sl + 1], axis=0)), extra_reads=[eidx[:]])
                    dg = Dg[sl % 4]
                    k.act(dg[:], identb, AF.Copy, scale=aa[:, sl:sl + 1])
                    k.mm(PB[5][:], dg[:], gb[:, 0:512], start=(sl == 0), stop=(sl == 127))
                    k.mm(PB[6][:], dg[:], gb[:, 512:1024], start=(sl == 0), stop=(sl == 127))
                    if sl % 4 == 3:
                        yield
                ssq4 = smb[:, 0:1]
                k.act(TMPt[:, 0:512], PB[5][:], AF.Square, accum_out=ssq4)
                k.act(TMPt[:, 512:1024], PB[6][:], AF.Square, accum_out=smb[:, 1:2])
                k.tt(ssq4, ssq4, smb[:, 1:2], ALU.add)
                k.ts(ssq4, ssq4, 1.0 / D, EPS, ALU.mult, ALU.add)
                k.act(ssq4, ssq4, AF.Sqrt)
                k.recip(ssq4, ssq4)
                k.stt(TMPt[:, 0:512], PB[5][:], ssq4, rep[:, 5, 0:512], ALU.mult, ALU.mult)
                k.stt(TMPt[:, 512:1024], PB[6][:], ssq4, rep[:, 5, 512:1024], ALU.mult, ALU.mult)
                k.tt(TMPt[:], TMPt[:], X1[:], ALU.add)
                k.dma("sp", out_d[r0:r0 + 128, :], TMPt[:])
                yield

            prev = None
            for ti in range(ntile):
                fa = k.record(front(s, ti, ti % 2))
                fb = k.record(back(*prev)) if (prev is not None and stop_after is None) else []
                if fb:
                    k.emit_merged(fa, fb)
                else:
                    k.emit_merged(fa, [])
                prev = (s, ti, ti % 2)
            if stop_after is None:
                k.emit_merged(k.record(back(*prev)), [])
        k.barrier()
    return nc


def make_consts():
    c = np.zeros((128, 1024), np.float32)
    c[:, 0:128] = np.eye(128, dtype=np.float32)
    s = np.arange(128)[:, None]
    t = np.arange(128)[None, :]
    c[:, 128:256] = (s <= t).astype(np.float32)
    c[:, 256:384] = 1.0
    c[:, 384] = (np.arange(128) < 64).astype(np.float32)
    c[:, 385] = (np.arange(128) >= 64).astype(np.float32)
    c[:, 576:592] = np.arange(16, dtype=np.float32)[None, :]
    c[:, 592] = 1.0
    return c


_PROG = {}


def kernel(x, c, w_ada, b_ada, norm1_pre, norm1_post, w_in, conv_a_w, conv_qk_w, b_igate, b_fgate, mh_norm_w,
           w_branch_a, w_branch_m, w_out, norm2_pre, norm2_post, peer_wq, peer_subkeys, peer_u, peer_v):
    f = lambda a: np.ascontiguousarray(np.asarray(a, dtype=np.float32))
    if "nc" not in _PROG:
        _PROG["nc"] = build_program()
    nc = _PROG["nc"]
    shared = {
        "w_ada": f(w_ada[0]), "b_ada": f(b_ada), "norm1_pre": f(norm1_pre), "norm1_post": f(norm1_post),
        "w_in": f(w_in[0]), "conv_a_w": f(conv_a_w[0]), "conv_qk_w": f(conv_qk_w[0]), "b_igate": f(b_igate),
        "b_fgate": f(b_fgate), "mh_norm_w": f(mh_norm_w), "w_branch_a": f(w_branch_a[0]),
        "w_branch_m": f(w_branch_m[0]), "w_out": f(w_out[0]), "peer_wq": f(peer_wq[0]),
        "norm2_pre": f(norm2_pre), "norm2_post": f(norm2_post),
        "peer_subkeys": f(peer_subkeys[0]).reshape(16, 128, 128), "peer_u": f(peer_u[0]), "peer_v": f(peer_v[0]),
        "cst": make_consts(),
    }
    xs = f(x)
    cs = f(c)
    in_maps = []
    for i in range(NCORES):
        m = dict(shared)
        m["x"] = xs[2 * i:2 * i + 2].reshape(2 * SEQ, D)
        m["c"] = cs[2 * i:2 * i + 2]
        in_maps.append(m)
    res = run_bass_kernel_spmd(nc, in_maps, core_ids=list(range(NCORES)))
    out = np.concatenate([r["out"].reshape(2, SEQ, D) for r in res.results], axis=0)
    return out.astype(np.float32)
```

```python
from contextlib import ExitStack
import numpy as np
import concourse.bass as bass
import concourse.mybir as mybir
from concourse.bass_utils import run_bass_kernel_spmd

F32 = mybir.dt.float32
BF16 = mybir.dt.bfloat16
U32 = mybir.dt.uint32
I32 = mybir.dt.int32
AF = mybir.ActivationFunctionType
ALU = mybir.AluOpType
AX = mybir.AxisListType

D = 1024
NCORES = 8
SEQ = 4096
NSEQ_CORE = 2
IN_W = 8208
EPS = 1e-6
NEG = -1.0e30

O_CG, O_BG, O_XIN, O_Q, O_K, O_V, O_O, O_I, O_F, O_GA, O_GM = 0, 1024, 2048, 3072, 3584, 4096, 5120, 6144, 6152, 6160, 7184

WBLOCKS = []
for c0 in range(0, 6144, 512):
    WBLOCKS.append(("w_in", c0, 512))
WBLOCKS.append(("w_in", 6144, 16))
for c0 in range(6160, 8208, 512):
    WBLOCKS.append(("w_in", c0, 512))
for nm in ("w_branch_a", "w_branch_m", "w_out"):
    for c0 in (0, 512):
        WBLOCKS.append((nm, c0, 512))
for c0 in range(0, 2048, 512):
    WBLOCKS.append(("peer_wq", c0, 512))
NBLK = len(WBLOCKS)
NGBUF = 16
RESIDENT = tuple(range(0, 4))
BLK = {(nm, c0): i for i, (nm, c0, n) in enumerate(WBLOCKS)}


class K:
    def __init__(self, nc, es):
        self.nc = nc
        self.es = es
        self.eng = {"pe": nc.tensor, "act": nc.scalar, "dve": nc.vector, "pool": nc.gpsimd, "sp": nc.sync}
        self.esem = {e: es.enter_context(nc.semaphore("sem_" + e)) for e in self.eng}
        self.ecnt = {e: 0 for e in self.eng}
        self.semobj = {("e", e): self.esem[e] for e in self.eng}
        self.waited = {e: {} for e in self.eng}
        self.tr = {}
        self.dsem = {}
        self.dcnt = {}
        self.same_engine_sync = {"pe": False, "act": True, "dve": True, "pool": True, "sp": True}
        self.rec = None

    def _reg(self, h, name):
        self.tr[name] = {"w": {}, "r": {}}
        return h

    def sb(self, name, shape, dt, es=None):
        return self._reg((es or self.es).enter_context(self.nc.sbuf_tensor(name, shape, dt)), name)

    def ps(self, name, shape, dt):
        return self._reg(self.es.enter_context(self.nc.psum_tensor(name, shape, dt)), name)

    def dram(self, name, shape, dt, kind):
        t = self.nc.dram_tensor(name, shape, dt, kind=kind)
        self.tr[name] = {"w": {}, "r": {}}
        return t.ap()

    def _rec(self, ap):
        return self.tr[ap.tensor.name if hasattr(ap, "tensor") else ap.name]

    def _name(self, ap):
        try:
            return ap.tensor.name
        except Exception:
            return ap.name

    def _deps(self, reads, writes):
        ev = {}
        for ap in reads:
            rec = self.tr[self._name(ap)]
            for k, v in rec["w"].items():
                ev[k] = max(ev.get(k, 0), v)
        for ap in writes:
            rec = self.tr[self._name(ap)]
            for k, v in rec["w"].items():
                ev[k] = max(ev.get(k, 0), v)
            for k, v in rec["r"].items():
                ev[k] = max(ev.get(k, 0), v)
        return ev

    def _wait(self, e, ev):
        for k, v in ev.items():
            if k == ("e", e) and not self.same_engine_sync[e]:
                continue
            if self.waited[e].get(k, 0) < v:
                self.eng[e].wait_ge(self.semobj[k], v)
                self.waited[e][k] = v

    def _mark(self, key, val, reads, writes):
        for ap in reads:
            rec = self.tr[self._name(ap)]
            rec["r"][key] = max(rec["r"].get(key, 0), val)
        for ap in writes:
            rec = self.tr[self._name(ap)]
            rec["w"][key] = max(rec["w"].get(key, 0), val)

    def op(self, e, fn, reads, writes):
        if self.rec is not None:
            self.rec.append(("op", e, fn, list(reads), list(writes)))
            return None
        self._wait(e, self._deps(reads, writes))
        ins = fn(self.eng[e])
        self.ecnt[e] += 1
        ins.then_inc(self.esem[e], 1)
        self._mark(("e", e), self.ecnt[e], reads, writes)
        return ins

    def dma(self, e, out, in_, fn=None, extra_reads=(), **kw):
        if self.rec is not None:
            self.rec.append(("dma", e, out, in_, fn, list(extra_reads), kw))
            return None
        rds = [in_] + list(extra_reads)
        self._wait(e, self._deps(rds, [out]))
        dn = self._name(out)
        if dn not in self.dsem:
            self.dsem[dn] = self.es.enter_context(self.nc.semaphore("dsem_" + dn))
            self.dcnt[dn] = 0
            self.semobj[("d", dn)] = self.dsem[dn]
        if fn is None:
            ins = self.eng[e].dma_start(out=out, in_=in_, **kw)
        else:
            ins = fn(self.eng[e])
        self.dcnt[dn] += 16
        ins.then_inc(self.dsem[dn], 16)
        self._mark(("d", dn), self.dcnt[dn], rds, [out])
        return ins

    def record(self, gen):
        self.rec = []
        for _ in gen:
            pass
        r, self.rec = self.rec, None
        return r

    def emit_merged(self, a, b):
        def cost(x):
            if x[0] == "dma":
                return 1.5 if x[1] == "pool" else 3.0
            e = x[1]
            try:
                shp = x[4][0].shape
                n = 1
                for d in shp[1:]:
                    n *= d
            except Exception:
                n = 128
            if e == "pe":
                return 0.04 + n / 2400.0
            if e == "act":
                return 0.15 + n / 1200.0
            return 0.1 + n / 960.0

        def pos(st):
            c = [cost(x) for x in st]
            tot = sum(c) or 1.0
            out, acc = [], 0.0
            for ci in c:
                out.append((acc + 0.5 * ci) / tot)
                acc += ci
            return out
        pa, pb = pos(a), pos(b)
        items = [(pa[i], 0, i, x) for i, x in enumerate(a)] + [(pb[j], 1, j, x) for j, x in enumerate(b)]
        items.sort(key=lambda t: (t[0], t[1], t[2]))
        for _, _, _, x in items:
            if x[0] == "op":
                self.op(x[1], x[2], x[3], x[4])
            else:
                self.dma(x[1], x[2], x[3], fn=x[4], extra_reads=x[5], **x[6])

    def barrier(self):
        ev = {}
        for e in self.eng:
            if self.ecnt[e]:
                ev[("e", e)] = self.ecnt[e]
        for dn, c in self.dcnt.items():
            ev[("d", dn)] = c
        for e in self.eng:
            for k, v in ev.items():
                if k == ("e", e):
                    continue
                if self.waited[e].get(k, 0) < v:
                    self.eng[e].wait_ge(self.semobj[k], v)
                    self.waited[e][k] = v

    def mm(self, out, lhsT, rhs, start=True, stop=True):
        return self.op("pe", lambda g: g.matmul(out, lhsT, rhs, start=start, stop=stop), [lhsT, rhs], [out])

    def tp(self, out, in_, ident):
        return self.op("pe", lambda g: g.transpose(out, in_, ident), [in_, ident], [out])

    def act(self, out, in_, func, bias=None, scale=None, accum_out=None, extra_reads=()):
        kw = {}
        rd = [in_] + list(extra_reads)
        if bias is not None:
            kw["bias"] = bias
            if not isinstance(bias, (int, float)):
                rd.append(bias)
        if scale is not None:
            kw["scale"] = scale
            if not isinstance(scale, (int, float)):
                rd.append(scale)
        wr = [out]
        if accum_out is not None:
            kw["accum_out"] = accum_out
            wr.append(accum_out)
        return self.op("act", lambda g: g.activation(out, in_, func, **kw), rd, wr)

    def tt(self, out, in0, in1, op, e="dve"):
        return self.op(e, lambda g: g.tensor_tensor(out, in0, in1, op), [in0, in1], [out])

    def ts(self, out, in0, s1, s2, op0, op1=None, e="dve", accum_out=None):
        rd = [in0] + [s for s in (s1, s2) if s is not None and not isinstance(s, (int, float))]
        wr = [out] + ([accum_out] if accum_out is not None else [])
        kw = {}
        if op1 is not None:
            kw["op1"] = op1
        if accum_out is not None:
            kw["accum_out"] = accum_out
        return self.op(e, lambda g: g.tensor_scalar(out, in0, s1, s2, op0, **kw), rd, wr)

    def stt(self, out, in0, scalar, in1, op0, op1, accum_out=None):
        rd = [in0, in1] + ([scalar] if not isinstance(scalar, (int, float)) else [])
        wr = [out] + ([accum_out] if accum_out is not None else [])
        kw = {"accum_out": accum_out} if accum_out is not None else {}
        return self.op("dve", lambda g: g.scalar_tensor_tensor(out, in0, scalar, in1, op0, op1, **kw), rd, wr)

    def cp(self, out, in_, e="dve"):
        if e == "act":
            return self.op("act", lambda g: g.copy(out, in_), [in_], [out])
        return self.op(e, lambda g: g.tensor_copy(out, in_), [in_], [out])

    def memset(self, ap, val, e="dve"):
        return self.op(e, lambda g: g.memset(ap, val), [], [ap])

    def red(self, out, in_, op, axis=AX.X):
        return self.op("dve", lambda g: g.tensor_reduce(out, in_, axis, op), [in_], [out])

    def recip(self, out, in_):
        return self.op("dve", lambda g: g.reciprocal(out, in_), [in_], [out])


def build_program(nseq=NSEQ_CORE, ntile=SEQ // 128, dbg=None, stop_after=None):
    nc = bass.Bass("TRN2", target_bir_lowering=False)
    es = ExitStack()
    ntok = nseq * ntile * 128
    with es:
        k = K(nc, es)
        def din(name, shape, dt=F32):
            return nc.dram_tensor(name, shape, dt, kind="ExternalInput").ap()

        x_d = din("x", [nseq * SEQ, D])
        c_d = din("c", [nseq, D])
        w_ada_d = din("w_ada", [D, 6 * D])
        b_ada_d = din("b_ada", [1, 6 * D])
        n1pre_d = din("norm1_pre", [1, D])
        n1post_d = din("norm1_post", [1, D])
        w_in_d = din("w_in", [D, IN_W])
        conv_a_d = din("conv_a_w", [3, D])
        conv_qk_d = din("conv_qk_w", [4, D])
        big_d = din("b_igate", [1, 8])
        bfg_d = din("b_fgate", [1, 8])
        mhw_d = din("mh_norm_w", [1, D])
        wsrc = {"w_in": w_in_d,
                "w_branch_a": din("w_branch_a", [D, D]),
                "w_branch_m": din("w_branch_m", [D, D]),
                "w_out": din("w_out", [D, D]),
                "peer_wq": din("peer_wq", [D, 2048])}
        n2pre_d = din("norm2_pre", [1, D])
        n2post_d = din("norm2_post", [1, D])
        sk_d = din("peer_subkeys", [16, 128, 128])
        pu_d = din("peer_u", [16384, D])
        pv_d = din("peer_v", [16384, D])
        cst_d = din("cst", [128, 1024])
        for nm in ("x", "c", "w_ada", "b_ada", "norm1_pre", "norm1_post", "w_in", "conv_a_w", "conv_qk_w", "b_igate",
                   "b_fgate", "mh_norm_w", "w_branch_a", "w_branch_m", "w_out", "peer_wq", "norm2_pre", "norm2_post",
                   "peer_subkeys", "peer_u", "peer_v", "cst"):
            k.tr[nm] = {"w": {}, "r": {}}
        out_d = k.dram("out", [nseq * SEQ, D], F32, "ExternalOutput")
        wsc_d = k.dram("wsc", [NBLK, 128, 8 * 512], BF16, "Internal")
        ada_d = k.dram("ada_sc", [nseq, 6 * D], F32, "Internal")
        ub_d = k.dram("ub_sc", [16384, D], BF16, "Internal")
        vb_d = k.dram("vb_sc", [16384, D], BF16, "Internal")
        dbg_d = None
        if dbg is not None:
            dbg_d = k.dram("dbg", [128, dbg], F32, "ExternalOutput")

        cst = k.sb("cst_sb", [128, 1024], F32)
        identf = cst[:, 0:128]
        tri2 = cst[:, 128:256]
        ones128 = cst[:, 256:384]
        rowmask = cst[:, 384:386]
        iota16 = cst[:, 576:592]
        onescol = cst[:, 592:593]
        identb_t = k.sb("identb", [128, 128], BF16)
        onesb_t = k.sb("onesb", [128, 1], BF16)
        wbuf = [k.sb("wbuf%d" % i, [128, 8, 512], BF16) for i in range(2)]
        rep = k.sb("rep", [128, 6, 1024], F32)
        Fs = [k.sb("F%d" % i, [128, D], F32) for i in range(6)]
        Hs = [k.sb("H%d" % i, [128, D], BF16) for i in range(8)]
        UA = k.sb("UA", [128, 8, 130], F32)
        UQ = k.sb("UQ", [128, 8, 131], F32)
        cwa = k.sb("cwa", [128, 3, 8], F32)
        cwq = k.sb("cwq", [128, 4, 8], F32)
        skT = k.sb("skT", [128, 16, 128], BF16)
        Cf = k.sb("Cf", [128, 4, 128], F32)
        Cb = k.sb("Cb", [128, 4, 128], BF16)
        nf = k.sb("nf", [128, 4], F32)
        nb = k.sb("nb", [128, 4], BF16)
        sm = k.sb("sm", [128, 256], F32)
        sm2 = k.sb("sm2", [128, 256], F32)
        bif = k.sb("bif", [128, 16], F32)
        qT = k.sb("qT", [128, 16, 128], BF16)
        scb = k.sb("scb", [128, 128], F32)
        cand = k.sb("cand", [128, 256], F32)
        candb = k.sb("candb", [128, 256], F32)
        v1 = k.sb("v1", [128, 8, 16], F32)
        v2 = k.sb("v2", [128, 8, 16], F32)
        i1 = k.sb("i1", [128, 8, 16], U32)
        i2 = k.sb("i2", [128, 8, 16], U32)
        cv = k.sb("cv", [128, 8, 16], F32)
        ci = k.sb("ci", [128, 8, 16], U32)
        tkf = [k.sb("tkf%d" % i, [128, 8, 16], F32) for i in range(6)]
        tku = [k.sb("tku%d" % i, [128, 8, 16], U32) for i in range(2)]
        zz = k.sb("zz", [128, 128], F32)
        aa = k.sb("aa", [128, 128], F32)

        PB = [k.ps("PB%d" % i, [128, 512], F32) for i in range(7)]
        PT = k.ps("PT", [128, 1024], BF16)
        es2 = ExitStack()
        stage = [k.sb("stage%d" % i, [128, 8, 512], F32, es=es2) for i in range(2)]

        k.dma("sp", cst[:], cst_d[:, :])
        k.cp(identb_t[:], identf)
        k.cp(onesb_t[:], onescol)
        identb = identb_t[:]

        for j in range(3):
            k.dma("sp", cwa[:, j, :], conv_a_d[j:j + 1, :].rearrange("o (ch p) -> p (o ch)", p=128), allow_slow_non_contiguous=True)
        for j in range(4):
            k.dma("sp", cwq[:, j, :], conv_qk_d[j:j + 1, :].rearrange("o (ch p) -> p (o ch)", p=128), allow_slow_non_contiguous=True)
        k.dma("sp", bif[:, 0:8], big_d.partition_broadcast(128))
        k.dma("sp", bif[:, 8:16], bfg_d.partition_broadcast(128))

        cT = sm[:, 0:8 * nseq].rearrange("p (b kc) -> p b kc", b=nseq)
        for b_ in range(nseq):
            k.dma("sp", cT[:, b_, :], c_d[b_:b_ + 1, :].rearrange("o (kc p) -> p (o kc)", p=128), allow_slow_non_contiguous=True)
        scT = sm2[:, 0:8 * nseq].rearrange("p (b kc) -> p b kc", b=nseq)
        k.act(scT, cT, AF.Silu)
        adat = Fs[0]
        ada_sb = rep
        ada_flat = ada_sb[0:nseq].rearrange("p a d -> p (a d)")
        nvec = Fs[1]
        w_ada_v = w_ada_d.rearrange("(kc p) n -> p kc n", p=128)
        for g in range(12):
            st = stage[g % 2]
            k.dma("sp", st[:], w_ada_v[:, :, g * 512:(g + 1) * 512])
            for kc in range(8):
                k.mm(PB[g % 2][0:nseq, :], scT[:, :, kc], st[:, kc, :], start=(kc == 0), stop=(kc == 7))
            k.cp(ada_flat[:, g * 512:(g + 1) * 512], PB[g % 2][0:nseq, :])
        bad = Fs[2]
        for g in range(6):
            k.dma("sp", Fs[2 + (g % 2)][0:nseq, :], b_ada_d[:, g * D:(g + 1) * D].partition_broadcast(nseq))
            k.tt(ada_sb[0:nseq, g, :], ada_sb[0:nseq, g, :], Fs[2 + (g % 2)][0:nseq, :], ALU.add)
        comb = Fs[4]
        res6 = [Fs[4], Fs[5], Hs[0], Hs[1]]
        nv = Fs[1]
        k.dma("sp", nv[0:nseq, :], n1pre_d.partition_broadcast(nseq))
        k.stt(Fs[4][0:nseq, :], ada_sb[0:nseq, 1, :], 1.0, nv[0:nseq, :], ALU.add, ALU.mult)
        k.dma("sp", ada_d[:, 0 * D:1 * D], Fs[4][0:nseq, :])
        k.dma("sp", ada_d[:, 1 * D:2 * D], ada_sb[0:nseq, 0, :])
        k.dma("sp", Fs[2][0:nseq, :], n1post_d.partition_broadcast(nseq))
        k.tt(Fs[5][0:nseq, :], ada_sb[0:nseq, 2, :], Fs[2][0:nseq, :], ALU.mult)
        k.dma("sp", ada_d[:, 2 * D:3 * D], Fs[5][0:nseq, :])
        k.dma("sp", Fs[3][0:nseq, :], n2pre_d.partition_broadcast(nseq))
        k.stt(Fs[0][0:nseq, :], ada_sb[0:nseq, 4, :], 1.0, Fs[3][0:nseq, :], ALU.add, ALU.mult)
        k.dma("sp", ada_d[:, 3 * D:4 * D], Fs[0][0:nseq, :])
        k.dma("sp", ada_d[:, 4 * D:5 * D], ada_sb[0:nseq, 3, :])
        k.dma("sp", nv[0:nseq, :], n2post_d.partition_broadcast(nseq))
        k.tt(Fs[4][0:nseq, :], ada_sb[0:nseq, 5, :], nv[0:nseq, :], ALU.mult)
        k.dma("sp", ada_d[:, 5 * D:6 * D], Fs[4][0:nseq, :])

        mhw = sm[:, 16:24]
        k.dma("sp", mhw, mhw_d.rearrange("o (kc p) -> p (o kc)", p=128), allow_slow_non_contiguous=True)
        for bi, (nm, c0, ncol) in enumerate(WBLOCKS):
            st = stage[bi % 2]
            wb = wbuf[bi % 2]
            src = wsrc[nm].rearrange("(kc p) n -> p kc n", p=128)
            k.dma("sp", st[:, :, 0:ncol], src[:, :, c0:c0 + ncol])
            if nm == "w_branch_m":
                for kc in range(8):
                    k.ts(wb[:, kc, 0:ncol], st[:, kc, 0:ncol], mhw[:, kc:kc + 1], None, ALU.mult)
            else:
                k.cp(wb[:, 0:4, 0:ncol], st[:, 0:4, 0:ncol], e="dve")
                k.cp(wb[:, 4:8, 0:ncol], st[:, 4:8, 0:ncol], e="act")
            k.dma("pool", wsc_d[bi].rearrange("p (kc n) -> p kc n", n=512)[:, :, 0:ncol], wb[:, :, 0:ncol])
        for (src_d, dst_d) in ((pu_d, ub_d), (pv_d, vb_d)):
            sv_ = src_d.rearrange("(c p r) d -> c p (r d)", p=128, r=4)
            dv_ = dst_d.rearrange("(c p r) d -> c p (r d)", p=128, r=4)
            for c_ in range(32):
                stf = stage[c_ % 2][:].rearrange("p a n -> p (a n)")
                wbf = wbuf[c_ % 2][:].rearrange("p a n -> p (a n)")
                k.dma("sp", stf, sv_[c_])
                k.cp(wbf[:, 0:2048], stf[:, 0:2048], e="dve")
                k.cp(wbf[:, 2048:4096], stf[:, 2048:4096], e="act")
                k.dma("pool", dv_[c_], wbf)
        for hc in range(16):
            st = stage[hc % 2]
            k.dma("sp", st[:, 0, 0:128], sk_d[hc])
            k.tp(PB[hc % 2][:, 0:128], st[:, 0, 0:128], identf)
            k.cp(skT[:, hc, :], PB[hc % 2][:, 0:128])
        k.barrier()
        es2.close()
        gbufs = [k.sb("gbuf%d" % i, [128, D], BF16) for i in range(NGBUF)]
        resident = {}
        for bi in RESIDENT:
            rt = k.sb("wres%d" % bi, [128, 8, 512], BF16)
            k.dma("sp", rt[:], wsc_d[bi].rearrange("p (kc n) -> p kc n", n=512))
            resident[bi] = rt
        X1p = [k.sb("X1p%d" % i, [128, D], F32) for i in range(2)]
        H2p = [k.sb("H2p%d" % i, [128, D], F32) for i in range(2)]
        eidxp = [k.sb("eidxp%d" % i, [128, 128], U32) for i in range(2)]
        gatep = [k.sb("gatep%d" % i, [128, 128], F32) for i in range(2)]
        JKb = k.sb("JKb", [128, D], BF16)
        TMPt = k.sb("TMPt", [128, D], F32)
        Dg = [k.sb("Dg%d" % i, [128, 128], BF16) for i in range(4)]
        smb = k.sb("smb", [128, 8], F32)

        wstate = {"n": 0}

        def load_block(bi):
            if bi in resident:
                return resident[bi]
            wb = wbuf[wstate["n"] % 2]
            wstate["n"] += 1
            ncol = WBLOCKS[bi][2]
            k.dma("sp", wb[:, :, 0:ncol], wsc_d[bi].rearrange("p (kc n) -> p kc n", n=512)[:, :, 0:ncol])
            return wb

        def dump(ap, col0, ncols, parts=128):
            if dbg_d is not None:
                k.dma("sp", dbg_d[0:parts, col0:col0 + ncols], ap)

        for s in range(nseq):
            k.dma("sp", rep[:].rearrange("p a d -> p (a d)"), ada_d[s:s + 1, :].partition_broadcast(128))
            W1, SH1, G1N, W2, SH2, G2N = [rep[:, i, :] for i in range(6)]
            k.memset(UA[:, :, 0:2], 0.0)
            k.memset(UQ[:, :, 0:3], 0.0)
            k.memset(Cf[:], 0.0)
            k.memset(Cb[:], 0.0)
            k.memset(nf[:], 0.0)
            k.memset(nb[:], 0.0)
            def front(s, ti, par):
                r0 = s * SEQ + ti * 128
                eidx, gate = eidxp[par], gatep[par]
                X = X1p[par]
                k.dma("sp", X[:], x_d[r0:r0 + 128, :])
                ssq = sm[:, 32:33]
                k.act(Fs[0][:], X[:], AF.Square, accum_out=ssq)
                rstd = sm[:, 33:34]
                k.ts(sm[:, 34:35], ssq, 1.0 / D, EPS, ALU.mult, ALU.add)
                k.act(sm[:, 35:36], sm[:, 34:35], AF.Sqrt)
                k.recip(rstd, sm[:, 35:36])
                k.stt(Fs[0][:], X[:], rstd, W1, ALU.mult, ALU.mult)
                hb = Hs[0]
                k.tt(hb[:], Fs[0][:], SH1, ALU.add)
                for kc in range(8):
                    k.tp(PT[:, kc * 128:(kc + 1) * 128], hb[:, kc * 128:(kc + 1) * 128], identb)
                hT = Hs[1]
                k.cp(hT[:], PT[:])
                hTv = hT[:].rearrange("p (kc t) -> p kc t", t=128)
                if stop_after == "hT":
                    k.cp(Fs[1][:], hT[:])
                    dump(Fs[1][:], 0, 1024)
                    return
                yield
                CG, BG, XIN = Fs[1], Fs[2], Fs[3]
                SGA, SGM = Hs[2], Hs[3]
                Vb = Hs[4]
                SO = Fs[4]
                IFt = sm[:, 40:56]

                def fm_block(bi, dst_fn, pbi):
                    wb = load_block(bi)
                    pb = PB[pbi]
                    for j in range(4):
                        for kc in range(8):
                            k.mm(pb[:, j * 128:(j + 1) * 128], wb[:, kc, j * 128:(j + 1) * 128], hTv[:, kc, :],
                                 start=(kc == 0), stop=(kc == 7))
                    dst_fn(pb)

                def evac_plain(dst3, e):
                    def f(pb):
                        k.cp(dst3, pb[:].rearrange("p (j t) -> p j t", t=128), e=e)
                    return f

                def evac_sig(dst3):
                    def f(pb):
                        k.act(dst3, pb[:].rearrange("p (j t) -> p j t", t=128), AF.Sigmoid)
                    return f

                def v3(t, lo):
                    return t[:].rearrange("p (j t) -> p j t", t=128)[:, lo:lo + 4, :]

                fm_block(0, evac_plain(v3(CG, 0), "act"), 0)
                fm_block(1, evac_plain(v3(CG, 4), "dve"), 1)
                yield
                fm_block(2, evac_plain(v3(BG, 0), "act"), 0)
                fm_block(3, evac_plain(v3(BG, 4), "dve"), 1)
                yield
                fm_block(4, evac_plain(v3(XIN, 0), "act"), 0)
                fm_block(5, evac_plain(v3(XIN, 4), "dve"), 1)
                yield
                fm_block(6, evac_plain(UQ[:, 0:4, 3:131], "act"), 0)
                fm_block(7, evac_plain(UQ[:, 4:8, 3:131], "dve"), 1)
                yield
                for gi, bi in enumerate((8, 9, 10, 11)):
                    wb = load_block(bi)
                    pb = PB[2 + gi % 2]
                    for kc in range(8):
                        k.mm(pb[:], hTv[:, kc, :], wb[:, kc, :], start=(kc == 0), stop=(kc == 7))
                    if gi < 2:
                        k.cp(Vb[:, gi * 512:(gi + 1) * 512], pb[:], e="dve")
                    else:
                        k.act(SO[:, (gi - 2) * 512:(gi - 1) * 512], pb[:], AF.Sigmoid)
                wb = load_block(12)
                for kc in range(8):
                    k.mm(PB[4][:, 0:16], hTv[:, kc, :], wb[:, kc, 0:16], start=(kc == 0), stop=(kc == 7))
                k.tt(IFt, PB[4][:, 0:16], bif[:], ALU.add)
                yield
                fm_block(13, evac_sig(v3(SGA, 0)), 0)
                fm_block(14, evac_sig(v3(SGA, 4)), 1)
                yield
                fm_block(15, evac_sig(v3(SGM, 0)), 0)
                fm_block(16, evac_sig(v3(SGM, 4)), 1)
                if stop_after == "proj":
                    dump(CG[:], 0, 1024)
                    dump(UQ[:, :, 3:131], 1024, 1024)
                    k.cp(Fs[0][:], Vb[:])
                    dump(Fs[0][:], 2048, 1024)
                    dump(SO[:], 3072, 1024)
                    dump(IFt, 4096, 16)
                    k.cp(Fs[5][:], SGM[:])
                    dump(Fs[5][:], 4112, 1024)
                    return
                yield
                CG3 = CG[:].rearrange("p (j t) -> p j t", t=128)
                BG3 = BG[:].rearrange("p (j t) -> p j t", t=128)
                XIN3 = XIN[:].rearrange("p (j t) -> p j t", t=128)
                k.tt(UA[:, :, 2:130], CG3, XIN3, ALU.mult)
                T0 = Fs[0][:].rearrange("p (j t) -> p j t", t=128)
                T1 = CG3
                k.tt(T0, UA[:, :, 0:128], cwa[:, 0, :].unsqueeze(2).to_broadcast([128, 8, 128]), ALU.mult)
                k.tt(T1, UA[:, :, 1:129], cwa[:, 1, :].unsqueeze(2).to_broadcast([128, 8, 128]), ALU.mult)
                k.tt(T0, T0, T1, ALU.add)
                k.tt(T1, UA[:, :, 2:130], cwa[:, 2, :].unsqueeze(2).to_broadcast([128, 8, 128]), ALU.mult)
                k.tt(T0, T0, T1, ALU.add)
                yaT = Hs[5]
                k.tt(yaT[:].rearrange("p (j t) -> p j t", t=128), T0, BG3, ALU.mult)
                k.cp(UA[:, :, 0:2], UA[:, :, 128:130])
                yield
                T1 = XIN3
                k.tt(T0, UQ[:, :, 0:128], cwq[:, 0, :].unsqueeze(2).to_broadcast([128, 8, 128]), ALU.mult)
                for j in range(1, 4):
                    k.tt(T1, UQ[:, :, j:j + 128], cwq[:, j, :].unsqueeze(2).to_broadcast([128, 8, 128]), ALU.mult)
                    k.tt(T0, T0, T1, ALU.add)
                qkT = Hs[6]
                qk3 = qkT[:].rearrange("p (j t) -> p j t", t=128)
                k.act(qk3, T0, AF.Silu)
                k.cp(UQ[:, :, 0:3], UQ[:, :, 128:131])
                yield
                IG = IFt[:, 0:8]
                FG = IFt[:, 8:16]
                lf = sm[:, 56:64]
                k.act(lf, FG, AF.Exp, scale=-1.0)
                k.act(lf, lf, AF.Ln, bias=1.0)
                k.ts(lf, lf, -1.0, None, ALU.mult)
                k.mm(PB[4][:, 16:24], tri2, lf)
                k.mm(PB[4][:, 24:32], ones128, lf)
                Bt = sm[:, 64:72]
                BL = sm[:, 72:80]
                k.cp(Bt, PB[4][:, 16:24])
                k.cp(BL, PB[4][:, 24:32])
                u = sm[:, 88:96]
                eB = sm[:, 96:104]
                wk = sm[:, 104:112]
                eBL = sm[:, 112:116]
                k.tt(u, IG, Bt, ALU.subtract)
                k.tt(wk, u, BL, ALU.add)
                k.act(u, u, AF.Exp)
                k.act(wk, wk, AF.Exp)
                k.act(eB, Bt, AF.Exp)
                BLv = BL.rearrange("p (j two) -> p j two", two=2)
                k.act(eBL[0:64, :], BLv[0:64, :, 0], AF.Exp)
                k.act(eBL[64:128, :], BLv[64:128, :, 1], AF.Exp)
                for j in range(4):
                    k.tp(PT[:, j * 128:(j + 1) * 128], qk3[:, 4 + j, :], identb)
                kw = Hs[7]
                kw3 = kw[:, 0:512].rearrange("p (h d) -> p h d", d=64)
                k.tt(kw3, PT[:, 0:512].rearrange("p (h d) -> p h d", d=64), wk.unsqueeze(2).to_broadcast([128, 8, 64]), ALU.mult)
                yield
                Qm = [Fs[0][:, 0:256].bitcast(BF16).rearrange("p (j t) -> p j t", t=128),
                      Fs[0][:, 256:512].bitcast(BF16).rearrange("p (j t) -> p j t", t=128)]
                for e_ in range(2):
                    k.ts(Qm[e_], qk3[:, 0:4, :], rowmask[:, e_:e_ + 1], None, ALU.mult)
                SB = [PB[2], PB[3]]
                for h in range(8):
                    k.mm(SB[h // 4][:, (h % 4) * 128:(h % 4 + 1) * 128], qk3[:, 4 + h // 2, :], Qm[h % 2][:, h // 2, :])
                SwT = Hs[1]
                Sw3 = SwT[:].rearrange("p (h t) -> p h t", t=128)
                S3 = Fs[5][:].rearrange("p (h t) -> p h t", t=128)
                for g2 in range(2):
                    k.tt(S3[:, g2 * 4:(g2 + 1) * 4, :], SB[g2][:].rearrange("p (h t) -> p h t", t=128),
                         u[:, g2 * 4:(g2 + 1) * 4].unsqueeze(2).to_broadcast([128, 4, 128]), ALU.mult)
                k.tt(Sw3, S3, tri2.unsqueeze(1).to_broadcast([128, 8, 128]), ALU.mult)
                yield
                Vb3 = Vb[:].rearrange("p (h d) -> p h d", d=128)
                NUM = [PB[0], PB[1]]
                DEN = PB[4][:, 40:48]
                for h in range(8):
                    q_h = Qm[h % 2][:, h // 2, :]
                    numo = NUM[h // 4][:, (h % 4) * 128:(h % 4 + 1) * 128]
                    k.mm(numo, Sw3[:, h, :], Vb3[:, h, :], start=True, stop=False)
                    k.mm(numo, q_h, Cb[:, h // 2, :], start=False, stop=True)
                    k.mm(DEN[:, h:h + 1], Sw3[:, h, :], onesb_t[:], start=True, stop=False)
                    k.mm(DEN[:, h:h + 1], q_h, nb[:, h // 2:h // 2 + 1], start=False, stop=True)
                yield
                kwp = kw[:, 0:512].rearrange("p (j d) -> p j d", d=128)
                for h in range(8):
                    k.mm(PB[2 + h % 2][:, (h // 2) * 128:(h // 2 + 1) * 128], kwp[:, h // 2, :], Vb3[:, h, :])
                for j in range(4):
                    k.mm(PB[4][:, 48 + j:49 + j], kwp[:, j, :], onesb_t[:])
                k.tt(Cf[:], Cf[:], eBL.unsqueeze(2).to_broadcast([128, 4, 128]), ALU.mult)
                k.tt(Cf[0:64], Cf[0:64], PB[2][0:64, :].rearrange("p (j d) -> p j d", d=128), ALU.add)
                k.tt(Cf[64:128], Cf[64:128], PB[3][64:128, :].rearrange("p (j d) -> p j d", d=128), ALU.add)
                k.cp(Cb[:], Cf[:], e="act")
                k.tt(nf[:], nf[:], eBL, ALU.mult)
                k.tt(nf[:], nf[:], PB[4][:, 48:52], ALU.add)
                k.cp(nb[:], nf[:], e="act")
                yield
                dn = sm[:, 120:128]
                k.stt(dn, DEN, 0.125, eB, ALU.mult, ALU.mult)
                k.act(dn, dn, AF.Abs)
                k.ts(dn, dn, 1.0, None, ALU.max)
                k.recip(dn, dn)
                k.stt(dn, eB, 0.125, dn, ALU.mult, ALU.mult)
                HN = Fs[5]
                HN3 = HN[:].rearrange("p (h d) -> p h d", d=128)
                for g2 in range(2):
                    k.tt(HN3[:, g2 * 4:(g2 + 1) * 4, :], NUM[g2][:].rearrange("p (h d) -> p h d", d=128),
                         dn[:, g2 * 4:(g2 + 1) * 4].unsqueeze(2).to_broadcast([128, 4, 128]), ALU.mult)
                if stop_after == "mlstm":
                    dump(HN[:], 0, 1024)
                    k.cp(Fs[0][:], yaT[:])
                    dump(Fs[0][:], 1024, 1024)
                    return
                yield
                k.tt(Fs[0][:], HN[:], HN[:], ALU.mult)
                hss = sm[:, 128:136]
                k.red(hss, Fs[0][:].rearrange("p (h d) -> p h d", d=128), ALU.add)
                k.ts(hss, hss, 1.0 / 128, EPS, ALU.mult, ALU.add)
                k.act(hss, hss, AF.Sqrt)
                k.recip(hss, hss)
                k.tt(HN3, HN3, hss.unsqueeze(2).to_broadcast([128, 8, 128]), ALU.mult)
                ymb = Hs[0]
                k.tt(ymb[:], HN[:], SO[:], ALU.mult)
                for kc in range(8):
                    k.tp(PT[:, kc * 128:(kc + 1) * 128], ymb[:, kc * 128:(kc + 1) * 128], identb)
                ymT = Hs[4]
                k.cp(ymT[:], PT[:])
                ymT3 = ymT[:].rearrange("p (kc t) -> p kc t", t=128)
                yaT3 = yaT[:].rearrange("p (kc t) -> p kc t", t=128)
                yield
                MIX = Fs[0]
                MIX3 = MIX[:].rearrange("p (j t) -> p j t", t=128)

                def branch(bname, src3, sg, first):
                    for half in range(2):
                        wb = load_block(BLK[(bname, half * 512)])
                        pb = PB[2 + half]
                        for j in range(4):
                            for kc in range(8):
                                k.mm(pb[:, j * 128:(j + 1) * 128], wb[:, kc, j * 128:(j + 1) * 128], src3[:, kc, :],
                                     start=(kc == 0), stop=(kc == 7))
                        sg3 = sg[:].rearrange("p (j t) -> p j t", t=128)[:, half * 4:(half + 1) * 4, :]
                        dst = MIX3[:, half * 4:(half + 1) * 4, :]
                        p3 = pb[:].rearrange("p (j t) -> p j t", t=128)
                        if first:
                            k.tt(dst, p3, sg3, ALU.mult)
                        else:
                            t3 = Fs[1][:].rearrange("p (j t) -> p j t", t=128)[:, half * 4:(half + 1) * 4, :]
                            k.tt(t3, p3, sg3, ALU.mult)
                            k.tt(dst, dst, t3, ALU.add)

                branch("w_branch_a", yaT3, SGA, True)
                yield
                branch("w_branch_m", ymT3, SGM, False)
                yield
                mixT = Hs[2]
                k.cp(mixT[:], MIX[:], e="act")
                mixT3 = mixT[:].rearrange("p (kc t) -> p kc t", t=128)
                for half in range(2):
                    wb = load_block(BLK[("w_out", half * 512)])
                    for kc in range(8):
                        k.mm(PB[half][:], mixT3[:, kc, :], wb[:, kc, :], start=(kc == 0), stop=(kc == 7))
                yield
                Y = Fs[1]
                k.cp(Y[:, 0:512], PB[0][:], e="act")
                k.cp(Y[:, 512:1024], PB[1][:], e="dve")
                ssq2 = sm[:, 136:137]
                k.act(Fs[0][:], Y[:], AF.Square, accum_out=ssq2)
                k.ts(ssq2, ssq2, 1.0 / D, EPS, ALU.mult, ALU.add)
                k.act(ssq2, ssq2, AF.Sqrt)
                k.recip(ssq2, ssq2)
                k.stt(Fs[0][:], Y[:], ssq2, G1N, ALU.mult, ALU.mult)
                X1 = X1p[par]
                k.tt(X1[:], Fs[0][:], X[:], ALU.add)
                if stop_after == "sub1":
                    k.dma("sp", out_d[r0:r0 + 128, :], X1[:])
                    return
                yield
                ssq3 = sm[:, 137:138]
                k.act(Fs[0][:], X1[:], AF.Square, accum_out=ssq3)
                k.ts(ssq3, ssq3, 1.0 / D, EPS, ALU.mult, ALU.add)
                k.act(ssq3, ssq3, AF.Sqrt)
                k.recip(ssq3, ssq3)
                k.stt(Fs[0][:], X1[:], ssq3, W2, ALU.mult, ALU.mult)
                H2 = H2p[par]
                k.tt(H2[:], Fs[0][:], SH2, ALU.add)
                h2b = Hs[0]
                k.cp(h2b[:], H2[:], e="act")
                for kc in range(8):
                    k.tp(PT[:, kc * 128:(kc + 1) * 128], h2b[:, kc * 128:(kc + 1) * 128], identb)
                h2T = Hs[1]
                k.cp(h2T[:], PT[:])
                h2T3 = h2T[:].rearrange("p (kc t) -> p kc t", t=128)
                yield
                for g4 in range(4):
                    wb = load_block(BLK[("peer_wq", g4 * 512)])
                    pb = PB[2 + g4 % 2]
                    for j in range(4):
                        for kc in range(8):
                            k.mm(pb[:, j * 128:(j + 1) * 128], wb[:, kc, j * 128:(j + 1) * 128], h2T3[:, kc, :],
                                 start=(kc == 0), stop=(kc == 7))
                    k.cp(qT[:, g4 * 4:(g4 + 1) * 4, :], pb[:].rearrange("p (j t) -> p j t", t=128), e=("act" if g4 % 2 else "dve"))
                yield
                scv = [Fs[1][:].rearrange("p (j n) -> p j n", n=128), Fs[2][:].rearrange("p (j n) -> p j n", n=128)]
                tk0v = Fs[3][:].rearrange("p (h a b) -> p h a b", a=16, b=16)
                for g4 in range(4):
                    pb = PB[g4 % 2]
                    for j in range(4):
                        hc = g4 * 4 + j
                        k.mm(pb[:, j * 128:(j + 1) * 128], qT[:, hc, :], skT[:, hc, :])
                    k.cp(scv[g4 // 2][:, (g4 % 2) * 4:(g4 % 2) * 4 + 4, :], pb[:].rearrange("p (j n) -> p j n", n=128), e=("act" if g4 % 2 else "dve"))
                yield
                dv = k.eng["dve"]
                for h in range(8):
                    for half, (vv, ii) in enumerate(((v1, i1), (v2, i2))):
                        s_ = scv[(2 * h + half) // 8][:, (2 * h + half) % 8, :]
                        k.op("dve", lambda g, vv=vv, h=h, s_=s_: g.max(vv[:, h, 0:8], s_), [s_], [vv[:]])
                        k.op("dve", lambda g, vv=vv, ii=ii, h=h, s_=s_: g.max_index(ii[:, h, 0:8], vv[:, h, 0:8], s_), [s_, vv[:]], [ii[:]])
                        k.op("dve", lambda g, vv=vv, h=h, s_=s_: g.match_replace(scb[:], vv[:, h, 0:8], s_, NEG), [s_, vv[:]], [scb[:]])
                        k.op("dve", lambda g, vv=vv, h=h: g.max(vv[:, h, 8:16], scb[:]), [scb[:]], [vv[:]])
                        k.op("dve", lambda g, vv=vv, ii=ii, h=h: g.max_index(ii[:, h, 8:16], vv[:, h, 8:16], scb[:]), [scb[:], vv[:]], [ii[:]])
                    yield
                    c3 = cand[:].rearrange("p (a b) -> p a b", b=16)
                    k.tt(c3, v1[:, h, :].unsqueeze(2).to_broadcast([128, 16, 16]),
                         v2[:, h, :].unsqueeze(1).to_broadcast([128, 16, 16]), ALU.add)
                    k.op("dve", lambda g, h=h: g.max(cv[:, h, 0:8], cand[:]), [cand[:]], [cv[:]])
                    k.op("dve", lambda g, h=h: g.max_index(ci[:, h, 0:8], cv[:, h, 0:8], cand[:]), [cand[:], cv[:]], [ci[:]])
                    k.op("dve", lambda g, h=h: g.match_replace(candb[:], cv[:, h, 0:8], cand[:], NEG), [cand[:], cv[:]], [candb[:]])
                    k.op("dve", lambda g, h=h: g.max(cv[:, h, 8:16], candb[:]), [candb[:]], [cv[:]])
                    k.op("dve", lambda g, h=h: g.max_index(ci[:, h, 8:16], cv[:, h, 8:16], candb[:]), [candb[:], cv[:]], [ci[:]])
                yield
                ua, ub_ = tku[0], tku[1]
                k.ts(ua[:], ci[:], 4, None, ALU.logical_shift_right)
                k.ts(ub_[:], ci[:], 15, None, ALU.bitwise_and)
                af, bf_, i1f, i2f, isel, jsel = tkf
                k.cp(af[:], ua[:])
                k.cp(bf_[:], ub_[:])
                k.cp(i1f[:], i1[:])
                k.cp(i2f[:], i2[:])
                io4 = iota16.unsqueeze(1).unsqueeze(1).to_broadcast([128, 4, 16, 16])
                for (xf, tab, dst) in ((af, i1f, isel), (bf_, i2f, jsel)):
                    for hh in range(2):
                        hs4 = slice(hh * 4, hh * 4 + 4)
                        k.tt(tk0v, xf[:, hs4, :].unsqueeze(3).to_broadcast([128, 4, 16, 16]), io4, ALU.is_equal)
                        k.tt(tk0v, tk0v, tab[:, hs4, :].unsqueeze(2).to_broadcast([128, 4, 16, 16]), ALU.mult)
                        k.red(dst[:, hs4, :], tk0v, ALU.add)
                ef = af
                k.stt(ef[:], isel[:], 128.0, jsel[:], ALU.mult, ALU.add)
                k.cp(eidx[:].rearrange("p (h k) -> p h k", k=16), ef[:])
                yield
                gx = bf_
                k.tt(gx[:], cv[:], cv[:, :, 0:1].to_broadcast([128, 8, 16]), ALU.subtract)
                k.act(gx[:], gx[:], AF.Exp)
                gs = sm[:, 144:152]
                k.red(gs, gx[:], ALU.add)
                k.recip(gs, gs)
                k.tt(gate[:].rearrange("p (h k) -> p h k", k=16), gx[:], gs.unsqueeze(2).to_broadcast([128, 8, 16]), ALU.mult)
                if stop_after == "topk":
                    k.cp(Fs[0][:, 0:128], eidx[:])
                    dump(Fs[0][:, 0:128], 0, 128)
                    dump(gate[:], 128, 128)
                    dump(H2[:], 256, 1024)
                    return
                yield
            def back(s, ti, par):
                r0 = s * SEQ + ti * 128
                X1, H2, eidx, gate = X1p[par], H2p[par], eidxp[par], gatep[par]
                NG = len(gbufs)
                for sl in range(128):
                    gb = gbufs[sl % NG][:]
                    k.dma("pool", gb, ub_d, fn=lambda g, gb=gb, sl=sl, eidx=eidx: g.indirect_dma_start(
                        out=gb, out_offset=None, in_=ub_d[:, :],
                        in_offset=bass.IndirectOffsetOnAxis(ap=eidx[:, sl:sl + 1], axis=0)), extra_reads=[eidx[:]])
                    k.stt(JKb[:], gb, 1.0, H2[:], ALU.mult, ALU.mult, accum_out=zz[:, sl:sl + 1])
                    if sl % 4 == 3:
                        yield
                k.tt(aa[:], zz[:], zz[:], ALU.mult)
                k.ts(aa[:], aa[:], 0.044715, 1.0, ALU.mult, ALU.add)
                k.tt(aa[:], aa[:], zz[:], ALU.mult)
                k.act(aa[:], aa[:], AF.Sigmoid, scale=1.5957691216057308)
                k.tt(aa[:], aa[:], zz[:], ALU.mult)
                k.tt(aa[:], aa[:], gate[:], ALU.mult)
                yield
                for sl in range(128):
                    gb = gbufs[sl % NG][:]
                    k.dma("pool", gb, vb_d, fn=lambda g, gb=gb, sl=sl, eidx=eidx: g.indirect_dma_start(
                        out=gb, out_offset=None, in_=vb_d[:, :],
                        in_offset=bass.IndirectOffsetOnAxis(ap=eidx[:, sl:sl + 1], axis=0)), extra_reads=[eidx[:]])
                    dg = Dg[sl % 4]
                    k.act(dg[:], identb, AF.Copy, scale=aa[:, sl:sl + 1])
                    k.mm(PB[5][:], dg[:], gb[:, 0:512], start=(sl == 0), stop=(sl == 127))
                    k.mm(PB[6][:], dg[:], gb[:, 512:1024], start=(sl == 0), stop=(sl == 127))
                    if sl % 4 == 3:
                        yield
                ssq4 = smb[:, 0:1]
                k.act(TMPt[:, 0:512], PB[5][:], AF.Square, accum_out=ssq4)
                k.act(TMPt[:, 512:1024], PB[6][:], AF.Square, accum_out=smb[:, 1:2])
                k.tt(ssq4, ssq4, smb[:, 1:2], ALU.add)
                k.ts(ssq4, ssq4, 1.0 / D, EPS, ALU.mult, ALU.add)
                k.act(ssq4, ssq4, AF.Sqrt)
                k.recip(ssq4, ssq4)
                k.stt(TMPt[:, 0:512], PB[5][:], ssq4, rep[:, 5, 0:512], ALU.mult, ALU.mult)
                k.stt(TMPt[:, 512:1024], PB[6][:], ssq4, rep[:, 5, 512:1024], ALU.mult, ALU.mult)
                k.tt(TMPt[:], TMPt[:], X1[:], ALU.add)
                k.dma("sp", out_d[r0:r0 + 128, :], TMPt[:])
                yield

            prev = None
            for ti in range(ntile):
                fa = k.record(front(s, ti, ti % 2))
                fb = k.record(back(*prev)) if (prev is not None and stop_after is None) else []
                if fb:
                    k.emit_merged(fa, fb)
                else:
                    k.emit_merged(fa, [])
                prev = (s, ti, ti % 2)
            if stop_after is None:
                k.emit_merged(k.record(back(*prev)), [])
        k.barrier()
    return nc


def make_consts():
    c = np.zeros((128, 1024), np.float32)
    c[:, 0:128] = np.eye(128, dtype=np.float32)
    s = np.arange(128)[:, None]
    t = np.arange(128)[None, :]
    c[:, 128:256] = (s <= t).astype(np.float32)
    c[:, 256:384] = 1.0
    c[:, 384] = (np.arange(128) < 64).astype(np.float32)
    c[:, 385] = (np.arange(128) >= 64).astype(np.float32)
    c[:, 576:592] = np.arange(16, dtype=np.float32)[None, :]
    c[:, 592] = 1.0
    return c


_PROG = {}


def kernel(x, c, w_ada, b_ada, norm1_pre, norm1_post, w_in, conv_a_w, conv_qk_w, b_igate, b_fgate, mh_norm_w,
           w_branch_a, w_branch_m, w_out, norm2_pre, norm2_post, peer_wq, peer_subkeys, peer_u, peer_v):
    f = lambda a: np.ascontiguousarray(np.asarray(a, dtype=np.float32))
    if "nc" not in _PROG:
        _PROG["nc"] = build_program()
    nc = _PROG["nc"]
    shared = {
        "w_ada": f(w_ada[0]), "b_ada": f(b_ada), "norm1_pre": f(norm1_pre), "norm1_post": f(norm1_post),
        "w_in": f(w_in[0]), "conv_a_w": f(conv_a_w[0]), "conv_qk_w": f(conv_qk_w[0]), "b_igate": f(b_igate),
        "b_fgate": f(b_fgate), "mh_norm_w": f(mh_norm_w), "w_branch_a": f(w_branch_a[0]),
        "w_branch_m": f(w_branch_m[0]), "w_out": f(w_out[0]), "peer_wq": f(peer_wq[0]),
        "norm2_pre": f(norm2_pre), "norm2_post": f(norm2_post),
        "peer_subkeys": f(peer_subkeys[0]).reshape(16, 128, 128), "peer_u": f(peer_u[0]), "peer_v": f(peer_v[0]),
        "cst": make_consts(),
    }
    xs = f(x)
    cs = f(c)
    in_maps = []
    for i in range(NCORES):
        m = dict(shared)
        m["x"] = xs[2 * i:2 * i + 2].reshape(2 * SEQ, D)
        m["c"] = cs[2 * i:2 * i + 2]
        in_maps.append(m)
    res = run_bass_kernel_spmd(nc, in_maps, core_ids=list(range(NCORES)))
    out = np.concatenate([r["out"].reshape(2, SEQ, D) for r in res.results], axis=0)
    return out.astype(np.float32)
```

```python
from contextlib import ExitStack
import numpy as np
import concourse.bass as bass
import concourse.mybir as mybir
from concourse.bass_utils import run_bass_kernel_spmd

F32 = mybir.dt.float32
BF16 = mybir.dt.bfloat16
U32 = mybir.dt.uint32
I32 = mybir.dt.int32
AF = mybir.ActivationFunctionType
ALU = mybir.AluOpType
AX = mybir.AxisListType

D = 1024
NCORES = 8
SEQ = 4096
NSEQ_CORE = 2
IN_W = 8208
EPS = 1e-6
NEG = -1.0e30

O_CG, O_BG, O_XIN, O_Q, O_K, O_V, O_O, O_I, O_F, O_GA, O_GM = 0, 1024, 2048, 3072, 3584, 4096, 5120, 6144, 6152, 6160, 7184

WBLOCKS = []
for c0 in range(0, 6144, 512):
    WBLOCKS.append(("w_in", c0, 512))
WBLOCKS.append(("w_in", 6144, 16))
for c0 in range(6160, 8208, 512):
    WBLOCKS.append(("w_in", c0, 512))
for nm in ("w_branch_a", "w_branch_m", "w_out"):
    for c0 in (0, 512):
        WBLOCKS.append((nm, c0, 512))
for c0 in range(0, 2048, 512):
    WBLOCKS.append(("peer_wq", c0, 512))
NBLK = len(WBLOCKS)
NGBUF = 16
RESIDENT = tuple(range(0, 4))
BLK = {(nm, c0): i for i, (nm, c0, n) in enumerate(WBLOCKS)}


class K:
    def __init__(self, nc, es):
        self.nc = nc
        self.es = es
        self.eng = {"pe": nc.tensor, "act": nc.scalar, "dve": nc.vector, "pool": nc.gpsimd, "sp": nc.sync}
        self.esem = {e: es.enter_context(nc.semaphore("sem_" + e)) for e in self.eng}
        self.ecnt = {e: 0 for e in self.eng}
        self.semobj = {("e", e): self.esem[e] for e in self.eng}
        self.waited = {e: {} for e in self.eng}
        self.tr = {}
        self.dsem = {}
        self.dcnt = {}
        self.same_engine_sync = {"pe": False, "act": True, "dve": True, "pool": True, "sp": True}
        self.rec = None

    def _reg(self, h, name):
        self.tr[name] = {"w": {}, "r": {}}
        return h

    def sb(self, name, shape, dt, es=None):
        return self._reg((es or self.es).enter_context(self.nc.sbuf_tensor(name, shape, dt)), name)

    def ps(self, name, shape, dt):
        return self._reg(self.es.enter_context(self.nc.psum_tensor(name, shape, dt)), name)

    def dram(self, name, shape, dt, kind):
        t = self.nc.dram_tensor(name, shape, dt, kind=kind)
        self.tr[name] = {"w": {}, "r": {}}
        return t.ap()

    def _rec(self, ap):
        return self.tr[ap.tensor.name if hasattr(ap, "tensor") else ap.name]

    def _name(self, ap):
        try:
            return ap.tensor.name
        except Exception:
            return ap.name

    def _deps(self, reads, writes):
        ev = {}
        for ap in reads:
            rec = self.tr[self._name(ap)]
            for k, v in rec["w"].items():
                ev[k] = max(ev.get(k, 0), v)
        for ap in writes:
            rec = self.tr[self._name(ap)]
            for k, v in rec["w"].items():
                ev[k] = max(ev.get(k, 0), v)
            for k, v in rec["r"].items():
                ev[k] = max(ev.get(k, 0), v)
        return ev

    def _wait(self, e, ev):
        for k, v in ev.items():
            if k == ("e", e) and not self.same_engine_sync[e]:
                continue
            if self.waited[e].get(k, 0) < v:
                self.eng[e].wait_ge(self.semobj[k], v)
                self.waited[e][k] = v

    def _mark(self, key, val, reads, writes):
        for ap in reads:
            rec = self.tr[self._name(ap)]
            rec["r"][key] = max(rec["r"].get(key, 0), val)
        for ap in writes:
            rec = self.tr[self._name(ap)]
            rec["w"][key] = max(rec["w"].get(key, 0), val)

    def op(self, e, fn, reads, writes):
        if self.rec is not None:
            self.rec.append(("op", e, fn, list(reads), list(writes)))
            return None
        self._wait(e, self._deps(reads, writes))
        ins = fn(self.eng[e])
        self.ecnt[e] += 1
        ins.then_inc(self.esem[e], 1)
        self._mark(("e", e), self.ecnt[e], reads, writes)
        return ins

    def dma(self, e, out, in_, fn=None, extra_reads=(), **kw):
        if self.rec is not None:
            self.rec.append(("dma", e, out, in_, fn, list(extra_reads), kw))
            return None
        rds = [in_] + list(extra_reads)
        self._wait(e, self._deps(rds, [out]))
        dn = self._name(out)
        if dn not in self.dsem:
            self.dsem[dn] = self.es.enter_context(self.nc.semaphore("dsem_" + dn))
            self.dcnt[dn] = 0
            self.semobj[("d", dn)] = self.dsem[dn]
        if fn is None:
            ins = self.eng[e].dma_start(out=out, in_=in_, **kw)
        else:
            ins = fn(self.eng[e])
        self.dcnt[dn] += 16
        ins.then_inc(self.dsem[dn], 16)
        self._mark(("d", dn), self.dcnt[dn], rds, [out])
        return ins

    def record(self, gen):
        self.rec = []
        for _ in gen:
            pass
        r, self.rec = self.rec, None
        return r

    def emit_merged(self, a, b):
        def cost(x):
            if x[0] == "dma":
                return 1.5 if x[1] == "pool" else 3.0
            e = x[1]
            try:
                shp = x[4][0].shape
                n = 1
                for d in shp[1:]:
                    n *= d
            except Exception:
                n = 128
            if e == "pe":
                return 0.04 + n / 2400.0
            if e == "act":
                return 0.15 + n / 1200.0
            return 0.1 + n / 960.0

        def pos(st):
            c = [cost(x) for x in st]
            tot = sum(c) or 1.0
            out, acc = [], 0.0
            for ci in c:
                out.append((acc + 0.5 * ci) / tot)
                acc += ci
            return out
        pa, pb = pos(a), pos(b)
        items = [(0.9 * pa[i], 0, i, x) for i, x in enumerate(a)] + [(pb[j], 1, j, x) for j, x in enumerate(b)]
        items.sort(key=lambda t: (t[0], t[1], t[2]))
        for _, _, _, x in items:
            if x[0] == "op":
                self.op(x[1], x[2], x[3], x[4])
            else:
                self.dma(x[1], x[2], x[3], fn=x[4], extra_reads=x[5], **x[6])

    def barrier(self):
        ev = {}
        for e in self.eng:
            if self.ecnt[e]:
                ev[("e", e)] = self.ecnt[e]
        for dn, c in self.dcnt.items():
            ev[("d", dn)] = c
        for e in self.eng:
            for k, v in ev.items():
                if k == ("e", e):
                    continue
                if self.waited[e].get(k, 0) < v:
                    self.eng[e].wait_ge(self.semobj[k], v)
                    self.waited[e][k] = v

    def mm(self, out, lhsT, rhs, start=True, stop=True):
        return self.op("pe", lambda g: g.matmul(out, lhsT, rhs, start=start, stop=stop), [lhsT, rhs], [out])

    def tp(self, out, in_, ident):
        return self.op("pe", lambda g: g.transpose(out, in_, ident), [in_, ident], [out])

    def act(self, out, in_, func, bias=None, scale=None, accum_out=None, extra_reads=()):
        kw = {}
        rd = [in_] + list(extra_reads)
        if bias is not None:
            kw["bias"] = bias
            if not isinstance(bias, (int, float)):
                rd.append(bias)
        if scale is not None:
            kw["scale"] = scale
            if not isinstance(scale, (int, float)):
                rd.append(scale)
        wr = [out]
        if accum_out is not None:
            kw["accum_out"] = accum_out
            wr.append(accum_out)
        return self.op("act", lambda g: g.activation(out, in_, func, **kw), rd, wr)

    def tt(self, out, in0, in1, op, e="dve"):
        return self.op(e, lambda g: g.tensor_tensor(out, in0, in1, op), [in0, in1], [out])

    def ts(self, out, in0, s1, s2, op0, op1=None, e="dve", accum_out=None):
        rd = [in0] + [s for s in (s1, s2) if s is not None and not isinstance(s, (int, float))]
        wr = [out] + ([accum_out] if accum_out is not None else [])
        kw = {}
        if op1 is not None:
            kw["op1"] = op1
        if accum_out is not None:
            kw["accum_out"] = accum_out
        return self.op(e, lambda g: g.tensor_scalar(out, in0, s1, s2, op0, **kw), rd, wr)

    def stt(self, out, in0, scalar, in1, op0, op1, accum_out=None):
        rd = [in0, in1] + ([scalar] if not isinstance(scalar, (int, float)) else [])
        wr = [out] + ([accum_out] if accum_out is not None else [])
        kw = {"accum_out": accum_out} if accum_out is not None else {}
        return self.op("dve", lambda g: g.scalar_tensor_tensor(out, in0, scalar, in1, op0, op1, **kw), rd, wr)

    def cp(self, out, in_, e="dve"):
        if e == "act":
            return self.op("act", lambda g: g.copy(out, in_), [in_], [out])
        return self.op(e, lambda g: g.tensor_copy(out, in_), [in_], [out])

    def memset(self, ap, val, e="dve"):
        return self.op(e, lambda g: g.memset(ap, val), [], [ap])

    def red(self, out, in_, op, axis=AX.X):
        return self.op("dve", lambda g: g.tensor_reduce(out, in_, axis, op), [in_], [out])

    def recip(self, out, in_):
        return self.op("dve", lambda g: g.reciprocal(out, in_), [in_], [out])


def build_program(nseq=NSEQ_CORE, ntile=SEQ // 128, dbg=None, stop_after=None):
    nc = bass.Bass("TRN2", target_bir_lowering=False)
    es = ExitStack()
    ntok = nseq * ntile * 128
    with es:
        k = K(nc, es)
        def din(name, shape, dt=F32):
            return nc.dram_tensor(name, shape, dt, kind="ExternalInput").ap()

        x_d = din("x", [nseq * SEQ, D])
        c_d = din("c", [nseq, D])
        w_ada_d = din("w_ada", [D, 6 * D])
        b_ada_d = din("b_ada", [1, 6 * D])
        n1pre_d = din("norm1_pre", [1, D])
        n1post_d = din("norm1_post", [1, D])
        w_in_d = din("w_in", [D, IN_W])
        conv_a_d = din("conv_a_w", [3, D])
        conv_qk_d = din("conv_qk_w", [4, D])
        big_d = din("b_igate", [1, 8])
        bfg_d = din("b_fgate", [1, 8])
        mhw_d = din("mh_norm_w", [1, D])
        wsrc = {"w_in": w_in_d,
                "w_branch_a": din("w_branch_a", [D, D]),
                "w_branch_m": din("w_branch_m", [D, D]),
                "w_out": din("w_out", [D, D]),
                "peer_wq": din("peer_wq", [D, 2048])}
        n2pre_d = din("norm2_pre", [1, D])
        n2post_d = din("norm2_post", [1, D])
        sk_d = din("peer_subkeys", [16, 128, 128])
        pu_d = din("peer_u", [16384, D])
        pv_d = din("peer_v", [16384, D])
        cst_d = din("cst", [128, 1024])
        for nm in ("x", "c", "w_ada", "b_ada", "norm1_pre", "norm1_post", "w_in", "conv_a_w", "conv_qk_w", "b_igate",
                   "b_fgate", "mh_norm_w", "w_branch_a", "w_branch_m", "w_out", "peer_wq", "norm2_pre", "norm2_post",
                   "peer_subkeys", "peer_u", "peer_v", "cst"):
            k.tr[nm] = {"w": {}, "r": {}}
        out_d = k.dram("out", [nseq * SEQ, D], F32, "ExternalOutput")
        wsc_d = k.dram("wsc", [NBLK, 128, 8 * 512], BF16, "Internal")
        ada_d = k.dram("ada_sc", [nseq, 6 * D], F32, "Internal")
        ub_d = k.dram("ub_sc", [16384, D], BF16, "Internal")
        vb_d = k.dram("vb_sc", [16384, D], BF16, "Internal")
        dbg_d = None
        if dbg is not None:
            dbg_d = k.dram("dbg", [128, dbg], F32, "ExternalOutput")

        cst = k.sb("cst_sb", [128, 1024], F32)
        identf = cst[:, 0:128]
        tri2 = cst[:, 128:256]
        ones128 = cst[:, 256:384]
        rowmask = cst[:, 384:386]
        iota16 = cst[:, 576:592]
        onescol = cst[:, 592:593]
        identb_t = k.sb("identb", [128, 128], BF16)
        onesb_t = k.sb("onesb", [128, 1], BF16)
        wbuf = [k.sb("wbuf%d" % i, [128, 8, 512], BF16) for i in range(2)]
        rep = k.sb("rep", [128, 6, 1024], F32)
        Fs = [k.sb("F%d" % i, [128, D], F32) for i in range(6)]
        Hs = [k.sb("H%d" % i, [128, D], BF16) for i in range(8)]
        UA = k.sb("UA", [128, 8, 130], F32)
        UQ = k.sb("UQ", [128, 8, 131], F32)
        cwa = k.sb("cwa", [128, 3, 8], F32)
        cwq = k.sb("cwq", [128, 4, 8], F32)
        skT = k.sb("skT", [128, 16, 128], BF16)
        Cf = k.sb("Cf", [128, 4, 128], F32)
        Cb = k.sb("Cb", [128, 4, 128], BF16)
        nf = k.sb("nf", [128, 4], F32)
        nb = k.sb("nb", [128, 4], BF16)
        sm = k.sb("sm", [128, 256], F32)
        sm2 = k.sb("sm2", [128, 256], F32)
        bif = k.sb("bif", [128, 16], F32)
        qT = k.sb("qT", [128, 16, 128], BF16)
        scb = k.sb("scb", [128, 128], F32)
        cand = k.sb("cand", [128, 256], F32)
        candb = k.sb("candb", [128, 256], F32)
        v1 = k.sb("v1", [128, 8, 16], F32)
        v2 = k.sb("v2", [128, 8, 16], F32)
        i1 = k.sb("i1", [128, 8, 16], U32)
        i2 = k.sb("i2", [128, 8, 16], U32)
        cv = k.sb("cv", [128, 8, 16], F32)
        ci = k.sb("ci", [128, 8, 16], U32)
        tkf = [k.sb("tkf%d" % i, [128, 8, 16], F32) for i in range(6)]
        tku = [k.sb("tku%d" % i, [128, 8, 16], U32) for i in range(2)]
        zz = k.sb("zz", [128, 128], F32)
        aa = k.sb("aa", [128, 128], F32)

        PB = [k.ps("PB%d" % i, [128, 512], F32) for i in range(7)]
        PT = k.ps("PT", [128, 1024], BF16)
        es2 = ExitStack()
        stage = [k.sb("stage%d" % i, [128, 8, 512], F32, es=es2) for i in range(2)]

        k.dma("sp", cst[:], cst_d[:, :])
        k.cp(identb_t[:], identf)
        k.cp(onesb_t[:], onescol)
        identb = identb_t[:]

        for j in range(3):
            k.dma("sp", cwa[:, j, :], conv_a_d[j:j + 1, :].rearrange("o (ch p) -> p (o ch)", p=128), allow_slow_non_contiguous=True)
        for j in range(4):
            k.dma("sp", cwq[:, j, :], conv_qk_d[j:j + 1, :].rearrange("o (ch p) -> p (o ch)", p=128), allow_slow_non_contiguous=True)
        k.dma("sp", bif[:, 0:8], big_d.partition_broadcast(128))
        k.dma("sp", bif[:, 8:16], bfg_d.partition_broadcast(128))

        cT = sm[:, 0:8 * nseq].rearrange("p (b kc) -> p b kc", b=nseq)
        for b_ in range(nseq):
            k.dma("sp", cT[:, b_, :], c_d[b_:b_ + 1, :].rearrange("o (kc p) -> p (o kc)", p=128), allow_slow_non_contiguous=True)
        scT = sm2[:, 0:8 * nseq].rearrange("p (b kc) -> p b kc", b=nseq)
        k.act(scT, cT, AF.Silu)
        adat = Fs[0]
        ada_sb = rep
        ada_flat = ada_sb[0:nseq].rearrange("p a d -> p (a d)")
        nvec = Fs[1]
        w_ada_v = w_ada_d.rearrange("(kc p) n -> p kc n", p=128)
        for g in range(12):
            st = stage[g % 2]
            k.dma("sp", st[:], w_ada_v[:, :, g * 512:(g + 1) * 512])
            for kc in range(8):
                k.mm(PB[g % 2][0:nseq, :], scT[:, :, kc], st[:, kc, :], start=(kc == 0), stop=(kc == 7))
            k.cp(ada_flat[:, g * 512:(g + 1) * 512], PB[g % 2][0:nseq, :])
        bad = Fs[2]
        for g in range(6):
            k.dma("sp", Fs[2 + (g % 2)][0:nseq, :], b_ada_d[:, g * D:(g + 1) * D].partition_broadcast(nseq))
            k.tt(ada_sb[0:nseq, g, :], ada_sb[0:nseq, g, :], Fs[2 + (g % 2)][0:nseq, :], ALU.add)
        comb = Fs[4]
        res6 = [Fs[4], Fs[5], Hs[0], Hs[1]]
        nv = Fs[1]
        k.dma("sp", nv[0:nseq, :], n1pre_d.partition_broadcast(nseq))
        k.stt(Fs[4][0:nseq, :], ada_sb[0:nseq, 1, :], 1.0, nv[0:nseq, :], ALU.add, ALU.mult)
        k.dma("sp", ada_d[:, 0 * D:1 * D], Fs[4][0:nseq, :])
        k.dma("sp", ada_d[:, 1 * D:2 * D], ada_sb[0:nseq, 0, :])
        k.dma("sp", Fs[2][0:nseq, :], n1post_d.partition_broadcast(nseq))
        k.tt(Fs[5][0:nseq, :], ada_sb[0:nseq, 2, :], Fs[2][0:nseq, :], ALU.mult)
        k.dma("sp", ada_d[:, 2 * D:3 * D], Fs[5][0:nseq, :])
        k.dma("sp", Fs[3][0:nseq, :], n2pre_d.partition_broadcast(nseq))
        k.stt(Fs[0][0:nseq, :], ada_sb[0:nseq, 4, :], 1.0, Fs[3][0:nseq, :], ALU.add, ALU.mult)
        k.dma("sp", ada_d[:, 3 * D:4 * D], Fs[0][0:nseq, :])
        k.dma("sp", ada_d[:, 4 * D:5 * D], ada_sb[0:nseq, 3, :])
        k.dma("sp", nv[0:nseq, :], n2post_d.partition_broadcast(nseq))
        k.tt(Fs[4][0:nseq, :], ada_sb[0:nseq, 5, :], nv[0:nseq, :], ALU.mult)
        k.dma("sp", ada_d[:, 5 * D:6 * D], Fs[4][0:nseq, :])

        mhw = sm[:, 16:24]
        k.dma("sp", mhw, mhw_d.rearrange("o (kc p) -> p (o kc)", p=128), allow_slow_non_contiguous=True)
        for bi, (nm, c0, ncol) in enumerate(WBLOCKS):
            st = stage[bi % 2]
            wb = wbuf[bi % 2]
            src = wsrc[nm].rearrange("(kc p) n -> p kc n", p=128)
            k.dma("sp", st[:, :, 0:ncol], src[:, :, c0:c0 + ncol])
            if nm == "w_branch_m":
                for kc in range(8):
                    k.ts(wb[:, kc, 0:ncol], st[:, kc, 0:ncol], mhw[:, kc:kc + 1], None, ALU.mult)
            else:
                k.cp(wb[:, 0:4, 0:ncol], st[:, 0:4, 0:ncol], e="dve")
                k.cp(wb[:, 4:8, 0:ncol], st[:, 4:8, 0:ncol], e="act")
            k.dma("pool", wsc_d[bi].rearrange("p (kc n) -> p kc n", n=512)[:, :, 0:ncol], wb[:, :, 0:ncol])
        for (src_d, dst_d) in ((pu_d, ub_d), (pv_d, vb_d)):
            sv_ = src_d.rearrange("(c p r) d -> c p (r d)", p=128, r=4)
            dv_ = dst_d.rearrange("(c p r) d -> c p (r d)", p=128, r=4)
            for c_ in range(32):
                stf = stage[c_ % 2][:].rearrange("p a n -> p (a n)")
                wbf = wbuf[c_ % 2][:].rearrange("p a n -> p (a n)")
                k.dma("sp", stf, sv_[c_])
                k.cp(wbf[:, 0:2048], stf[:, 0:2048], e="dve")
                k.cp(wbf[:, 2048:4096], stf[:, 2048:4096], e="act")
                k.dma("pool", dv_[c_], wbf)
        for hc in range(16):
            st = stage[hc % 2]
            k.dma("sp", st[:, 0, 0:128], sk_d[hc])
            k.tp(PB[hc % 2][:, 0:128], st[:, 0, 0:128], identf)
            k.cp(skT[:, hc, :], PB[hc % 2][:, 0:128])
        k.barrier()
        es2.close()
        gbufs = [k.sb("gbuf%d" % i, [128, D], BF16) for i in range(NGBUF)]
        resident = {}
        for bi in RESIDENT:
            rt = k.sb("wres%d" % bi, [128, 8, 512], BF16)
            k.dma("sp", rt[:], wsc_d[bi].rearrange("p (kc n) -> p kc n", n=512))
            resident[bi] = rt
        X1p = [k.sb("X1p%d" % i, [128, D], F32) for i in range(2)]
        H2p = [k.sb("H2p%d" % i, [128, D], F32) for i in range(2)]
        eidxp = [k.sb("eidxp%d" % i, [128, 128], U32) for i in range(2)]
        gatep = [k.sb("gatep%d" % i, [128, 128], F32) for i in range(2)]
        JKb = k.sb("JKb", [128, D], BF16)
        TMPt = k.sb("TMPt", [128, D], F32)
        Dg = [k.sb("Dg%d" % i, [128, 128], BF16) for i in range(4)]
        smb = k.sb("smb", [128, 8], F32)

        wstate = {"n": 0}

        def load_block(bi):
            if bi in resident:
                return resident[bi]
            wb = wbuf[wstate["n"] % 2]
            wstate["n"] += 1
            ncol = WBLOCKS[bi][2]
            k.dma("sp", wb[:, :, 0:ncol], wsc_d[bi].rearrange("p (kc n) -> p kc n", n=512)[:, :, 0:ncol])
            return wb

        def dump(ap, col0, ncols, parts=128):
            if dbg_d is not None:
                k.dma("sp", dbg_d[0:parts, col0:col0 + ncols], ap)

        for s in range(nseq):
            k.dma("sp", rep[:].rearrange("p a d -> p (a d)"), ada_d[s:s + 1, :].partition_broadcast(128))
            W1, SH1, G1N, W2, SH2, G2N = [rep[:, i, :] for i in range(6)]
            k.memset(UA[:, :, 0:2], 0.0)
            k.memset(UQ[:, :, 0:3], 0.0)
            k.memset(Cf[:], 0.0)
            k.memset(Cb[:], 0.0)
            k.memset(nf[:], 0.0)
            k.memset(nb[:], 0.0)
            def front(s, ti, par):
                r0 = s * SEQ + ti * 128
                eidx, gate = eidxp[par], gatep[par]
                X = X1p[par]
                k.dma("sp", X[:], x_d[r0:r0 + 128, :])
                ssq = sm[:, 32:33]
                k.act(Fs[0][:], X[:], AF.Square, accum_out=ssq)
                rstd = sm[:, 33:34]
                k.ts(sm[:, 34:35], ssq, 1.0 / D, EPS, ALU.mult, ALU.add)
                k.act(sm[:, 35:36], sm[:, 34:35], AF.Sqrt)
                k.recip(rstd, sm[:, 35:36])
                k.stt(Fs[0][:], X[:], rstd, W1, ALU.mult, ALU.mult)
                hb = Hs[0]
                k.tt(hb[:], Fs[0][:], SH1, ALU.add)
                for kc in range(8):
                    k.tp(PT[:, kc * 128:(kc + 1) * 128], hb[:, kc * 128:(kc + 1) * 128], identb)
                hT = Hs[1]
                k.cp(hT[:], PT[:])
                hTv = hT[:].rearrange("p (kc t) -> p kc t", t=128)
                if stop_after == "hT":
                    k.cp(Fs[1][:], hT[:])
                    dump(Fs[1][:], 0, 1024)
                    return
                yield
                CG, BG, XIN = Fs[1], Fs[2], Fs[3]
                SGA, SGM = Hs[2], Hs[3]
                Vb = Hs[4]
                SO = Fs[4]
                IFt = sm[:, 40:56]

                def fm_block(bi, dst_fn, pbi):
                    wb = load_block(bi)
                    pb = PB[pbi]
                    for j in range(4):
                        for kc in range(8):
                            k.mm(pb[:, j * 128:(j + 1) * 128], wb[:, kc, j * 128:(j + 1) * 128], hTv[:, kc, :],
                                 start=(kc == 0), stop=(kc == 7))
                    dst_fn(pb)

                def evac_plain(dst3, e):
                    def f(pb):
                        k.cp(dst3, pb[:].rearrange("p (j t) -> p j t", t=128), e=e)
                    return f

                def evac_sig(dst3):
                    def f(pb):
                        k.act(dst3, pb[:].rearrange("p (j t) -> p j t", t=128), AF.Sigmoid)
                    return f

                def v3(t, lo):
                    return t[:].rearrange("p (j t) -> p j t", t=128)[:, lo:lo + 4, :]

                fm_block(0, evac_plain(v3(CG, 0), "act"), 0)
                fm_block(1, evac_plain(v3(CG, 4), "dve"), 1)
                yield
                fm_block(2, evac_plain(v3(BG, 0), "act"), 0)
                fm_block(3, evac_plain(v3(BG, 4), "dve"), 1)
                yield
                fm_block(4, evac_plain(v3(XIN, 0), "act"), 0)
                fm_block(5, evac_plain(v3(XIN, 4), "dve"), 1)
                yield
                fm_block(6, evac_plain(UQ[:, 0:4, 3:131], "act"), 0)
                fm_block(7, evac_plain(UQ[:, 4:8, 3:131], "dve"), 1)
                yield
                for gi, bi in enumerate((8, 9, 10, 11)):
                    wb = load_block(bi)
                    pb = PB[2 + gi % 2]
                    for kc in range(8):
                        k.mm(pb[:], hTv[:, kc, :], wb[:, kc, :], start=(kc == 0), stop=(kc == 7))
                    if gi < 2:
                        k.cp(Vb[:, gi * 512:(gi + 1) * 512], pb[:], e="dve")
                    else:
                        k.act(SO[:, (gi - 2) * 512:(gi - 1) * 512], pb[:], AF.Sigmoid)
                wb = load_block(12)
                for kc in range(8):
                    k.mm(PB[4][:, 0:16], hTv[:, kc, :], wb[:, kc, 0:16], start=(kc == 0), stop=(kc == 7))
                k.tt(IFt, PB[4][:, 0:16], bif[:], ALU.add)
                yield
                fm_block(13, evac_sig(v3(SGA, 0)), 0)
                fm_block(14, evac_sig(v3(SGA, 4)), 1)
                yield
                fm_block(15, evac_sig(v3(SGM, 0)), 0)
                fm_block(16, evac_sig(v3(SGM, 4)), 1)
                if stop_after == "proj":
                    dump(CG[:], 0, 1024)
                    dump(UQ[:, :, 3:131], 1024, 1024)
                    k.cp(Fs[0][:], Vb[:])
                    dump(Fs[0][:], 2048, 1024)
                    dump(SO[:], 3072, 1024)
                    dump(IFt, 4096, 16)
                    k.cp(Fs[5][:], SGM[:])
                    dump(Fs[5][:], 4112, 1024)
                    return
                yield
                CG3 = CG[:].rearrange("p (j t) -> p j t", t=128)
                BG3 = BG[:].rearrange("p (j t) -> p j t", t=128)
                XIN3 = XIN[:].rearrange("p (j t) -> p j t", t=128)
                k.tt(UA[:, :, 2:130], CG3, XIN3, ALU.mult)
                T0 = Fs[0][:].rearrange("p (j t) -> p j t", t=128)
                T1 = CG3
                k.tt(T0, UA[:, :, 0:128], cwa[:, 0, :].unsqueeze(2).to_broadcast([128, 8, 128]), ALU.mult)
                k.tt(T1, UA[:, :, 1:129], cwa[:, 1, :].unsqueeze(2).to_broadcast([128, 8, 128]), ALU.mult)
                k.tt(T0, T0, T1, ALU.add)
                k.tt(T1, UA[:, :, 2:130], cwa[:, 2, :].unsqueeze(2).to_broadcast([128, 8, 128]), ALU.mult)
                k.tt(T0, T0, T1, ALU.add)
                yaT = Hs[5]
                k.tt(yaT[:].rearrange("p (j t) -> p j t", t=128), T0, BG3, ALU.mult)
                k.cp(UA[:, :, 0:2], UA[:, :, 128:130])
                yield
                T1 = XIN3
                k.tt(T0, UQ[:, :, 0:128], cwq[:, 0, :].unsqueeze(2).to_broadcast([128, 8, 128]), ALU.mult)
                for j in range(1, 4):
                    k.tt(T1, UQ[:, :, j:j + 128], cwq[:, j, :].unsqueeze(2).to_broadcast([128, 8, 128]), ALU.mult)
                    k.tt(T0, T0, T1, ALU.add)
                qkT = Hs[6]
                qk3 = qkT[:].rearrange("p (j t) -> p j t", t=128)
                k.act(qk3, T0, AF.Silu)
                k.cp(UQ[:, :, 0:3], UQ[:, :, 128:131])
                yield
                IG = IFt[:, 0:8]
                FG = IFt[:, 8:16]
                lf = sm[:, 56:64]
                k.act(lf, FG, AF.Exp, scale=-1.0)
                k.act(lf, lf, AF.Ln, bias=1.0)
                k.ts(lf, lf, -1.0, None, ALU.mult)
                k.mm(PB[4][:, 16:24], tri2, lf)
                k.mm(PB[4][:, 24:32], ones128, lf)
                Bt = sm[:, 64:72]
                BL = sm[:, 72:80]
                k.cp(Bt, PB[4][:, 16:24])
                k.cp(BL, PB[4][:, 24:32])
                u = sm[:, 88:96]
                eB = sm[:, 96:104]
                wk = sm[:, 104:112]
                eBL = sm[:, 112:116]
                k.tt(u, IG, Bt, ALU.subtract)
                k.tt(wk, u, BL, ALU.add)
                k.act(u, u, AF.Exp)
                k.act(wk, wk, AF.Exp)
                k.act(eB, Bt, AF.Exp)
                BLv = BL.rearrange("p (j two) -> p j two", two=2)
                k.act(eBL[0:64, :], BLv[0:64, :, 0], AF.Exp)
                k.act(eBL[64:128, :], BLv[64:128, :, 1], AF.Exp)
                for j in range(4):
                    k.tp(PT[:, j * 128:(j + 1) * 128], qk3[:, 4 + j, :], identb)
                kw = Hs[7]
                kw3 = kw[:, 0:512].rearrange("p (h d) -> p h d", d=64)
                k.tt(kw3, PT[:, 0:512].rearrange("p (h d) -> p h d", d=64), wk.unsqueeze(2).to_broadcast([128, 8, 64]), ALU.mult)
                yield
                Qm = [Fs[0][:, 0:256].bitcast(BF16).rearrange("p (j t) -> p j t", t=128),
                      Fs[0][:, 256:512].bitcast(BF16).rearrange("p (j t) -> p j t", t=128)]
                for e_ in range(2):
                    k.ts(Qm[e_], qk3[:, 0:4, :], rowmask[:, e_:e_ + 1], None, ALU.mult)
                SB = [PB[2], PB[3]]
                for h in range(8):
                    k.mm(SB[h // 4][:, (h % 4) * 128:(h % 4 + 1) * 128], qk3[:, 4 + h // 2, :], Qm[h % 2][:, h // 2, :])
                SwT = Hs[1]
                Sw3 = SwT[:].rearrange("p (h t) -> p h t", t=128)
                S3 = Fs[5][:].rearrange("p (h t) -> p h t", t=128)
                for g2 in range(2):
                    k.tt(S3[:, g2 * 4:(g2 + 1) * 4, :], SB[g2][:].rearrange("p (h t) -> p h t", t=128),
                         u[:, g2 * 4:(g2 + 1) * 4].unsqueeze(2).to_broadcast([128, 4, 128]), ALU.mult)
                k.tt(Sw3, S3, tri2.unsqueeze(1).to_broadcast([128, 8, 128]), ALU.mult)
                yield
                Vb3 = Vb[:].rearrange("p (h d) -> p h d", d=128)
                NUM = [PB[0], PB[1]]
                DEN = PB[4][:, 40:48]
                for h in range(8):
                    q_h = Qm[h % 2][:, h // 2, :]
                    numo = NUM[h // 4][:, (h % 4) * 128:(h % 4 + 1) * 128]
                    k.mm(numo, Sw3[:, h, :], Vb3[:, h, :], start=True, stop=False)
                    k.mm(numo, q_h, Cb[:, h // 2, :], start=False, stop=True)
                    k.mm(DEN[:, h:h + 1], Sw3[:, h, :], onesb_t[:], start=True, stop=False)
                    k.mm(DEN[:, h:h + 1], q_h, nb[:, h // 2:h // 2 + 1], start=False, stop=True)
                yield
                kwp = kw[:, 0:512].rearrange("p (j d) -> p j d", d=128)
                for h in range(8):
                    k.mm(PB[2 + h % 2][:, (h // 2) * 128:(h // 2 + 1) * 128], kwp[:, h // 2, :], Vb3[:, h, :])
                for j in range(4):
                    k.mm(PB[4][:, 48 + j:49 + j], kwp[:, j, :], onesb_t[:])
                k.tt(Cf[:], Cf[:], eBL.unsqueeze(2).to_broadcast([128, 4, 128]), ALU.mult)
                k.tt(Cf[0:64], Cf[0:64], PB[2][0:64, :].rearrange("p (j d) -> p j d", d=128), ALU.add)
                k.tt(Cf[64:128], Cf[64:128], PB[3][64:128, :].rearrange("p (j d) -> p j d", d=128), ALU.add)
                k.cp(Cb[:], Cf[:], e="act")
                k.tt(nf[:], nf[:], eBL, ALU.mult)
                k.tt(nf[:], nf[:], PB[4][:, 48:52], ALU.add)
                k.cp(nb[:], nf[:], e="act")
                yield
                dn = sm[:, 120:128]
                k.stt(dn, DEN, 0.125, eB, ALU.mult, ALU.mult)
                k.act(dn, dn, AF.Abs)
                k.ts(dn, dn, 1.0, None, ALU.max)
                k.recip(dn, dn)
                k.stt(dn, eB, 0.125, dn, ALU.mult, ALU.mult)
                HN = Fs[5]
                HN3 = HN[:].rearrange("p (h d) -> p h d", d=128)
                for g2 in range(2):
                    k.tt(HN3[:, g2 * 4:(g2 + 1) * 4, :], NUM[g2][:].rearrange("p (h d) -> p h d", d=128),
                         dn[:, g2 * 4:(g2 + 1) * 4].unsqueeze(2).to_broadcast([128, 4, 128]), ALU.mult)
                if stop_after == "mlstm":
                    dump(HN[:], 0, 1024)
                    k.cp(Fs[0][:], yaT[:])
                    dump(Fs[0][:], 1024, 1024)
                    return
                yield
                k.tt(Fs[0][:], HN[:], HN[:], ALU.mult)
                hss = sm[:, 128:136]
                k.red(hss, Fs[0][:].rearrange("p (h d) -> p h d", d=128), ALU.add)
                k.ts(hss, hss, 1.0 / 128, EPS, ALU.mult, ALU.add)
                k.act(hss, hss, AF.Sqrt)
                k.recip(hss, hss)
                k.tt(HN3, HN3, hss.unsqueeze(2).to_broadcast([128, 8, 128]), ALU.mult)
                ymb = Hs[0]
                k.tt(ymb[:], HN[:], SO[:], ALU.mult)
                for kc in range(8):
                    k.tp(PT[:, kc * 128:(kc + 1) * 128], ymb[:, kc * 128:(kc + 1) * 128], identb)
                ymT = Hs[4]
                k.cp(ymT[:], PT[:])
                ymT3 = ymT[:].rearrange("p (kc t) -> p kc t", t=128)
                yaT3 = yaT[:].rearrange("p (kc t) -> p kc t", t=128)
                yield
                MIX = Fs[0]
                MIX3 = MIX[:].rearrange("p (j t) -> p j t", t=128)

                def branch(bname, src3, sg, first):
                    for half in range(2):
                        wb = load_block(BLK[(bname, half * 512)])
                        pb = PB[2 + half]
                        for j in range(4):
                            for kc in range(8):
                                k.mm(pb[:, j * 128:(j + 1) * 128], wb[:, kc, j * 128:(j + 1) * 128], src3[:, kc, :],
                                     start=(kc == 0), stop=(kc == 7))
                        sg3 = sg[:].rearrange("p (j t) -> p j t", t=128)[:, half * 4:(half + 1) * 4, :]
                        dst = MIX3[:, half * 4:(half + 1) * 4, :]
                        p3 = pb[:].rearrange("p (j t) -> p j t", t=128)
                        if first:
                            k.tt(dst, p3, sg3, ALU.mult)
                        else:
                            t3 = Fs[1][:].rearrange("p (j t) -> p j t", t=128)[:, half * 4:(half + 1) * 4, :]
                            k.tt(t3, p3, sg3, ALU.mult)
                            k.tt(dst, dst, t3, ALU.add)

                branch("w_branch_a", yaT3, SGA, True)
                yield
                branch("w_branch_m", ymT3, SGM, False)
                yield
                mixT = Hs[2]
                k.cp(mixT[:], MIX[:], e="act")
                mixT3 = mixT[:].rearrange("p (kc t) -> p kc t", t=128)
                for half in range(2):
                    wb = load_block(BLK[("w_out", half * 512)])
                    for kc in range(8):
                        k.mm(PB[half][:], mixT3[:, kc, :], wb[:, kc, :], start=(kc == 0), stop=(kc == 7))
                yield
                Y = Fs[1]
                k.cp(Y[:, 0:512], PB[0][:], e="act")
                k.cp(Y[:, 512:1024], PB[1][:], e="dve")
                ssq2 = sm[:, 136:137]
                k.act(Fs[0][:], Y[:], AF.Square, accum_out=ssq2)
                k.ts(ssq2, ssq2, 1.0 / D, EPS, ALU.mult, ALU.add)
                k.act(ssq2, ssq2, AF.Sqrt)
                k.recip(ssq2, ssq2)
                k.stt(Fs[0][:], Y[:], ssq2, G1N, ALU.mult, ALU.mult)
                X1 = X1p[par]
                k.tt(X1[:], Fs[0][:], X[:], ALU.add)
                if stop_after == "sub1":
                    k.dma("sp", out_d[r0:r0 + 128, :], X1[:])
                    return
                yield
                ssq3 = sm[:, 137:138]
                k.act(Fs[0][:], X1[:], AF.Square, accum_out=ssq3)
                k.ts(ssq3, ssq3, 1.0 / D, EPS, ALU.mult, ALU.add)
                k.act(ssq3, ssq3, AF.Sqrt)
                k.recip(ssq3, ssq3)
                k.stt(Fs[0][:], X1[:], ssq3, W2, ALU.mult, ALU.mult)
                H2 = H2p[par]
                k.tt(H2[:], Fs[0][:], SH2, ALU.add)
                h2b = Hs[0]
                k.cp(h2b[:], H2[:], e="act")
                for kc in range(8):
                    k.tp(PT[:, kc * 128:(kc + 1) * 128], h2b[:, kc * 128:(kc + 1) * 128], identb)
                h2T = Hs[1]
                k.cp(h2T[:], PT[:])
                h2T3 = h2T[:].rearrange("p (kc t) -> p kc t", t=128)
                yield
                for g4 in range(4):
                    wb = load_block(BLK[("peer_wq", g4 * 512)])
                    pb = PB[2 + g4 % 2]
                    for j in range(4):
                        for kc in range(8):
                            k.mm(pb[:, j * 128:(j + 1) * 128], wb[:, kc, j * 128:(j + 1) * 128], h2T3[:, kc, :],
                                 start=(kc == 0), stop=(kc == 7))
                    k.cp(qT[:, g4 * 4:(g4 + 1) * 4, :], pb[:].rearrange("p (j t) -> p j t", t=128), e=("act" if g4 % 2 else "dve"))
                yield
                scv = [Fs[1][:].rearrange("p (j n) -> p j n", n=128), Fs[2][:].rearrange("p (j n) -> p j n", n=128)]
                tk0v = Fs[3][:].rearrange("p (h a b) -> p h a b", a=16, b=16)
                for g4 in range(4):
                    pb = PB[g4 % 2]
                    for j in range(4):
                        hc = g4 * 4 + j
                        k.mm(pb[:, j * 128:(j + 1) * 128], qT[:, hc, :], skT[:, hc, :])
                    k.cp(scv[g4 // 2][:, (g4 % 2) * 4:(g4 % 2) * 4 + 4, :], pb[:].rearrange("p (j n) -> p j n", n=128), e=("act" if g4 % 2 else "dve"))
                yield
                dv = k.eng["dve"]
                for h in range(8):
                    for half, (vv, ii) in enumerate(((v1, i1), (v2, i2))):
                        s_ = scv[(2 * h + half) // 8][:, (2 * h + half) % 8, :]
                        k.op("dve", lambda g, vv=vv, h=h, s_=s_: g.max(vv[:, h, 0:8], s_), [s_], [vv[:]])
                        k.op("dve", lambda g, vv=vv, ii=ii, h=h, s_=s_: g.max_index(ii[:, h, 0:8], vv[:, h, 0:8], s_), [s_, vv[:]], [ii[:]])
                        k.op("dve", lambda g, vv=vv, h=h, s_=s_: g.match_replace(scb[:], vv[:, h, 0:8], s_, NEG), [s_, vv[:]], [scb[:]])
                        k.op("dve", lambda g, vv=vv, h=h: g.max(vv[:, h, 8:16], scb[:]), [scb[:]], [vv[:]])
                        k.op("dve", lambda g, vv=vv, ii=ii, h=h: g.max_index(ii[:, h, 8:16], vv[:, h, 8:16], scb[:]), [scb[:], vv[:]], [ii[:]])
                    yield
                    c3 = cand[:].rearrange("p (a b) -> p a b", b=16)
                    k.tt(c3, v1[:, h, :].unsqueeze(2).to_broadcast([128, 16, 16]),
                         v2[:, h, :].unsqueeze(1).to_broadcast([128, 16, 16]), ALU.add)
                    k.op("dve", lambda g, h=h: g.max(cv[:, h, 0:8], cand[:]), [cand[:]], [cv[:]])
                    k.op("dve", lambda g, h=h: g.max_index(ci[:, h, 0:8], cv[:, h, 0:8], cand[:]), [cand[:], cv[:]], [ci[:]])
                    k.op("dve", lambda g, h=h: g.match_replace(candb[:], cv[:, h, 0:8], cand[:], NEG), [cand[:], cv[:]], [candb[:]])
                    k.op("dve", lambda g, h=h: g.max(cv[:, h, 8:16], candb[:]), [candb[:]], [cv[:]])
                    k.op("dve", lambda g, h=h: g.max_index(ci[:, h, 8:16], cv[:, h, 8:16], candb[:]), [candb[:], cv[:]], [ci[:]])
                yield
                ua, ub_ = tku[0], tku[1]
                k.ts(ua[:], ci[:], 4, None, ALU.logical_shift_right)
                k.ts(ub_[:], ci[:], 15, None, ALU.bitwise_and)
                af, bf_, i1f, i2f, isel, jsel = tkf
                k.cp(af[:], ua[:])
                k.cp(bf_[:], ub_[:])
                k.cp(i1f[:], i1[:])
                k.cp(i2f[:], i2[:])
                io4 = iota16.unsqueeze(1).unsqueeze(1).to_broadcast([128, 4, 16, 16])
                for (xf, tab, dst) in ((af, i1f, isel), (bf_, i2f, jsel)):
                    for hh in range(2):
                        hs4 = slice(hh * 4, hh * 4 + 4)
                        k.tt(tk0v, xf[:, hs4, :].unsqueeze(3).to_broadcast([128, 4, 16, 16]), io4, ALU.is_equal)
                        k.tt(tk0v, tk0v, tab[:, hs4, :].unsqueeze(2).to_broadcast([128, 4, 16, 16]), ALU.mult)
                        k.red(dst[:, hs4, :], tk0v, ALU.add)
                ef = af
                k.stt(ef[:], isel[:], 128.0, jsel[:], ALU.mult, ALU.add)
                k.cp(eidx[:].rearrange("p (h k) -> p h k", k=16), ef[:])
                yield
                gx = bf_
                k.tt(gx[:], cv[:], cv[:, :, 0:1].to_broadcast([128, 8, 16]), ALU.subtract)
                k.act(gx[:], gx[:], AF.Exp)
                gs = sm[:, 144:152]
                k.red(gs, gx[:], ALU.add)
                k.recip(gs, gs)
                k.tt(gate[:].rearrange("p (h k) -> p h k", k=16), gx[:], gs.unsqueeze(2).to_broadcast([128, 8, 16]), ALU.mult)
                if stop_after == "topk":
                    k.cp(Fs[0][:, 0:128], eidx[:])
                    dump(Fs[0][:, 0:128], 0, 128)
                    dump(gate[:], 128, 128)
                    dump(H2[:], 256, 1024)
                    return
                yield
            def back(s, ti, par):
                r0 = s * SEQ + ti * 128
                X1, H2, eidx, gate = X1p[par], H2p[par], eidxp[par], gatep[par]
                NG = len(gbufs)
                for sl in range(128):
                    gb = gbufs[sl % NG][:]
                    k.dma("pool", gb, ub_d, fn=lambda g, gb=gb, sl=sl, eidx=eidx: g.indirect_dma_start(
                        out=gb, out_offset=None, in_=ub_d[:, :],
                        in_offset=bass.IndirectOffsetOnAxis(ap=eidx[:, sl:sl + 1], axis=0)), extra_reads=[eidx[:]])
                    k.stt(JKb[:], gb, 1.0, H2[:], ALU.mult, ALU.mult, accum_out=zz[:, sl:sl + 1])
                    if sl % 4 == 3:
                        yield
                k.tt(aa[:], zz[:], zz[:], ALU.mult)
                k.ts(aa[:], aa[:], 0.044715, 1.0, ALU.mult, ALU.add)
                k.tt(aa[:], aa[:], zz[:], ALU.mult)
                k.act(aa[:], aa[:], AF.Sigmoid, scale=1.5957691216057308)
                k.tt(aa[:], aa[:], zz[:], ALU.mult)
                k.tt(aa[:], aa[:], gate[:], ALU.mult)
                yield
                for sl in range(128):
                    gb = gbufs[sl % NG][:]
                    k.dma("pool", gb, vb_d, fn=lambda g, gb=gb, sl=sl, eidx=eidx: g.indirect_dma_start(
                        out=gb, out_offset=None, in_=vb_d[:, :],
                        in_offset=bass.IndirectOffsetOnAxis(ap=eidx[:, sl:sl + 1], axis=0)), extra_reads=[eidx[:]])
                    dg = Dg[sl % 4]
                    k.act(dg[:], identb, AF.Copy, scale=aa[:, sl:sl + 1])
                    k.mm(PB[5][:], dg[:], gb[:, 0:512], start=(sl == 0), stop=(sl == 127))
                    k.mm(PB[6][:], dg[:], gb[:, 512:1024], start=(sl == 0), stop=(sl == 127))
                    if sl % 4 == 3:
                        yield
                ssq4 = smb[:, 0:1]
                k.act(TMPt[:, 0:512], PB[5][:], AF.Square, accum_out=ssq4)
                k.act(TMPt[:, 512:1024], PB[6][:], AF.Square, accum_out=smb[:, 1:2])
                k.tt(ssq4, ssq4, smb[:, 1:2], ALU.add)
                k.ts(ssq4, ssq4, 1.0 / D, EPS, ALU.mult, ALU.add)
                k.act(ssq4, ssq4, AF.Sqrt)
                k.recip(ssq4, ssq4)
                k.stt(TMPt[:, 0:512], PB[5][:], ssq4, rep[:, 5, 0:512], ALU.mult, ALU.mult)
                k.stt(TMPt[:, 512:1024], PB[6][:], ssq4, rep[:, 5, 512:1024], ALU.mult, ALU.mult)
                k.tt(TMPt[:], TMPt[:], X1[:], ALU.add)
                k.dma("sp", out_d[r0:r0 + 128, :], TMPt[:])
                yield

            prev = None
            for ti in range(ntile):
                fa = k.record(front(s, ti, ti % 2))
                fb = k.record(back(*prev)) if (prev is not None and stop_after is None) else []
                if fb:
                    k.emit_merged(fa, fb)
                else:
                    k.emit_merged(fa, [])
                prev = (s, ti, ti % 2)
            if stop_after is None:
                k.emit_merged(k.record(back(*prev)), [])
        k.barrier()
    return nc


def make_consts():
    c = np.zeros((128, 1024), np.float32)
    c[:, 0:128] = np.eye(128, dtype=np.float32)
    s = np.arange(128)[:, None]
    t = np.arange(128)[None, :]
    c[:, 128:256] = (s <= t).astype(np.float32)
    c[:, 256:384] = 1.0
    c[:, 384] = (np.arange(128) < 64).astype(np.float32)
    c[:, 385] = (np.arange(128) >= 64).astype(np.float32)
    c[:, 576:592] = np.arange(16, dtype=np.float32)[None, :]
    c[:, 592] = 1.0
    return c


_PROG = {}


def kernel(x, c, w_ada, b_ada, norm1_pre, norm1_post, w_in, conv_a_w, conv_qk_w, b_igate, b_fgate, mh_norm_w,
           w_branch_a, w_branch_m, w_out, norm2_pre, norm2_post, peer_wq, peer_subkeys, peer_u, peer_v):
    f = lambda a: np.ascontiguousarray(np.asarray(a, dtype=np.float32))
    if "nc" not in _PROG:
        _PROG["nc"] = build_program()
    nc = _PROG["nc"]
    shared = {
        "w_ada": f(w_ada[0]), "b_ada": f(b_ada), "norm1_pre": f(norm1_pre), "norm1_post": f(norm1_post),
        "w_in": f(w_in[0]), "conv_a_w": f(conv_a_w[0]), "conv_qk_w": f(conv_qk_w[0]), "b_igate": f(b_igate),
        "b_fgate": f(b_fgate), "mh_norm_w": f(mh_norm_w), "w_branch_a": f(w_branch_a[0]),
        "w_branch_m": f(w_branch_m[0]), "w_out": f(w_out[0]), "peer_wq": f(peer_wq[0]),
        "norm2_pre": f(norm2_pre), "norm2_post": f(norm2_post),
        "peer_subkeys": f(peer_subkeys[0]).reshape(16, 128, 128), "peer_u": f(peer_u[0]), "peer_v": f(peer_v[0]),
        "cst": make_consts(),
    }
    xs = f(x)
    cs = f(c)
    in_maps = []
    for i in range(NCORES):
        m = dict(shared)
        m["x"] = xs[2 * i:2 * i + 2].reshape(2 * SEQ, D)
        m["c"] = cs[2 * i:2 * i + 2]
        in_maps.append(m)
    res = run_bass_kernel_spmd(nc, in_maps, core_ids=list(range(NCORES)))
    out = np.concatenate([r["out"].reshape(2, SEQ, D) for r in res.results], axis=0)
    return out.astype(np.float32)
```

```python
from contextlib import ExitStack
import numpy as np
import concourse.bass as bass
import concourse.mybir as mybir
from concourse.bass_utils import run_bass_kernel_spmd

F32 = mybir.dt.float32
BF16 = mybir.dt.bfloat16
U32 = mybir.dt.uint32
I32 = mybir.dt.int32
AF = mybir.ActivationFunctionType
ALU = mybir.AluOpType
AX = mybir.AxisListType

D = 1024
NCORES = 8
SEQ = 4096
NSEQ_CORE = 2
IN_W = 8208
EPS = 1e-6
NEG = -1.0e30

O_CG, O_BG, O_XIN, O_Q, O_K, O_V, O_O, O_I, O_F, O_GA, O_GM = 0, 1024, 2048, 3072, 3584, 4096, 5120, 6144, 6152, 6160, 7184

WBLOCKS = []
for c0 in range(0, 6144, 512):
    WBLOCKS.append(("w_in", c0, 512))
WBLOCKS.append(("w_in", 6144, 16))
for c0 in range(6160, 8208, 512):
    WBLOCKS.append(("w_in", c0, 512))
for nm in ("w_branch_a", "w_branch_m", "w_out"):
    for c0 in (0, 512):
        WBLOCKS.append((nm, c0, 512))
for c0 in range(0, 2048, 512):
    WBLOCKS.append(("peer_wq", c0, 512))
NBLK = len(WBLOCKS)
NGBUF = 16
RESIDENT = tuple(range(0, 4))
BLK = {(nm, c0): i for i, (nm, c0, n) in enumerate(WBLOCKS)}


class K:
    def __init__(self, nc, es):
        self.nc = nc
        self.es = es
        self.eng = {"pe": nc.tensor, "act": nc.scalar, "dve": nc.vector, "pool": nc.gpsimd, "sp": nc.sync}
        self.esem = {e: es.enter_context(nc.semaphore("sem_" + e)) for e in self.eng}
        self.ecnt = {e: 0 for e in self.eng}
        self.semobj = {("e", e): self.esem[e] for e in self.eng}
        self.waited = {e: {} for e in self.eng}
        self.tr = {}
        self.dsem = {}
        self.dcnt = {}
        self.same_engine_sync = {"pe": False, "act": True, "dve": True, "pool": True, "sp": True}
        self.rec = None

    def _reg(self, h, name):
        self.tr[name] = {"w": {}, "r": {}}
        return h

    def sb(self, name, shape, dt, es=None):
        return self._reg((es or self.es).enter_context(self.nc.sbuf_tensor(name, shape, dt)), name)

    def ps(self, name, shape, dt):
        return self._reg(self.es.enter_context(self.nc.psum_tensor(name, shape, dt)), name)

    def dram(self, name, shape, dt, kind):
        t = self.nc.dram_tensor(name, shape, dt, kind=kind)
        self.tr[name] = {"w": {}, "r": {}}
        return t.ap()

    def _rec(self, ap):
        return self.tr[ap.tensor.name if hasattr(ap, "tensor") else ap.name]

    def _name(self, ap):
        try:
            return ap.tensor.name
        except Exception:
            return ap.name

    def _deps(self, reads, writes):
        ev = {}
        for ap in reads:
            rec = self.tr[self._name(ap)]
            for k, v in rec["w"].items():
                ev[k] = max(ev.get(k, 0), v)
        for ap in writes:
            rec = self.tr[self._name(ap)]
            for k, v in rec["w"].items():
                ev[k] = max(ev.get(k, 0), v)
            for k, v in rec["r"].items():
                ev[k] = max(ev.get(k, 0), v)
        return ev

    def _wait(self, e, ev):
        for k, v in ev.items():
            if k == ("e", e) and not self.same_engine_sync[e]:
                continue
            if self.waited[e].get(k, 0) < v:
                self.eng[e].wait_ge(self.semobj[k], v)
                self.waited[e][k] = v

    def _mark(self, key, val, reads, writes):
        for ap in reads:
            rec = self.tr[self._name(ap)]
            rec["r"][key] = max(rec["r"].get(key, 0), val)
        for ap in writes:
            rec = self.tr[self._name(ap)]
            rec["w"][key] = max(rec["w"].get(key, 0), val)

    def op(self, e, fn, reads, writes):
        if self.rec is not None:
            self.rec.append(("op", e, fn, list(reads), list(writes)))
            return None
        self._wait(e, self._deps(reads, writes))
        ins = fn(self.eng[e])
        self.ecnt[e] += 1
        ins.then_inc(self.esem[e], 1)
        self._mark(("e", e), self.ecnt[e], reads, writes)
        return ins

    def dma(self, e, out, in_, fn=None, extra_reads=(), **kw):
        if self.rec is not None:
            self.rec.append(("dma", e, out, in_, fn, list(extra_reads), kw))
            return None
        rds = [in_] + list(extra_reads)
        self._wait(e, self._deps(rds, [out]))
        dn = self._name(out)
        if dn not in self.dsem:
            self.dsem[dn] = self.es.enter_context(self.nc.semaphore("dsem_" + dn))
            self.dcnt[dn] = 0
            self.semobj[("d", dn)] = self.dsem[dn]
        if fn is None:
            ins = self.eng[e].dma_start(out=out, in_=in_, **kw)
        else:
            ins = fn(self.eng[e])
        self.dcnt[dn] += 16
        ins.then_inc(self.dsem[dn], 16)
        self._mark(("d", dn), self.dcnt[dn], rds, [out])
        return ins

    def record(self, gen):
        self.rec = []
        for _ in gen:
            pass
        r, self.rec = self.rec, None
        return r

    def emit_merged(self, a, b):
        def cost(x):
            if x[0] == "dma":
                return 1.5 if x[1] == "pool" else 3.0
            e = x[1]
            try:
                shp = x[4][0].shape
                n = 1
                for d in shp[1:]:
                    n *= d
            except Exception:
                n = 128
            if e == "pe":
                return 0.04 + n / 2400.0
            if e == "act":
                return 0.15 + n / 1200.0
            return 0.1 + n / 960.0

        def pos(st):
            c = [cost(x) for x in st]
            tot = sum(c) or 1.0
            out, acc = [], 0.0
            for ci in c:
                out.append((acc + 0.5 * ci) / tot)
                acc += ci
            return out
        pa, pb = pos(a), pos(b)
        items = [(0.9 * pa[i], 0, i, x) for i, x in enumerate(a)] + [(pb[j], 1, j, x) for j, x in enumerate(b)]
        items.sort(key=lambda t: (t[0], t[1], t[2]))
        for _, _, _, x in items:
            if x[0] == "op":
                self.op(x[1], x[2], x[3], x[4])
            else:
                self.dma(x[1], x[2], x[3], fn=x[4], extra_reads=x[5], **x[6])

    def barrier(self):
        ev = {}
        for e in self.eng:
            if self.ecnt[e]:
                ev[("e", e)] = self.ecnt[e]
        for dn, c in self.dcnt.items():
            ev[("d", dn)] = c
        for e in self.eng:
            for k, v in ev.items():
                if k == ("e", e):
                    continue
                if self.waited[e].get(k, 0) < v:
                    self.eng[e].wait_ge(self.semobj[k], v)
                    self.waited[e][k] = v

    def mm(self, out, lhsT, rhs, start=True, stop=True):
        return self.op("pe", lambda g: g.matmul(out, lhsT, rhs, start=start, stop=stop), [lhsT, rhs], [out])

    def tp(self, out, in_, ident):
        return self.op("pe", lambda g: g.transpose(out, in_, ident), [in_, ident], [out])

    def act(self, out, in_, func, bias=None, scale=None, accum_out=None, extra_reads=()):
        kw = {}
        rd = [in_] + list(extra_reads)
        if bias is not None:
            kw["bias"] = bias
            if not isinstance(bias, (int, float)):
                rd.append(bias)
        if scale is not None:
            kw["scale"] = scale
            if not isinstance(scale, (int, float)):
                rd.append(scale)
        wr = [out]
        if accum_out is not None:
            kw["accum_out"] = accum_out
            wr.append(accum_out)
        return self.op("act", lambda g: g.activation(out, in_, func, **kw), rd, wr)

    def tt(self, out, in0, in1, op, e="dve"):
        return self.op(e, lambda g: g.tensor_tensor(out, in0, in1, op), [in0, in1], [out])

    def ts(self, out, in0, s1, s2, op0, op1=None, e="dve", accum_out=None):
        rd = [in0] + [s for s in (s1, s2) if s is not None and not isinstance(s, (int, float))]
        wr = [out] + ([accum_out] if accum_out is not None else [])
        kw = {}
        if op1 is not None:
            kw["op1"] = op1
        if accum_out is not None:
            kw["accum_out"] = accum_out
        return self.op(e, lambda g: g.tensor_scalar(out, in0, s1, s2, op0, **kw), rd, wr)

    def stt(self, out, in0, scalar, in1, op0, op1, accum_out=None):
        rd = [in0, in1] + ([scalar] if not isinstance(scalar, (int, float)) else [])
        wr = [out] + ([accum_out] if accum_out is not None else [])
        kw = {"accum_out": accum_out} if accum_out is not None else {}
        return self.op("dve", lambda g: g.scalar_tensor_tensor(out, in0, scalar, in1, op0, op1, **kw), rd, wr)

    def cp(self, out, in_, e="dve"):
        if e == "act":
            return self.op("act", lambda g: g.copy(out, in_), [in_], [out])
        return self.op(e, lambda g: g.tensor_copy(out, in_), [in_], [out])

    def memset(self, ap, val, e="dve"):
        return self.op(e, lambda g: g.memset(ap, val), [], [ap])

    def red(self, out, in_, op, axis=AX.X):
        return self.op("dve", lambda g: g.tensor_reduce(out, in_, axis, op), [in_], [out])

    def recip(self, out, in_):
        return self.op("dve", lambda g: g.reciprocal(out, in_), [in_], [out])


def build_program(nseq=NSEQ_CORE, ntile=SEQ // 128, dbg=None, stop_after=None):
    nc = bass.Bass("TRN2", target_bir_lowering=False)
    es = ExitStack()
    ntok = nseq * ntile * 128
    with es:
        k = K(nc, es)
        def din(name, shape, dt=F32):
            return nc.dram_tensor(name, shape, dt, kind="ExternalInput").ap()

        x_d = din("x", [nseq * SEQ, D])
        c_d = din("c", [nseq, D])
        w_ada_d = din("w_ada", [D, 6 * D])
        b_ada_d = din("b_ada", [1, 6 * D])
        n1pre_d = din("norm1_pre", [1, D])
        n1post_d = din("norm1_post", [1, D])
        w_in_d = din("w_in", [D, IN_W])
        conv_a_d = din("conv_a_w", [3, D])
        conv_qk_d = din("conv_qk_w", [4, D])
        big_d = din("b_igate", [1, 8])
        bfg_d = din("b_fgate", [1, 8])
        mhw_d = din("mh_norm_w", [1, D])
        wsrc = {"w_in": w_in_d,
                "w_branch_a": din("w_branch_a", [D, D]),
                "w_branch_m": din("w_branch_m", [D, D]),
                "w_out": din("w_out", [D, D]),
                "peer_wq": din("peer_wq", [D, 2048])}
        n2pre_d = din("norm2_pre", [1, D])
        n2post_d = din("norm2_post", [1, D])
        sk_d = din("peer_subkeys", [16, 128, 128])
        pu_d = din("peer_u", [16384, D])
        pv_d = din("peer_v", [16384, D])
        cst_d = din("cst", [128, 1024])
        for nm in ("x", "c", "w_ada", "b_ada", "norm1_pre", "norm1_post", "w_in", "conv_a_w", "conv_qk_w", "b_igate",
                   "b_fgate", "mh_norm_w", "w_branch_a", "w_branch_m", "w_out", "peer_wq", "norm2_pre", "norm2_post",
                   "peer_subkeys", "peer_u", "peer_v", "cst"):
            k.tr[nm] = {"w": {}, "r": {}}
        out_d = k.dram("out", [nseq * SEQ, D], F32, "ExternalOutput")
        wsc_d = k.dram("wsc", [NBLK, 128, 8 * 512], BF16, "Internal")
        ada_d = k.dram("ada_sc", [nseq, 6 * D], F32, "Internal")
        ub_d = k.dram("ub_sc", [16384, D], BF16, "Internal")
        vb_d = k.dram("vb_sc", [16384, D], BF16, "Internal")
        dbg_d = None
        if dbg is not None:
            dbg_d = k.dram("dbg", [128, dbg], F32, "ExternalOutput")

        cst = k.sb("cst_sb", [128, 1024], F32)
        identf = cst[:, 0:128]
        tri2 = cst[:, 128:256]
        ones128 = cst[:, 256:384]
        rowmask = cst[:, 384:386]
        iota16 = cst[:, 576:592]
        onescol = cst[:, 592:593]
        identb_t = k.sb("identb", [128, 128], BF16)
        onesb_t = k.sb("onesb", [128, 1], BF16)
        wbuf = [k.sb("wbuf%d" % i, [128, 8, 512], BF16) for i in range(2)]
        rep = k.sb("rep", [128, 6, 1024], F32)
        Fs = [k.sb("F%d" % i, [128, D], F32) for i in range(6)]
        Hs = [k.sb("H%d" % i, [128, D], BF16) for i in range(8)]
        UA = k.sb("UA", [128, 8, 130], F32)
        UQ = k.sb("UQ", [128, 8, 131], F32)
        cwa = k.sb("cwa", [128, 3, 8], F32)
        cwq = k.sb("cwq", [128, 4, 8], F32)
        skT = k.sb("skT", [128, 16, 128], BF16)
        Cf = k.sb("Cf", [128, 4, 128], F32)
        Cb = k.sb("Cb", [128, 4, 128], BF16)
        nf = k.sb("nf", [128, 4], F32)
        nb = k.sb("nb", [128, 4], BF16)
        sm = k.sb("sm", [128, 256], F32)
        sm2 = k.sb("sm2", [128, 256], F32)
        bif = k.sb("bif", [128, 16], F32)
        qT = k.sb("qT", [128, 16, 128], BF16)
        scb = k.sb("scb", [128, 128], F32)
        cand = k.sb("cand", [128, 256], F32)
        candb = k.sb("candb", [128, 256], F32)
        v1 = k.sb("v1", [128, 8, 16], F32)
        v2 = k.sb("v2", [128, 8, 16], F32)
        i1 = k.sb("i1", [128, 8, 16], U32)
        i2 = k.sb("i2", [128, 8, 16], U32)
        cv = k.sb("cv", [128, 8, 16], F32)
        ci = k.sb("ci", [128, 8, 16], U32)
        tkf = [k.sb("tkf%d" % i, [128, 8, 16], F32) for i in range(6)]
        tku = [k.sb("tku%d" % i, [128, 8, 16], U32) for i in range(2)]
        zz = k.sb("zz", [128, 128], F32)
        aa = k.sb("aa", [128, 128], F32)

        PB = [k.ps("PB%d" % i, [128, 512], F32) for i in range(7)]
        PT = k.ps("PT", [128, 1024], BF16)
        es2 = ExitStack()
        stage = [k.sb("stage%d" % i, [128, 8, 512], F32, es=es2) for i in range(2)]

        k.dma("sp", cst[:], cst_d[:, :])
        k.cp(identb_t[:], identf)
        k.cp(onesb_t[:], onescol)
        identb = identb_t[:]

        for j in range(3):
            k.dma("sp", cwa[:, j, :], conv_a_d[j:j + 1, :].rearrange("o (ch p) -> p (o ch)", p=128), allow_slow_non_contiguous=True)
        for j in range(4):
            k.dma("sp", cwq[:, j, :], conv_qk_d[j:j + 1, :].rearrange("o (ch p) -> p (o ch)", p=128), allow_slow_non_contiguous=True)
        k.dma("sp", bif[:, 0:8], big_d.partition_broadcast(128))
        k.dma("sp", bif[:, 8:16], bfg_d.partition_broadcast(128))

        cT = sm[:, 0:8 * nseq].rearrange("p (b kc) -> p b kc", b=nseq)
        for b_ in range(nseq):
            k.dma("sp", cT[:, b_, :], c_d[b_:b_ + 1, :].rearrange("o (kc p) -> p (o kc)", p=128), allow_slow_non_contiguous=True)
        scT = sm2[:, 0:8 * nseq].rearrange("p (b kc) -> p b kc", b=nseq)
        k.act(scT, cT, AF.Silu)
        adat = Fs[0]
        ada_sb = rep
        ada_flat = ada_sb[0:nseq].rearrange("p a d -> p (a d)")
        nvec = Fs[1]
        w_ada_v = w_ada_d.rearrange("(kc p) n -> p kc n", p=128)
        for g in range(12):
            st = stage[g % 2]
            k.dma("sp", st[:], w_ada_v[:, :, g * 512:(g + 1) * 512])
            for kc in range(8):
                k.mm(PB[g % 2][0:nseq, :], scT[:, :, kc], st[:, kc, :], start=(kc == 0), stop=(kc == 7))
            k.cp(ada_flat[:, g * 512:(g + 1) * 512], PB[g % 2][0:nseq, :])
        bad = Fs[2]
        for g in range(6):
            k.dma("sp", Fs[2 + (g % 2)][0:nseq, :], b_ada_d[:, g * D:(g + 1) * D].partition_broadcast(nseq))
            k.tt(ada_sb[0:nseq, g, :], ada_sb[0:nseq, g, :], Fs[2 + (g % 2)][0:nseq, :], ALU.add)
        comb = Fs[4]
        res6 = [Fs[4], Fs[5], Hs[0], Hs[1]]
        nv = Fs[1]
        k.dma("sp", nv[0:nseq, :], n1pre_d.partition_broadcast(nseq))
        k.stt(Fs[4][0:nseq, :], ada_sb[0:nseq, 1, :], 1.0, nv[0:nseq, :], ALU.add, ALU.mult)
        k.dma("sp", ada_d[:, 0 * D:1 * D], Fs[4][0:nseq, :])
        k.dma("sp", ada_d[:, 1 * D:2 * D], ada_sb[0:nseq, 0, :])
        k.dma("sp", Fs[2][0:nseq, :], n1post_d.partition_broadcast(nseq))
        k.tt(Fs[5][0:nseq, :], ada_sb[0:nseq, 2, :], Fs[2][0:nseq, :], ALU.mult)
        k.dma("sp", ada_d[:, 2 * D:3 * D], Fs[5][0:nseq, :])
        k.dma("sp", Fs[3][0:nseq, :], n2pre_d.partition_broadcast(nseq))
        k.stt(Fs[0][0:nseq, :], ada_sb[0:nseq, 4, :], 1.0, Fs[3][0:nseq, :], ALU.add, ALU.mult)
        k.dma("sp", ada_d[:, 3 * D:4 * D], Fs[0][0:nseq, :])
        k.dma("sp", ada_d[:, 4 * D:5 * D], ada_sb[0:nseq, 3, :])
        k.dma("sp", nv[0:nseq, :], n2post_d.partition_broadcast(nseq))
        k.tt(Fs[4][0:nseq, :], ada_sb[0:nseq, 5, :], nv[0:nseq, :], ALU.mult)
        k.dma("sp", ada_d[:, 5 * D:6 * D], Fs[4][0:nseq, :])

        mhw = sm[:, 16:24]
        k.dma("sp", mhw, mhw_d.rearrange("o (kc p) -> p (o kc)", p=128), allow_slow_non_contiguous=True)
        for bi, (nm, c0, ncol) in enumerate(WBLOCKS):
            st = stage[bi % 2]
            wb = wbuf[bi % 2]
            src = wsrc[nm].rearrange("(kc p) n -> p kc n", p=128)
            k.dma("sp", st[:, :, 0:ncol], src[:, :, c0:c0 + ncol])
            if nm == "w_branch_m":
                for kc in range(8):
                    k.ts(wb[:, kc, 0:ncol], st[:, kc, 0:ncol], mhw[:, kc:kc + 1], None, ALU.mult)
            else:
                k.cp(wb[:, 0:4, 0:ncol], st[:, 0:4, 0:ncol], e="dve")
                k.cp(wb[:, 4:8, 0:ncol], st[:, 4:8, 0:ncol], e="act")
            k.dma("pool", wsc_d[bi].rearrange("p (kc n) -> p kc n", n=512)[:, :, 0:ncol], wb[:, :, 0:ncol])
        for (src_d, dst_d) in ((pu_d, ub_d), (pv_d, vb_d)):
            sv_ = src_d.rearrange("(c p r) d -> c p (r d)", p=128, r=4)
            dv_ = dst_d.rearrange("(c p r) d -> c p (r d)", p=128, r=4)
            for c_ in range(32):
                stf = stage[c_ % 2][:].rearrange("p a n -> p (a n)")
                wbf = wbuf[c_ % 2][:].rearrange("p a n -> p (a n)")
                k.dma("sp", stf, sv_[c_])
                k.cp(wbf[:, 0:2048], stf[:, 0:2048], e="dve")
                k.cp(wbf[:, 2048:4096], stf[:, 2048:4096], e="act")
                k.dma("pool", dv_[c_], wbf)
        for hc in range(16):
            st = stage[hc % 2]
            k.dma("sp", st[:, 0, 0:128], sk_d[hc])
            k.tp(PB[hc % 2][:, 0:128], st[:, 0, 0:128], identf)
            k.cp(skT[:, hc, :], PB[hc % 2][:, 0:128])
        k.barrier()
        es2.close()
        gbufs = [k.sb("gbuf%d" % i, [128, D], BF16) for i in range(NGBUF)]
        resident = {}
        for bi in RESIDENT:
            rt = k.sb("wres%d" % bi, [128, 8, 512], BF16)
            k.dma("sp", rt[:], wsc_d[bi].rearrange("p (kc n) -> p kc n", n=512))
            resident[bi] = rt
        X1p = [k.sb("X1p%d" % i, [128, D], F32) for i in range(2)]
        H2p = [k.sb("H2p%d" % i, [128, D], BF16) for i in range(2)]
        eidxp = [k.sb("eidxp%d" % i, [128, 128], U32) for i in range(2)]
        gatep = [k.sb("gatep%d" % i, [128, 128], F32) for i in range(2)]
        JKs = [k.sb("JKb%d" % i, [128, D], BF16) for i in range(3)]
        TMPt = k.sb("TMPt", [128, D], F32)
        Dg = [k.sb("Dg%d" % i, [128, 128], BF16) for i in range(4)]
        smb = k.sb("smb", [128, 8], F32)

        wstate = {"n": 0}

        def load_block(bi):
            if bi in resident:
                return resident[bi]
            wb = wbuf[wstate["n"] % 2]
            wstate["n"] += 1
            ncol = WBLOCKS[bi][2]
            k.dma("sp", wb[:, :, 0:ncol], wsc_d[bi].rearrange("p (kc n) -> p kc n", n=512)[:, :, 0:ncol])
            return wb

        def dump(ap, col0, ncols, parts=128):
            if dbg_d is not None:
                k.dma("sp", dbg_d[0:parts, col0:col0 + ncols], ap)

        for s in range(nseq):
            k.dma("sp", rep[:].rearrange("p a d -> p (a d)"), ada_d[s:s + 1, :].partition_broadcast(128))
            W1, SH1, G1N, W2, SH2, G2N = [rep[:, i, :] for i in range(6)]
            k.memset(UA[:, :, 0:2], 0.0)
            k.memset(UQ[:, :, 0:3], 0.0)
            k.memset(Cf[:], 0.0)
            k.memset(Cb[:], 0.0)
            k.memset(nf[:], 0.0)
            k.memset(nb[:], 0.0)
            def front(s, ti, par):
                r0 = s * SEQ + ti * 128
                eidx, gate = eidxp[par], gatep[par]
                X = X1p[par]
                k.dma("sp", X[:], x_d[r0:r0 + 128, :])
                ssq = sm[:, 32:33]
                k.act(Fs[0][:], X[:], AF.Square, accum_out=ssq)
                rstd = sm[:, 33:34]
                k.ts(sm[:, 34:35], ssq, 1.0 / D, EPS, ALU.mult, ALU.add)
                k.act(sm[:, 35:36], sm[:, 34:35], AF.Sqrt)
                k.recip(rstd, sm[:, 35:36])
                k.stt(Fs[0][:], X[:], rstd, W1, ALU.mult, ALU.mult)
                hb = Hs[0]
                k.tt(hb[:], Fs[0][:], SH1, ALU.add)
                for kc in range(8):
                    k.tp(PT[:, kc * 128:(kc + 1) * 128], hb[:, kc * 128:(kc + 1) * 128], identb)
                hT = Hs[1]
                k.cp(hT[:], PT[:])
                hTv = hT[:].rearrange("p (kc t) -> p kc t", t=128)
                if stop_after == "hT":
                    k.cp(Fs[1][:], hT[:])
                    dump(Fs[1][:], 0, 1024)
                    return
                yield
                CG, BG, XIN = Fs[1], Fs[2], Fs[3]
                SGA, SGM = Hs[2], Hs[3]
                Vb = Hs[4]
                SO = Fs[4]
                IFt = sm[:, 40:56]

                def fm_block(bi, dst_fn, pbi):
                    wb = load_block(bi)
                    pb = PB[pbi]
                    for j in range(4):
                        for kc in range(8):
                            k.mm(pb[:, j * 128:(j + 1) * 128], wb[:, kc, j * 128:(j + 1) * 128], hTv[:, kc, :],
                                 start=(kc == 0), stop=(kc == 7))
                    dst_fn(pb)

                def evac_plain(dst3, e):
                    def f(pb):
                        k.cp(dst3, pb[:].rearrange("p (j t) -> p j t", t=128), e=e)
                    return f

                def evac_sig(dst3):
                    def f(pb):
                        k.act(dst3, pb[:].rearrange("p (j t) -> p j t", t=128), AF.Sigmoid)
                    return f

                def v3(t, lo):
                    return t[:].rearrange("p (j t) -> p j t", t=128)[:, lo:lo + 4, :]

                fm_block(0, evac_plain(v3(CG, 0), "act"), 0)
                fm_block(1, evac_plain(v3(CG, 4), "dve"), 1)
                yield
                fm_block(2, evac_plain(v3(BG, 0), "act"), 0)
                fm_block(3, evac_plain(v3(BG, 4), "dve"), 1)
                yield
                fm_block(4, evac_plain(v3(XIN, 0), "act"), 0)
                fm_block(5, evac_plain(v3(XIN, 4), "dve"), 1)
                yield
                fm_block(6, evac_plain(UQ[:, 0:4, 3:131], "act"), 0)
                fm_block(7, evac_plain(UQ[:, 4:8, 3:131], "dve"), 1)
                yield
                for gi, bi in enumerate((8, 9, 10, 11)):
                    wb = load_block(bi)
                    pb = PB[2 + gi % 2]
                    for kc in range(8):
                        k.mm(pb[:], hTv[:, kc, :], wb[:, kc, :], start=(kc == 0), stop=(kc == 7))
                    if gi < 2:
                        k.cp(Vb[:, gi * 512:(gi + 1) * 512], pb[:], e="dve")
                    else:
                        k.act(SO[:, (gi - 2) * 512:(gi - 1) * 512], pb[:], AF.Sigmoid)
                wb = load_block(12)
                for kc in range(8):
                    k.mm(PB[4][:, 0:16], hTv[:, kc, :], wb[:, kc, 0:16], start=(kc == 0), stop=(kc == 7))
                k.tt(IFt, PB[4][:, 0:16], bif[:], ALU.add)
                yield
                fm_block(13, evac_sig(v3(SGA, 0)), 0)
                fm_block(14, evac_sig(v3(SGA, 4)), 1)
                yield
                fm_block(15, evac_sig(v3(SGM, 0)), 0)
                fm_block(16, evac_sig(v3(SGM, 4)), 1)
                if stop_after == "proj":
                    dump(CG[:], 0, 1024)
                    dump(UQ[:, :, 3:131], 1024, 1024)
                    k.cp(Fs[0][:], Vb[:])
                    dump(Fs[0][:], 2048, 1024)
                    dump(SO[:], 3072, 1024)
                    dump(IFt, 4096, 16)
                    k.cp(Fs[5][:], SGM[:])
                    dump(Fs[5][:], 4112, 1024)
                    return
                yield
                CG3 = CG[:].rearrange("p (j t) -> p j t", t=128)
                BG3 = BG[:].rearrange("p (j t) -> p j t", t=128)
                XIN3 = XIN[:].rearrange("p (j t) -> p j t", t=128)
                k.tt(UA[:, :, 2:130], CG3, XIN3, ALU.mult)
                T0 = Fs[0][:].rearrange("p (j t) -> p j t", t=128)
                T1 = CG3
                k.tt(T0, UA[:, :, 0:128], cwa[:, 0, :].unsqueeze(2).to_broadcast([128, 8, 128]), ALU.mult)
                k.tt(T1, UA[:, :, 1:129], cwa[:, 1, :].unsqueeze(2).to_broadcast([128, 8, 128]), ALU.mult)
                k.tt(T0, T0, T1, ALU.add)
                k.tt(T1, UA[:, :, 2:130], cwa[:, 2, :].unsqueeze(2).to_broadcast([128, 8, 128]), ALU.mult)
                k.tt(T0, T0, T1, ALU.add)
                yaT = Hs[5]
                k.tt(yaT[:].rearrange("p (j t) -> p j t", t=128), T0, BG3, ALU.mult)
                k.cp(UA[:, :, 0:2], UA[:, :, 128:130])
                yield
                T1 = XIN3
                k.tt(T0, UQ[:, :, 0:128], cwq[:, 0, :].unsqueeze(2).to_broadcast([128, 8, 128]), ALU.mult)
                for j in range(1, 4):
                    k.tt(T1, UQ[:, :, j:j + 128], cwq[:, j, :].unsqueeze(2).to_broadcast([128, 8, 128]), ALU.mult)
                    k.tt(T0, T0, T1, ALU.add)
                qkT = Hs[6]
                qk3 = qkT[:].rearrange("p (j t) -> p j t", t=128)
                k.act(qk3, T0, AF.Silu)
                k.cp(UQ[:, :, 0:3], UQ[:, :, 128:131])
                yield
                IG = IFt[:, 0:8]
                FG = IFt[:, 8:16]
                lf = sm[:, 56:64]
                k.act(lf, FG, AF.Exp, scale=-1.0)
                k.act(lf, lf, AF.Ln, bias=1.0)
                k.ts(lf, lf, -1.0, None, ALU.mult)
                k.mm(PB[4][:, 16:24], tri2, lf)
                k.mm(PB[4][:, 24:32], ones128, lf)
                Bt = sm[:, 64:72]
                BL = sm[:, 72:80]
                k.cp(Bt, PB[4][:, 16:24])
                k.cp(BL, PB[4][:, 24:32])
                u = sm[:, 88:96]
                eB = sm[:, 96:104]
                wk = sm[:, 104:112]
                eBL = sm[:, 112:116]
                k.tt(u, IG, Bt, ALU.subtract)
                k.tt(wk, u, BL, ALU.add)
                k.act(u, u, AF.Exp)
                k.act(wk, wk, AF.Exp)
                k.act(eB, Bt, AF.Exp)
                BLv = BL.rearrange("p (j two) -> p j two", two=2)
                k.act(eBL[0:64, :], BLv[0:64, :, 0], AF.Exp)
                k.act(eBL[64:128, :], BLv[64:128, :, 1], AF.Exp)
                for j in range(4):
                    k.tp(PT[:, j * 128:(j + 1) * 128], qk3[:, 4 + j, :], identb)
                kw = Hs[7]
                kw3 = kw[:, 0:512].rearrange("p (h d) -> p h d", d=64)
                k.tt(kw3, PT[:, 0:512].rearrange("p (h d) -> p h d", d=64), wk.unsqueeze(2).to_broadcast([128, 8, 64]), ALU.mult)
                yield
                Qm = [Fs[0][:, 0:256].bitcast(BF16).rearrange("p (j t) -> p j t", t=128),
                      Fs[0][:, 256:512].bitcast(BF16).rearrange("p (j t) -> p j t", t=128)]
                for e_ in range(2):
                    k.ts(Qm[e_], qk3[:, 0:4, :], rowmask[:, e_:e_ + 1], None, ALU.mult)
                SB = [PB[2], PB[3]]
                for h in range(8):
                    k.mm(SB[h // 4][:, (h % 4) * 128:(h % 4 + 1) * 128], qk3[:, 4 + h // 2, :], Qm[h % 2][:, h // 2, :])
                SwT = Hs[1]
                Sw3 = SwT[:].rearrange("p (h t) -> p h t", t=128)
                S3 = Fs[5][:].rearrange("p (h t) -> p h t", t=128)
                for g2 in range(2):
                    k.tt(S3[:, g2 * 4:(g2 + 1) * 4, :], SB[g2][:].rearrange("p (h t) -> p h t", t=128),
                         u[:, g2 * 4:(g2 + 1) * 4].unsqueeze(2).to_broadcast([128, 4, 128]), ALU.mult)
                k.tt(Sw3, S3, tri2.unsqueeze(1).to_broadcast([128, 8, 128]), ALU.mult)
                yield
                Vb3 = Vb[:].rearrange("p (h d) -> p h d", d=128)
                NUM = [PB[0], PB[1]]
                DEN = PB[4][:, 40:48]
                for h in range(8):
                    q_h = Qm[h % 2][:, h // 2, :]
                    numo = NUM[h // 4][:, (h % 4) * 128:(h % 4 + 1) * 128]
                    k.mm(numo, Sw3[:, h, :], Vb3[:, h, :], start=True, stop=False)
                    k.mm(numo, q_h, Cb[:, h // 2, :], start=False, stop=True)
                    k.mm(DEN[:, h:h + 1], Sw3[:, h, :], onesb_t[:], start=True, stop=False)
                    k.mm(DEN[:, h:h + 1], q_h, nb[:, h // 2:h // 2 + 1], start=False, stop=True)
                yield
                kwp = kw[:, 0:512].rearrange("p (j d) -> p j d", d=128)
                for h in range(8):
                    k.mm(PB[2 + h % 2][:, (h // 2) * 128:(h // 2 + 1) * 128], kwp[:, h // 2, :], Vb3[:, h, :])
                for j in range(4):
                    k.mm(PB[4][:, 48 + j:49 + j], kwp[:, j, :], onesb_t[:])
                k.tt(Cf[:], Cf[:], eBL.unsqueeze(2).to_broadcast([128, 4, 128]), ALU.mult)
                k.tt(Cf[0:64], Cf[0:64], PB[2][0:64, :].rearrange("p (j d) -> p j d", d=128), ALU.add)
                k.tt(Cf[64:128], Cf[64:128], PB[3][64:128, :].rearrange("p (j d) -> p j d", d=128), ALU.add)
                k.cp(Cb[:], Cf[:], e="act")
                k.tt(nf[:], nf[:], eBL, ALU.mult)
                k.tt(nf[:], nf[:], PB[4][:, 48:52], ALU.add)
                k.cp(nb[:], nf[:], e="act")
                yield
                dn = sm[:, 120:128]
                k.stt(dn, DEN, 0.125, eB, ALU.mult, ALU.mult)
                k.act(dn, dn, AF.Abs)
                k.ts(dn, dn, 1.0, None, ALU.max)
                k.recip(dn, dn)
                k.stt(dn, eB, 0.125, dn, ALU.mult, ALU.mult)
                HN = Fs[5]
                HN3 = HN[:].rearrange("p (h d) -> p h d", d=128)
                for g2 in range(2):
                    k.tt(HN3[:, g2 * 4:(g2 + 1) * 4, :], NUM[g2][:].rearrange("p (h d) -> p h d", d=128),
                         dn[:, g2 * 4:(g2 + 1) * 4].unsqueeze(2).to_broadcast([128, 4, 128]), ALU.mult)
                if stop_after == "mlstm":
                    dump(HN[:], 0, 1024)
                    k.cp(Fs[0][:], yaT[:])
                    dump(Fs[0][:], 1024, 1024)
                    return
                yield
                k.tt(Fs[0][:], HN[:], HN[:], ALU.mult)
                hss = sm[:, 128:136]
                k.red(hss, Fs[0][:].rearrange("p (h d) -> p h d", d=128), ALU.add)
                k.ts(hss, hss, 1.0 / 128, EPS, ALU.mult, ALU.add)
                k.act(hss, hss, AF.Sqrt)
                k.recip(hss, hss)
                k.tt(HN3, HN3, hss.unsqueeze(2).to_broadcast([128, 8, 128]), ALU.mult)
                ymb = Hs[0]
                k.tt(ymb[:], HN[:], SO[:], ALU.mult)
                for kc in range(8):
                    k.tp(PT[:, kc * 128:(kc + 1) * 128], ymb[:, kc * 128:(kc + 1) * 128], identb)
                ymT = Hs[4]
                k.cp(ymT[:], PT[:])
                ymT3 = ymT[:].rearrange("p (kc t) -> p kc t", t=128)
                yaT3 = yaT[:].rearrange("p (kc t) -> p kc t", t=128)
                yield
                MIX = Fs[0]
                MIX3 = MIX[:].rearrange("p (j t) -> p j t", t=128)

                def branch(bname, src3, sg, first):
                    for half in range(2):
                        wb = load_block(BLK[(bname, half * 512)])
                        pb = PB[2 + half]
                        for j in range(4):
                            for kc in range(8):
                                k.mm(pb[:, j * 128:(j + 1) * 128], wb[:, kc, j * 128:(j + 1) * 128], src3[:, kc, :],
                                     start=(kc == 0), stop=(kc == 7))
                        sg3 = sg[:].rearrange("p (j t) -> p j t", t=128)[:, half * 4:(half + 1) * 4, :]
                        dst = MIX3[:, half * 4:(half + 1) * 4, :]
                        p3 = pb[:].rearrange("p (j t) -> p j t", t=128)
                        if first:
                            k.tt(dst, p3, sg3, ALU.mult)
                        else:
                            t3 = Fs[1][:].rearrange("p (j t) -> p j t", t=128)[:, half * 4:(half + 1) * 4, :]
                            k.tt(t3, p3, sg3, ALU.mult)
                            k.tt(dst, dst, t3, ALU.add)

                branch("w_branch_a", yaT3, SGA, True)
                yield
                branch("w_branch_m", ymT3, SGM, False)
                yield
                mixT = Hs[2]
                k.cp(mixT[:], MIX[:], e="act")
                mixT3 = mixT[:].rearrange("p (kc t) -> p kc t", t=128)
                for half in range(2):
                    wb = load_block(BLK[("w_out", half * 512)])
                    for kc in range(8):
                        k.mm(PB[half][:], mixT3[:, kc, :], wb[:, kc, :], start=(kc == 0), stop=(kc == 7))
                yield
                Y = Fs[1]
                k.cp(Y[:, 0:512], PB[0][:], e="act")
                k.cp(Y[:, 512:1024], PB[1][:], e="dve")
                ssq2 = sm[:, 136:137]
                k.act(Fs[0][:], Y[:], AF.Square, accum_out=ssq2)
                k.ts(ssq2, ssq2, 1.0 / D, EPS, ALU.mult, ALU.add)
                k.act(ssq2, ssq2, AF.Sqrt)
                k.recip(ssq2, ssq2)
                k.stt(Fs[0][:], Y[:], ssq2, G1N, ALU.mult, ALU.mult)
                X1 = X1p[par]
                k.tt(X1[:], Fs[0][:], X[:], ALU.add)
                if stop_after == "sub1":
                    k.dma("sp", out_d[r0:r0 + 128, :], X1[:])
                    return
                yield
                ssq3 = sm[:, 137:138]
                k.act(Fs[0][:], X1[:], AF.Square, accum_out=ssq3)
                k.ts(ssq3, ssq3, 1.0 / D, EPS, ALU.mult, ALU.add)
                k.act(ssq3, ssq3, AF.Sqrt)
                k.recip(ssq3, ssq3)
                k.stt(Fs[0][:], X1[:], ssq3, W2, ALU.mult, ALU.mult)
                H2 = H2p[par]
                k.tt(H2[:], Fs[0][:], SH2, ALU.add)
                h2b = H2
                for kc in range(8):
                    k.tp(PT[:, kc * 128:(kc + 1) * 128], h2b[:, kc * 128:(kc + 1) * 128], identb)
                h2T = Hs[1]
                k.cp(h2T[:], PT[:])
                h2T3 = h2T[:].rearrange("p (kc t) -> p kc t", t=128)
                yield
                for g4 in range(4):
                    wb = load_block(BLK[("peer_wq", g4 * 512)])
                    pb = PB[2 + g4 % 2]
                    for j in range(4):
                        for kc in range(8):
                            k.mm(pb[:, j * 128:(j + 1) * 128], wb[:, kc, j * 128:(j + 1) * 128], h2T3[:, kc, :],
                                 start=(kc == 0), stop=(kc == 7))
                    k.cp(qT[:, g4 * 4:(g4 + 1) * 4, :], pb[:].rearrange("p (j t) -> p j t", t=128), e=("act" if g4 % 2 else "dve"))
                yield
                scv = [Fs[1][:].rearrange("p (j n) -> p j n", n=128), Fs[2][:].rearrange("p (j n) -> p j n", n=128)]
                tk0v = Fs[3][:].rearrange("p (h a b) -> p h a b", a=16, b=16)
                for g4 in range(4):
                    pb = PB[g4 % 2]
                    for j in range(4):
                        hc = g4 * 4 + j
                        k.mm(pb[:, j * 128:(j + 1) * 128], qT[:, hc, :], skT[:, hc, :])
                    k.cp(scv[g4 // 2][:, (g4 % 2) * 4:(g4 % 2) * 4 + 4, :], pb[:].rearrange("p (j n) -> p j n", n=128), e=("act" if g4 % 2 else "dve"))
                yield
                dv = k.eng["dve"]
                for h in range(8):
                    for half, (vv, ii) in enumerate(((v1, i1), (v2, i2))):
                        s_ = scv[(2 * h + half) // 8][:, (2 * h + half) % 8, :]
                        k.op("dve", lambda g, vv=vv, h=h, s_=s_: g.max(vv[:, h, 0:8], s_), [s_], [vv[:]])
                        k.op("dve", lambda g, vv=vv, ii=ii, h=h, s_=s_: g.max_index(ii[:, h, 0:8], vv[:, h, 0:8], s_), [s_, vv[:]], [ii[:]])
                        k.op("dve", lambda g, vv=vv, h=h, s_=s_: g.match_replace(scb[:], vv[:, h, 0:8], s_, NEG), [s_, vv[:]], [scb[:]])
                        k.op("dve", lambda g, vv=vv, h=h: g.max(vv[:, h, 8:16], scb[:]), [scb[:]], [vv[:]])
                        k.op("dve", lambda g, vv=vv, ii=ii, h=h: g.max_index(ii[:, h, 8:16], vv[:, h, 8:16], scb[:]), [scb[:], vv[:]], [ii[:]])
                    yield
                    c3 = cand[:].rearrange("p (a b) -> p a b", b=16)
                    k.tt(c3, v1[:, h, :].unsqueeze(2).to_broadcast([128, 16, 16]),
                         v2[:, h, :].unsqueeze(1).to_broadcast([128, 16, 16]), ALU.add)
                    k.op("dve", lambda g, h=h: g.max(cv[:, h, 0:8], cand[:]), [cand[:]], [cv[:]])
                    k.op("dve", lambda g, h=h: g.max_index(ci[:, h, 0:8], cv[:, h, 0:8], cand[:]), [cand[:], cv[:]], [ci[:]])
                    k.op("dve", lambda g, h=h: g.match_replace(candb[:], cv[:, h, 0:8], cand[:], NEG), [cand[:], cv[:]], [candb[:]])
                    k.op("dve", lambda g, h=h: g.max(cv[:, h, 8:16], candb[:]), [candb[:]], [cv[:]])
                    k.op("dve", lambda g, h=h: g.max_index(ci[:, h, 8:16], cv[:, h, 8:16], candb[:]), [candb[:], cv[:]], [ci[:]])
                yield
                ua, ub_ = tku[0], tku[1]
                k.ts(ua[:], ci[:], 4, None, ALU.logical_shift_right)
                k.ts(ub_[:], ci[:], 15, None, ALU.bitwise_and)
                af, bf_, i1f, i2f, isel, jsel = tkf
                k.cp(af[:], ua[:])
                k.cp(bf_[:], ub_[:])
                k.cp(i1f[:], i1[:])
                k.cp(i2f[:], i2[:])
                io4 = iota16.unsqueeze(1).unsqueeze(1).to_broadcast([128, 4, 16, 16])
                for (xf, tab, dst) in ((af, i1f, isel), (bf_, i2f, jsel)):
                    for hh in range(2):
                        hs4 = slice(hh * 4, hh * 4 + 4)
                        k.tt(tk0v, xf[:, hs4, :].unsqueeze(3).to_broadcast([128, 4, 16, 16]), io4, ALU.is_equal)
                        k.tt(tk0v, tk0v, tab[:, hs4, :].unsqueeze(2).to_broadcast([128, 4, 16, 16]), ALU.mult)
                        k.red(dst[:, hs4, :], tk0v, ALU.add)
                ef = af
                k.stt(ef[:], isel[:], 128.0, jsel[:], ALU.mult, ALU.add)
                k.cp(eidx[:].rearrange("p (h k) -> p h k", k=16), ef[:])
                yield
                gx = bf_
                k.tt(gx[:], cv[:], cv[:, :, 0:1].to_broadcast([128, 8, 16]), ALU.subtract)
                k.act(gx[:], gx[:], AF.Exp)
                gs = sm[:, 144:152]
                k.red(gs, gx[:], ALU.add)
                k.recip(gs, gs)
                k.tt(gate[:].rearrange("p (h k) -> p h k", k=16), gx[:], gs.unsqueeze(2).to_broadcast([128, 8, 16]), ALU.mult)
                if stop_after == "topk":
                    k.cp(Fs[0][:, 0:128], eidx[:])
                    dump(Fs[0][:, 0:128], 0, 128)
                    dump(gate[:], 128, 128)
                    dump(H2[:], 256, 1024)
                    return
                yield
            def back(s, ti, par):
                r0 = s * SEQ + ti * 128
                X1, H2, eidx, gate = X1p[par], H2p[par], eidxp[par], gatep[par]
                NG = len(gbufs)
                for sl in range(128):
                    gb = gbufs[sl % NG][:]
                    k.dma("pool", gb, ub_d, fn=lambda g, gb=gb, sl=sl, eidx=eidx: g.indirect_dma_start(
                        out=gb, out_offset=None, in_=ub_d[:, :],
                        in_offset=bass.IndirectOffsetOnAxis(ap=eidx[:, sl:sl + 1], axis=0)), extra_reads=[eidx[:]])
                    jk = JKs[sl % 3]
                    k.tt(jk[:], gb, H2[:], ALU.mult)
                    k.act(jk[:], jk[:], AF.Copy, accum_out=zz[:, sl:sl + 1])
                    if sl % 4 == 3:
                        yield
                k.tt(aa[:], zz[:], zz[:], ALU.mult)
                k.ts(aa[:], aa[:], 0.044715, 1.0, ALU.mult, ALU.add)
                k.tt(aa[:], aa[:], zz[:], ALU.mult)
                k.act(aa[:], aa[:], AF.Sigmoid, scale=1.5957691216057308)
                k.tt(aa[:], aa[:], zz[:], ALU.mult)
                k.tt(aa[:], aa[:], gate[:], ALU.mult)
                yield
                for sl in range(128):
                    gb = gbufs[sl % NG][:]
                    k.dma("pool", gb, vb_d, fn=lambda g, gb=gb, sl=sl, eidx=eidx: g.indirect_dma_start(
                        out=gb, out_offset=None, in_=vb_d[:, :],
                        in_offset=bass.IndirectOffsetOnAxis(ap=eidx[:, sl:sl + 1], axis=0)), extra_reads=[eidx[:]])
                    dg = Dg[sl % 4]
                    k.act(dg[:], identb, AF.Copy, scale=aa[:, sl:sl + 1])
                    k.mm(PB[5][:], dg[:], gb[:, 0:512], start=(sl == 0), stop=(sl == 127))
                    k.mm(PB[6][:], dg[:], gb[:, 512:1024], start=(sl == 0), stop=(sl == 127))
                    if sl % 4 == 3:
                        yield
                ssq4 = smb[:, 0:1]
                k.act(TMPt[:, 0:512], PB[5][:], AF.Square, accum_out=ssq4)
                k.act(TMPt[:, 512:1024], PB[6][:], AF.Square, accum_out=smb[:, 1:2])
                k.tt(ssq4, ssq4, smb[:, 1:2], ALU.add)
                k.ts(ssq4, ssq4, 1.0 / D, EPS, ALU.mult, ALU.add)
                k.act(ssq4, ssq4, AF.Sqrt)
                k.recip(ssq4, ssq4)
                k.stt(TMPt[:, 0:512], PB[5][:], ssq4, rep[:, 5, 0:512], ALU.mult, ALU.mult)
                k.stt(TMPt[:, 512:1024], PB[6][:], ssq4, rep[:, 5, 512:1024], ALU.mult, ALU.mult)
                k.tt(TMPt[:], TMPt[:], X1[:], ALU.add)
                k.dma("sp", out_d[r0:r0 + 128, :], TMPt[:])
                yield

            prev = None
            for ti in range(ntile):
                fa = k.record(front(s, ti, ti % 2))
                fb = k.record(back(*prev)) if (prev is not None and stop_after is None) else []
                if fb:
                    k.emit_merged(fa, fb)
                else:
                    k.emit_merged(fa, [])
                prev = (s, ti, ti % 2)
            if stop_after is None:
                k.emit_merged(k.record(back(*prev)), [])
        k.barrier()
    return nc


def make_consts():
    c = np.zeros((128, 1024), np.float32)
    c[:, 0:128] = np.eye(128, dtype=np.float32)
    s = np.arange(128)[:, None]
    t = np.arange(128)[None, :]
    c[:, 128:256] = (s <= t).astype(np.float32)
    c[:, 256:384] = 1.0
    c[:, 384] = (np.arange(128) < 64).astype(np.float32)
    c[:, 385] = (np.arange(128) >= 64).astype(np.float32)
    c[:, 576:592] = np.arange(16, dtype=np.float32)[None, :]
    c[:, 592] = 1.0
    return c


_PROG = {}


def kernel(x, c, w_ada, b_ada, norm1_pre, norm1_post, w_in, conv_a_w, conv_qk_w, b_igate, b_fgate, mh_norm_w,
           w_branch_a, w_branch_m, w_out, norm2_pre, norm2_post, peer_wq, peer_subkeys, peer_u, peer_v):
    f = lambda a: np.ascontiguousarray(np.asarray(a, dtype=np.float32))
    if "nc" not in _PROG:
        _PROG["nc"] = build_program()
    nc = _PROG["nc"]
    shared = {
        "w_ada": f(w_ada[0]), "b_ada": f(b_ada), "norm1_pre": f(norm1_pre), "norm1_post": f(norm1_post),
        "w_in": f(w_in[0]), "conv_a_w": f(conv_a_w[0]), "conv_qk_w": f(conv_qk_w[0]), "b_igate": f(b_igate),
        "b_fgate": f(b_fgate), "mh_norm_w": f(mh_norm_w), "w_branch_a": f(w_branch_a[0]),
        "w_branch_m": f(w_branch_m[0]), "w_out": f(w_out[0]), "peer_wq": f(peer_wq[0]),
        "norm2_pre": f(norm2_pre), "norm2_post": f(norm2_post),
        "peer_subkeys": f(peer_subkeys[0]).reshape(16, 128, 128), "peer_u": f(peer_u[0]), "peer_v": f(peer_v[0]),
        "cst": make_consts(),
    }
    xs = f(x)
    cs = f(c)
    in_maps = []
    for i in range(NCORES):
        m = dict(shared)
        m["x"] = xs[2 * i:2 * i + 2].reshape(2 * SEQ, D)
        m["c"] = cs[2 * i:2 * i + 2]
        in_maps.append(m)
    res = run_bass_kernel_spmd(nc, in_maps, core_ids=list(range(NCORES)))
    out = np.concatenate([r["out"].reshape(2, SEQ, D) for r in res.results], axis=0)
    return out.astype(np.float32)
```
